# Optimizing a Trainium2 kernel written in Bass

```python
import math
import jax, jax.numpy as jnp
from jax import lax
import numpy as np

D_MODEL = 1024
BATCH = 8
SEQ = 4096
DEPTH = 4

GRID_W = 64
CTX_LEN = 256
N_MIXERS = 3
N_LAYERS_A = (DEPTH + 2) // 3
N_LAYERS_B = (DEPTH + 1) // 3
N_LAYERS_C = DEPTH // 3
Q_BLOCK = 128
ROPE_BASE = 10000.0
EPS = 1e-6

MLA_HEADS = 16
MLA_Q_LORA = 512
MLA_KV_LORA = 256
MLA_NOPE = 64
MLA_ROPE = 32
MLA_V = 64
MLA_QK = MLA_NOPE + MLA_ROPE
MLA_DOWN = MLA_Q_LORA + MLA_KV_LORA + MLA_ROPE

DIFF_HEADS = 8
DIFF_HEAD_DIM = 64
DIFF_V_DIM = 2 * DIFF_HEAD_DIM

SG_CHUNK = 128
SG_WIDTH = 2 * D_MODEL
SG_GROUPS = 8
SG_GROUP_DIM = SG_WIDTH // SG_GROUPS

N_EXPERTS = 16
EXPERT_FF = D_MODEL
CAPACITY_FACTOR = 2

kernel_name = "hybrid_mla_diff_chunkmlp_ec_moe_dit"


def rms_norm(x, g):
    xf = x.astype(jnp.float32)
    y = xf * lax.rsqrt(jnp.mean(xf * xf, axis=-1, keepdims=True) + EPS)
    return (y * g).astype(x.dtype)


def layer_norm(x, g, b):
    xf = x.astype(jnp.float32)
    mu = jnp.mean(xf, axis=-1, keepdims=True)
    var = jnp.mean(jnp.square(xf - mu), axis=-1, keepdims=True)
    return ((xf - mu) * lax.rsqrt(var + EPS) * g + b).astype(x.dtype)


def modulate(h, shift, scale):
    return h * (1 + scale) + shift


def axial_rope(n_tokens, rot_dim):
    n_rows = n_tokens // GRID_W
    rows = jnp.repeat(jnp.arange(n_rows, dtype=jnp.float32), GRID_W)
    cols = jnp.tile(jnp.arange(GRID_W, dtype=jnp.float32), n_rows)
    n_freq = rot_dim // 4
    inv_freq = ROPE_BASE ** (-jnp.arange(n_freq, dtype=jnp.float32) / n_freq)
    ang = jnp.concatenate([rows[:, None] * inv_freq, cols[:, None] * inv_freq], axis=-1)
    return jnp.cos(ang), jnp.sin(ang)


def apply_rope(x, cos, sin):
    half = x.shape[-1] // 2
    x1, x2 = x[..., :half], x[..., half:]
    return jnp.concatenate([x1 * cos - x2 * sin, x1 * sin + x2 * cos], axis=-1)


def merge_heads(o):
    b, h, n, d = o.shape
    return o.transpose(0, 2, 1, 3).reshape(b, n, h * d)


def block_softmax_attention(q, k, v):
    b, h, n, dk = q.shape
    nb = n // Q_BLOCK
    scale = dk ** -0.5
    qb = jnp.moveaxis(q.reshape(b, h, nb, Q_BLOCK, dk), 2, 0)

    def one(qi):
        p = jax.nn.softmax(jnp.einsum('bhqd,bhkd->bhqk', qi, k) * scale, axis=-1)
        return jnp.einsum('bhqk,bhkd->bhqd', p.astype(v.dtype), v)

    out = lax.map(one, qb)
    return jnp.moveaxis(out, 0, 2).reshape(b, h, n, -1)


def block_diff_attention(q, k, v, lam):
    b, h, _, n, dk = q.shape
    nb = n // Q_BLOCK
    scale = dk ** -0.5
    qb = jnp.moveaxis(q.reshape(b, h, 2, nb, Q_BLOCK, dk), 3, 0)

    def one(qi):
        p = jax.nn.softmax(jnp.einsum('bhcqd,bhckd->bhcqk', qi, k) * scale, axis=-1)
        a = p[:, :, 0] - lam * p[:, :, 1]
        return jnp.einsum('bhqk,bhkd->bhqd', a.astype(v.dtype), v)

    out = lax.map(one, qb)
    return jnp.moveaxis(out, 0, 2).reshape(b, h, n, -1)


def mla_project_q(down, q_norm_g, w_uq, qn_g):
    b, n, _ = down.shape
    cq = rms_norm(down[..., :MLA_Q_LORA], q_norm_g)
    q = (cq @ w_uq).reshape(b, n, MLA_HEADS, MLA_QK)
    return rms_norm(q, qn_g).astype(jnp.float32).transpose(0, 2, 1, 3)


def mla_project_kv(down, kv_norm_g, w_ukv, kn_g):
    b, n, _ = down.shape
    ckv = rms_norm(down[..., MLA_Q_LORA:MLA_Q_LORA + MLA_KV_LORA], kv_norm_g)
    k_rope = down[..., MLA_Q_LORA + MLA_KV_LORA:]
    kv = (ckv @ w_ukv).reshape(b, n, MLA_HEADS, MLA_NOPE + MLA_V)
    k_nope, v = kv[..., :MLA_NOPE], kv[..., MLA_NOPE:]
    k = jnp.concatenate([k_nope, jnp.broadcast_to(k_rope[:, :, None, :], (b, n, MLA_HEADS, MLA_ROPE))], axis=-1)
    k = rms_norm(k, kn_g).astype(jnp.float32)
    return k.transpose(0, 2, 1, 3), v.transpose(0, 2, 1, 3)


def rope_tail(t, cos, sin):
    return jnp.concatenate([t[..., :MLA_NOPE], apply_rope(t[..., MLA_NOPE:], cos, sin)], axis=-1)


def mla_mixer(h_lat, h_ctx, w_in, q_norm_g, w_uq, kv_norm_g, w_ukv, qn_g, kn_g, w_out, cos, sin, need_ctx_out):
    d_lat = h_lat @ w_in
    d_ctx = h_ctx @ w_in
    q_lat = rope_tail(mla_project_q(d_lat, q_norm_g, w_uq, qn_g), cos, sin)
    k_lat, v_lat = mla_project_kv(d_lat, kv_norm_g, w_ukv, kn_g)
    k_lat = rope_tail(k_lat, cos, sin)
    k_ctx, v_ctx = mla_project_kv(d_ctx, kv_norm_g, w_ukv, kn_g)
    k_all = jnp.concatenate([k_ctx, k_lat], axis=2)
    v_all = jnp.concatenate([v_ctx, v_lat], axis=2)
    y_lat = merge_heads(block_softmax_attention(q_lat, k_all, v_all)) @ w_out
    y_ctx = None
    if need_ctx_out:
        q_ctx = mla_project_q(d_ctx, q_norm_g, w_uq, qn_g)
        y_ctx = merge_heads(block_softmax_attention(q_ctx, k_ctx, v_ctx)) @ w_out
    return y_lat, y_ctx


def diff_heads_qk(t, g):
    b, n, _ = t.shape
    t = rms_norm(t.reshape(b, n, DIFF_HEADS, 2, DIFF_HEAD_DIM), g)
    return t.astype(jnp.float32).transpose(0, 2, 3, 1, 4)


def diff_heads_v(t):
    b, n, _ = t.shape
    return t.reshape(b, n, DIFF_HEADS, DIFF_V_DIM).transpose(0, 2, 1, 3)


def diff_mixer(h_lat, h_ctx, w_in, qn_g, kn_g, lq1, lk1, lq2, lk2, sub_g, w_out, lam_init, cos, sin, need_ctx_out):
    D = D_MODEL
    qkv_lat = h_lat @ w_in
    q_lat = apply_rope(diff_heads_qk(qkv_lat[..., :D], qn_g), cos, sin)
    k_lat = apply_rope(diff_heads_qk(qkv_lat[..., D:2 * D], kn_g), cos, sin)
    v_lat = diff_heads_v(qkv_lat[..., 2 * D:])
    if need_ctx_out:
        qkv_ctx = h_ctx @ w_in
        kv_ctx = qkv_ctx[..., D:]
    else:
        kv_ctx = h_ctx @ w_in[:, D:]
    k_ctx = diff_heads_qk(kv_ctx[..., :D], kn_g)
    v_ctx = diff_heads_v(kv_ctx[..., D:])
    lam = (jnp.exp(jnp.sum(lq1.astype(jnp.float32) * lk1.astype(jnp.float32)))
           - jnp.exp(jnp.sum(lq2.astype(jnp.float32) * lk2.astype(jnp.float32))) + lam_init)

    def finish(o):
        return merge_heads(rms_norm(o, sub_g) * (1.0 - lam_init)) @ w_out

    k_all = jnp.concatenate([k_ctx, k_lat], axis=3)
    v_all = jnp.concatenate([v_ctx, v_lat], axis=2)
    y_lat = finish(block_diff_attention(q_lat, k_all, v_all, lam))
    y_ctx = None
    if need_ctx_out:
        q_ctx = diff_heads_qk(qkv_ctx[..., :D], qn_g)
        y_ctx = finish(block_diff_attention(q_ctx, k_ctx, v_ctx, lam))
    return y_lat, y_ctx


def chunk_gate(h, w_in, ln_g, ln_b, w_s, b_s, w_out):
    b, n, _ = h.shape
    z = jax.nn.gelu(h @ w_in)
    u, v = z[..., :SG_WIDTH], z[..., SG_WIDTH:]
    v = layer_norm(v, ln_g, ln_b).reshape(b, n // SG_CHUNK, SG_CHUNK, SG_GROUPS, SG_GROUP_DIM)
    v = jnp.einsum('gpq,bnqgc->bnpgc', w_s, v) + b_s.T[None, None, :, :, None]
    return (u * v.reshape(b, n, SG_WIDTH)) @ w_out


def chunk_mixer(h_lat, h_ctx, w_in, ln_g, ln_b, w_s, b_s, w_out, need_ctx_out):
    y_lat = chunk_gate(h_lat, w_in, ln_g, ln_b, w_s, b_s, w_out)
    y_ctx = chunk_gate(h_ctx, w_in, ln_g, ln_b, w_s, b_s, w_out) if need_ctx_out else None
    return y_lat, y_ctx


def ec_moe(h, router, w_gate, w_up, w_down):
    b, n, d = h.shape
    cap = CAPACITY_FACTOR * n // N_EXPERTS
    aff = jax.nn.softmax(jnp.einsum('bnd,de->bne', h, router).astype(jnp.float32), axis=-1)
    gates, idx = lax.top_k(aff.transpose(0, 2, 1), cap)
    xg = jax.vmap(lambda hb, ib: hb[ib])(h, idx)
    hid = jax.nn.silu(jnp.einsum('becd,edf->becf', xg, w_gate)) * jnp.einsum('becd,edf->becf', xg, w_up)
    y = jnp.einsum('becf,efd->becd', hid, w_down) * gates.astype(h.dtype)[..., None]
    return jax.vmap(lambda yb, ib: jnp.zeros((n, d), yb.dtype).at[ib.reshape(-1)].add(yb.reshape(-1, d)))(y, idx)


def setup_inputs(seed: int = 0) -> dict:
    key = jax.random.key(seed)
    ks = iter(jax.random.split(key, 64))
    D = D_MODEL

    def nrm(shape, scale=1.0):
        return jax.random.normal(next(ks), shape, jnp.float32) * scale

    def gain(shape):
        return 1.0 + nrm(shape, 0.05)

    return {
        "x": nrm((BATCH, SEQ, D)),
        "c": nrm((BATCH, D)),
        "ctx": nrm((BATCH, CTX_LEN, D)),
        "c_ctx": nrm((D,)),
        "ada_w": nrm((DEPTH, D, 6 * D), 0.5 * D ** -0.5),
        "ada_b": nrm((DEPTH, 6 * D), 0.02),
        "norm_mix_g": gain((DEPTH, D)),
        "norm_ffn_g": gain((DEPTH, D)),
        "mla_w_in": nrm((N_LAYERS_A, D, MLA_DOWN), D ** -0.5),
        "mla_q_norm_g": gain((N_LAYERS_A, MLA_Q_LORA)),
        "mla_w_uq": nrm((N_LAYERS_A, MLA_Q_LORA, MLA_HEADS * MLA_QK), MLA_Q_LORA ** -0.5),
        "mla_kv_norm_g": gain((N_LAYERS_A, MLA_KV_LORA)),
        "mla_w_ukv": nrm((N_LAYERS_A, MLA_KV_LORA, MLA_HEADS * (MLA_NOPE + MLA_V)), MLA_KV_LORA ** -0.5),
        "mla_qn_g": gain((N_LAYERS_A, MLA_QK)),
        "mla_kn_g": gain((N_LAYERS_A, MLA_QK)),
        "mla_w_out": nrm((N_LAYERS_A, MLA_HEADS * MLA_V, D), (MLA_HEADS * MLA_V) ** -0.5),
        "diff_w_in": nrm((N_LAYERS_B, D, 3 * D), D ** -0.5),
        "diff_qn_g": gain((N_LAYERS_B, DIFF_HEAD_DIM)),
        "diff_kn_g": gain((N_LAYERS_B, DIFF_HEAD_DIM)),
        "diff_lambda_q1": nrm((N_LAYERS_B, DIFF_HEAD_DIM), 0.1),
        "diff_lambda_k1": nrm((N_LAYERS_B, DIFF_HEAD_DIM), 0.1),
        "diff_lambda_q2": nrm((N_LAYERS_B, DIFF_HEAD_DIM), 0.1),
        "diff_lambda_k2": nrm((N_LAYERS_B, DIFF_HEAD_DIM), 0.1),
        "diff_sub_g": gain((N_LAYERS_B, DIFF_V_DIM)),
        "diff_w_out": nrm((N_LAYERS_B, D, D), D ** -0.5),
        "sg_w_in": nrm((N_LAYERS_C, D, 2 * SG_WIDTH), D ** -0.5),
        "sg_ln_g": gain((N_LAYERS_C, SG_WIDTH)),
        "sg_ln_b": nrm((N_LAYERS_C, SG_WIDTH), 0.02),
        "sg_w_s": nrm((N_LAYERS_C, SG_GROUPS, SG_CHUNK, SG_CHUNK), SG_CHUNK ** -0.5),
        "sg_b_s": 1.0 + nrm((N_LAYERS_C, SG_GROUPS, SG_CHUNK), 0.05),
        "sg_w_out": nrm((N_LAYERS_C, SG_WIDTH, D), SG_WIDTH ** -0.5),
        "moe_router": nrm((DEPTH, D, N_EXPERTS), D ** -0.5),
        "moe_w_gate": nrm((DEPTH, N_EXPERTS, D, EXPERT_FF), D ** -0.5),
        "moe_w_up": nrm((DEPTH, N_EXPERTS, D, EXPERT_FF), D ** -0.5),
        "moe_w_down": nrm((DEPTH, N_EXPERTS, EXPERT_FF, D), EXPERT_FF ** -0.5),
    }


def reference(x, c, ctx, c_ctx, ada_w, ada_b, norm_mix_g, norm_ffn_g,
              mla_w_in, mla_q_norm_g, mla_w_uq, mla_kv_norm_g, mla_w_ukv, mla_qn_g, mla_kn_g, mla_w_out,
              diff_w_in, diff_qn_g, diff_kn_g, diff_lambda_q1, diff_lambda_k1, diff_lambda_q2, diff_lambda_k2,
              diff_sub_g, diff_w_out,
              sg_w_in, sg_ln_g, sg_ln_b, sg_w_s, sg_b_s, sg_w_out,
              moe_router, moe_w_gate, moe_w_up, moe_w_down):
    n_lat = x.shape[1]
    cos_a, sin_a = axial_rope(n_lat, MLA_ROPE)
    cos_b, sin_b = axial_rope(n_lat, DIFF_HEAD_DIM)
    x_lat, x_ctx = x, ctx
    silu_c = jax.nn.silu(c)
    silu_cc = jax.nn.silu(c_ctx)[None]
    for i in range(DEPTH):
        kind = i % N_MIXERS
        j = i // N_MIXERS
        last = i == DEPTH - 1
        need_ctx_out = not last
        need_ctx_in = not (last and kind == 2)
        sh1, sc1, g1, sh2, sc2, g2 = jnp.split((silu_c @ ada_w[i] + ada_b[i])[:, None, :], 6, axis=-1)
        h_lat = modulate(rms_norm(x_lat, norm_mix_g[i]), sh1, sc1)
        h_ctx = None
        if need_ctx_in:
            csh1, csc1, cg1, csh2, csc2, cg2 = jnp.split((silu_cc @ ada_w[i] + ada_b[i])[:, None, :], 6, axis=-1)
            h_ctx = modulate(rms_norm(x_ctx, norm_mix_g[i]), csh1, csc1)
        if kind == 0:
            y_lat, y_ctx = mla_mixer(h_lat, h_ctx, mla_w_in[j], mla_q_norm_g[j], mla_w_uq[j], mla_kv_norm_g[j],
                                     mla_w_ukv[j], mla_qn_g[j], mla_kn_g[j], mla_w_out[j], cos_a, sin_a, need_ctx_out)
        elif kind == 1:
            lam_init = 0.8 - 0.6 * math.exp(-0.3 * i)
            y_lat, y_ctx = diff_mixer(h_lat, h_ctx, diff_w_in[j], diff_qn_g[j], diff_kn_g[j],
                                      diff_lambda_q1[j], diff_lambda_k1[j], diff_lambda_q2[j], diff_lambda_k2[j],
                                      diff_sub_g[j], diff_w_out[j], lam_init, cos_b, sin_b, need_ctx_out)
        else:
            y_lat, y_ctx = chunk_mixer(h_lat, h_ctx, sg_w_in[j], sg_ln_g[j], sg_ln_b[j], sg_w_s[j], sg_b_s[j],
                                       sg_w_out[j], need_ctx_out)
        x_lat = x_lat + g1 * y_lat
        x_lat = x_lat + g2 * ec_moe(modulate(rms_norm(x_lat, norm_ffn_g[i]), sh2, sc2),
                                    moe_router[i], moe_w_gate[i], moe_w_up[i], moe_w_down[i])
        if need_ctx_out:
            x_ctx = x_ctx + cg1 * y_ctx
            x_ctx = x_ctx + cg2 * ec_moe(modulate(rms_norm(x_ctx, norm_ffn_g[i]), csh2, csc2),
                                        moe_router[i], moe_w_gate[i], moe_w_up[i], moe_w_down[i])
    return x_lat
```

```python
import math
import os


def _os_environ_get(k, d):
    return os.environ.get(k, d)

from contextlib import ExitStack

import numpy as np
import concourse.bass as bass
import concourse.mybir as mybir
from concourse.bass_utils import run_bass_kernel_spmd

F32 = mybir.dt.float32
BF16 = mybir.dt.bfloat16
I32 = mybir.dt.int32
U32 = mybir.dt.uint32
ALU = mybir.AluOpType
AF = mybir.ActivationFunctionType

ENGS = ("pe", "act", "dve", "pool", "sp")
DMA_Q = ("sp", "act", "pool")

D = 1024
NCTX = 256
NLAT = 4096
NTOK = NCTX + NLAT
DEPTH = 4
EPS = 1e-6
NEXP = 16
CAP_L = 512
CAP_C = 32


class Res:
    __slots__ = ("lw", "rd")

    def __init__(self):
        self.lw = None
        self.rd = []


class Op:
    __slots__ = ("eng", "fn", "deps", "signal", "sigval", "is_dma", "dsem", "dval", "phase")

    def __init__(self, eng, fn, is_dma, phase):
        self.eng = eng
        self.fn = fn
        self.deps = []
        self.signal = False
        self.sigval = None
        self.is_dma = is_dma
        self.dsem = None
        self.dval = None
        self.phase = phase


class Prog:
    def __init__(self, nc, n_dma_sems=12):
        self.nc = nc
        self.ops = {e: [] for e in ENGS}
        self.nphase = 0
        self.n_dma_sems = n_dma_sems
        self.max_ops = 3000
        self.inline_wait = os.environ.get("NO_INLINE_WAIT") is None
        self.sems = {}
        self.dma_sems = {}

    def open(self, stack):
        nc = self.nc
        for e in ENGS:
            self.sems[e] = stack.enter_context(nc.semaphore("s_" + e))
        for q in DMA_Q:
            self.dma_sems[q] = [stack.enter_context(nc.semaphore("d_%s%d" % (q, i)))
                                for i in range(self.n_dma_sems)]
        self.sem_cnt = {e: 0 for e in ENGS}
        self.dma_cnt = {q: [0] * self.n_dma_sems for q in DMA_Q}
        self.dma_rr = {q: 0 for q in DMA_Q}
        self.waited = {}

    def op(self, eng, fn, reads=(), writes=(), dma=False):
        if max(len(v) for v in self.ops.values()) >= self.max_ops:
            self.flush()
        o = Op(eng, fn, dma, self.nphase)
        deps = []
        for r in reads:
            if r.lw is not None:
                deps.append(r.lw)
        for w in writes:
            if w.lw is not None:
                deps.append(w.lw)
            deps.extend(w.rd)
        seen = set()
        for d in deps:
            if id(d) in seen or d is o:
                continue
            seen.add(id(d))
            if (not d.is_dma) and d.phase != self.nphase:
                continue
            if (not d.is_dma) and (not dma) and d.eng == eng and eng == "pe":
                continue
            o.deps.append(d)
            d.signal = True
        for r in reads:
            r.rd.append(o)
        for w in writes:
            w.lw = o
            w.rd = []
        self.ops[eng].append(o)
        return o

    def dma(self, q, out, in_, reads=(), writes=(), **kw):
        return self.op(q, lambda e: e.dma_start(out=out, in_=in_, **kw), reads, writes, dma=True)

    def flush(self, final_wait=()):
        nc = self.nc
        ops = self.ops
        for e in ENGS:
            for o in ops[e]:
                if o.is_dma:
                    k = self.dma_rr[e]
                    self.dma_rr[e] = (k + 1) % self.n_dma_sems
                    self.dma_cnt[e][k] += 16
                    o.dsem = (e, k)
                    o.dval = self.dma_cnt[e][k]
                elif o.signal:
                    self.sem_cnt[e] += 1
                    o.sigval = self.sem_cnt[e]

        def emit_engine(e, engobj):
            waited = self.waited
            for o in ops[e]:
                need = {}
                for d in o.deps:
                    if d.is_dma:
                        key = ("d",) + d.dsem
                        val = d.dval
                    else:
                        key = ("c", d.eng)
                        val = d.sigval
                    if val > need.get(key, 0):
                        need[key] = val
                if o.is_dma:
                    key = ("d",) + o.dsem
                    if o.dval - 16 > need.get(key, 0):
                        need[key] = o.dval - 16
                todo = []
                for key, val in need.items():
                    wk = (e, key)
                    if waited.get(wk, 0) >= val:
                        continue
                    waited[wk] = val
                    sem = self.dma_sems[key[1]][key[2]] if key[0] == "d" else self.sems[key[1]]
                    todo.append((sem, val))
                inline = None
                if todo and self.inline_wait and not o.is_dma:
                    inline = todo.pop()
                for sem, val in todo:
                    engobj.wait_ge(sem, val)
                ins = o.fn(engobj)
                if inline is not None:
                    ins._wait_ge(inline[0], inline[1])
                if o.is_dma:
                    ins.then_inc(self.dma_sems[o.dsem[0]][o.dsem[1]], 16)
                elif o.signal:
                    ins.then_inc(self.sems[e], 1)
            if e == "sp":
                for o in final_wait:
                    engobj.wait_ge(self.dma_sems[o.dsem[0]][o.dsem[1]], o.dval)

        with nc.Block() as block:
            if ops["sp"] or final_wait:
                block.sync(lambda eng: emit_engine("sp", eng))
            if ops["act"]:
                block.scalar(lambda eng: emit_engine("act", eng))
            if ops["dve"]:
                block.vector(lambda eng: emit_engine("dve", eng))
            if ops["pool"]:
                block.gpsimd(lambda eng: emit_engine("pool", eng))
            if ops["pe"]:
                block.tensor(lambda eng: emit_engine("pe", eng))
        used = [e for e in ENGS if self.sem_cnt[e] > 0]
        if used and not final_wait:
            with nc.Block() as block:
                reg = {"sp": block.sync, "act": block.scalar, "dve": block.vector, "pool": block.gpsimd, "pe": block.tensor}
                for e in used:
                    reg[e](lambda eng, e=e: eng.sem_clear(self.sems[e]))
            for e in used:
                self.sem_cnt[e] = 0
            for wk in [wk for wk in self.waited if wk[1][0] == "c"]:
                del self.waited[wk]
        self.ops = {e: [] for e in ENGS}
        self.nphase += 1


class Defer:
    def __init__(self):
        self.p = None

    def push(self, fn):
        if self.p is not None:
            self.p()
        self.p = fn

    def flush(self):
        if self.p is not None:
            self.p()
        self.p = None


class Scope:
    def __init__(self, K):
        self.K = K
        self.st = ExitStack()
        self.n = 0

    def sb(self, shape, dt, name=None):
        self.n += 1
        nm = "%s_p%d_%d" % (name or "t", self.K.P.nphase, self.n)
        return self.st.enter_context(self.K.nc.sbuf_tensor(nm, list(shape), dt))

    def ps(self, shape, dt, name=None):
        self.n += 1
        nm = "%s_q%d_%d" % (name or "p", self.K.P.nphase, self.n)
        return self.st.enter_context(self.K.nc.psum_tensor(nm, list(shape), dt))

    def close(self, final_wait=()):
        self.K.P.flush(final_wait=final_wait)
        self.st.close()


WEIGHT_SPECS = [
    ("ada_w", [DEPTH, D, 6 * D]), ("ada_b", [DEPTH, 6 * D]),
    ("norm_mix_g", [DEPTH, D]), ("norm_ffn_g", [DEPTH, D]),
    ("mla_w_in", [2, D, 800]), ("mla_q_norm_g", [2, 512]), ("mla_w_uq", [2, 512, 1536]),
    ("mla_kv_norm_g", [2, 256]), ("mla_w_ukv", [2, 256, 2048]), ("mla_qn_g", [2, 96]),
    ("mla_kn_g", [2, 96]), ("mla_w_out", [2, D, D]),
    ("diff_w_in", [1, D, 3 * D]), ("diff_qn_g", [1, 64]), ("diff_kn_g", [1, 64]),
    ("diff_lambda_q1", [1, 64]), ("diff_lambda_k1", [1, 64]), ("diff_lambda_q2", [1, 64]),
    ("diff_lambda_k2", [1, 64]), ("diff_sub_g", [1, 128]), ("diff_w_out", [1, D, D]),
    ("sg_w_in", [1, D, 4096]), ("sg_ln_g", [1, 2048]), ("sg_ln_b", [1, 2048]),
    ("sg_w_s", [1, 8, 128, 128]), ("sg_b_s", [1, 8, 128]), ("sg_w_out", [1, 2048, D]),
    ("moe_router", [DEPTH, D, NEXP]), ("moe_w_gate", [DEPTH, NEXP, D, D]),
    ("moe_w_up", [DEPTH, NEXP, D, D]), ("moe_w_down", [DEPTH, NEXP, D, D]),
]


class Kern:
    def __init__(self, plan, debug=False, debug_layer=0):
        self.plan = plan
        self.debug = debug
        self.debug_layer = debug_layer
        self.nc = bass.Bass("TRN2", target_bir_lowering=False)
        self.P = Prog(self.nc)

    def xrows(self, gt, n=1):
        if gt < 2:
            return self.xc[gt * 128:(gt + n) * 128, :]
        return self.xl[(gt - 2) * 128:(gt - 2 + n) * 128, :]

    def hrows(self, gt, n=1):
        if gt < 2:
            return self.hc[gt * 128:(gt + n) * 128, :]
        return self.hl[(gt - 2) * 128:(gt - 2 + n) * 128, :]

    def xres(self, gt):
        return self.R_x[gt]

    def load_bc(self, S, layer, stream, slot, q="sp"):
        t = S.sb([128, D], F32, "bc")
        r = Res()
        src = self.modd[layer, stream, slot:slot + 1, :].partition_broadcast(128)
        self.P.dma(q, t[:], src, reads=[self.R_modd], writes=[r])
        return t, r

    def build(self):
        nc, P = self.nc, self.P
        dt_in = lambda name, shape, dt=F32: nc.dram_tensor(name, list(shape), dt, kind="ExternalInput").ap()
        self.xin = dt_in("xin", [NLAT, D])
        self.cin = dt_in("cin", [NCTX, D])
        self.cc = dt_in("cc", [128, 8, 2])
        self.W = {n: dt_in(n, s) for n, s in WEIGHT_SPECS}
        self.c_ident = dt_in("c_ident", [128, 128])
        self.c_rm96 = dt_in("c_rm96", [96, 96])
        self.c_rm128 = dt_in("c_rm128", [128, 128])
        self.c_cosB = dt_in("c_cosB", [128, NTOK])
        self.c_sinB = dt_in("c_sinB", [128, NTOK])
        self.c_cosA = dt_in("c_cosA", [96, NTOK])
        self.c_sinA = dt_in("c_sinA", [96, NTOK])
        self.xl = nc.dram_tensor("xl", [NLAT, D], F32, kind="ExternalOutput").ap()
        if self.debug:
            self.xc = nc.dram_tensor("xc", [NCTX, D], F32, kind="ExternalOutput").ap()
        else:
            self.xc = nc.dram_tensor("xc", [NCTX, D], F32).ap()
        self.hl = nc.dram_tensor("hl", [NLAT, D], BF16).ap()
        self.hc = nc.dram_tensor("hc", [NCTX, D], BF16).ap()
        self.modd = nc.dram_tensor("modd", [DEPTH, 2, 6, D], F32).ap()
        if self.debug:
            self.od = nc.dram_tensor("od", [D, NTOK], BF16, kind="ExternalOutput").ap()
            self.dbg_qt = nc.dram_tensor("dbg_qt", [128, NTOK], BF16, kind="ExternalOutput").ap()
            self.dbg_kt = nc.dram_tensor("dbg_kt", [128, NTOK], BF16, kind="ExternalOutput").ap()
            self.dbg_va = nc.dram_tensor("dbg_va", [128, 34 * 128], BF16, kind="ExternalOutput").ap()
        else:
            self.od = nc.dram_tensor("od", [D, NTOK], BF16).ap()
        self.R_x = [Res() for _ in range(34)]
        self.R_h = [Res() for _ in range(34)]
        self.R_modd = Res()
        self.R_od = Res()
        self.pd = nc.dram_tensor("pd", [2048, NTOK], BF16).ap()
        self.R_xl_all = Res()
        self.R_xc_all = Res()
        self.fence_d = nc.dram_tensor("fence_d", [128, 4], F32).ap()
        if self.debug:
            self.dbg_xe = nc.dram_tensor("dbg_xe", [128, 8 * 544], BF16, kind="ExternalOutput").ap()
            self.dbg_hid = nc.dram_tensor("dbg_hid", [128, 8 * 544], BF16, kind="ExternalOutput").ap()
            self.dbg_sg = nc.dram_tensor("dbg_sg", [128, 8 * 544], F32, kind="ExternalOutput").ap()
            self.dbg_xe2 = nc.dram_tensor("dbg_xe2", [128, 8 * 544], BF16, kind="ExternalOutput").ap()
            self.dbg_wg = nc.dram_tensor("dbg_wg", [128, 8 * 1024], BF16, kind="ExternalOutput").ap()
            self.dbg_aff = nc.dram_tensor("dbg_aff", [NEXP, NLAT], F32, kind="ExternalOutput").ap()
            self.dbg_idx = nc.dram_tensor("dbg_idx", [128, 4 * NEXP], I32, kind="ExternalOutput").ap()
            self.dbg_gate = nc.dram_tensor("dbg_gate", [128, 4 * NEXP], F32, kind="ExternalOutput").ap()

        with ExitStack() as top:
            P.open(top)
            self.top = top
            self.ident_f = top.enter_context(nc.sbuf_tensor("ident_f", [128, 128], F32))
            self.ident_b = top.enter_context(nc.sbuf_tensor("ident_b", [128, 128], BF16))
            self.ones_b = top.enter_context(nc.sbuf_tensor("ones_b", [128, 128], BF16))
            self.eps_t = top.enter_context(nc.sbuf_tensor("eps_t", [128, 4], F32))
            self.R_const = Res()
            P.op("dve", lambda e: e.memset(self.eps_t[:], EPS), [], [self.R_const])
            P.dma("sp", self.ident_f[:], self.c_ident, writes=[self.R_const])
            P.op("dve", lambda e: e.tensor_copy(out=self.ident_b[:], in_=self.ident_f[:]), [self.R_const], [self.R_const])
            P.op("dve", lambda e: e.memset(self.ones_b[:], 1.0), [], [self.R_const])
            for i in range(8):
                P.dma("sp", self.xl[i * 512:(i + 1) * 512, :], self.xin[i * 512:(i + 1) * 512, :],
                      writes=[self.R_x[2 + 4 * i + j] for j in range(4)])
            P.dma("sp", self.xc[:, :], self.cin[:, :], writes=[self.R_x[0], self.R_x[1]])
            self.phase_adaln()
            last_ops = []
            for step in self.plan:
                kind, layer = step
                if kind == "moe":
                    last_ops = self.phase_moe(layer)
                elif kind == "mix":
                    last_ops = self.phase_mix(layer)
            fin = []
            for r in self.R_x[2:]:
                if r.lw is not None and r.lw.is_dma:
                    fin.append(r.lw)
            fin = list({id(o): o for o in fin + [o for o in last_ops if o.is_dma]}.values())
            P.flush(final_wait=fin)
        return nc

    def phase_adaln(self):
        nc, P = self.nc, self.P
        S = Scope(self)
        ccs = S.sb([128, 8, 2], F32, "ccs")
        R_cc = Res()
        P.dma("sp", ccs[:], self.cc, writes=[R_cc])
        P.op("act", lambda e: e.activation(out=ccs[:], in_=ccs[:], func=AF.Silu), [R_cc], [R_cc])
        wst = [S.sb([128, 8, 512], F32, "adaw") for _ in range(2)]
        R_w = [Res(), Res()]
        pss = [S.ps([2, 512], F32, "adaps") for _ in range(2)]
        R_ps = [Res(), Res()]
        modv = S.sb([2, 6 * D], F32, "modv")
        R_mv = Res()
        bias = S.sb([2, 6 * D], F32, "adab")
        gm = S.sb([2, D], F32, "gm")
        gf = S.sb([2, D], F32, "gf")
        R_b = Res()
        n = 0
        for layer in range(DEPTH):
            P.dma("act", bias[:], self.W["ada_b"][layer:layer + 1, :].partition_broadcast(2), writes=[R_b])
            P.dma("act", gm[:], self.W["norm_mix_g"][layer:layer + 1, :].partition_broadcast(2), writes=[R_b])
            P.dma("act", gf[:], self.W["norm_ffn_g"][layer:layer + 1, :].partition_broadcast(2), writes=[R_b])
            for cb in range(12):
                b = n % 2
                n += 1
                src = self.W["ada_w"][layer, :, cb * 512:(cb + 1) * 512].rearrange("(k p) n -> p k n", p=128)
                P.dma("sp", wst[b][:], src, writes=[R_w[b]])
                for k in range(8):
                    P.op("pe", lambda e, b=b, k=k: e.matmul(pss[b][:, :], lhsT=ccs[:, k, :], rhs=wst[b][:, k, :],
                                                            start=(k == 0), stop=(k == 7)),
                         [R_cc, R_w[b]], [R_ps[b]])
                P.op("dve", lambda e, b=b, cb=cb: e.tensor_tensor(out=modv[:, cb * 512:(cb + 1) * 512], in0=pss[b][:, :],
                                                                   in1=bias[:, cb * 512:(cb + 1) * 512], op=ALU.add),
                     [R_ps[b], R_b], [R_mv])
            P.op("dve", lambda e: e.scalar_tensor_tensor(out=modv[:, D:2 * D], in0=modv[:, D:2 * D], scalar=1.0,
                                                         in1=gm[:], op0=ALU.add, op1=ALU.mult), [R_mv, R_b], [R_mv])
            P.op("dve", lambda e: e.scalar_tensor_tensor(out=modv[:, 4 * D:5 * D], in0=modv[:, 4 * D:5 * D], scalar=1.0,
                                                         in1=gf[:], op0=ALU.add, op1=ALU.mult), [R_mv, R_b], [R_mv])
            P.dma("sp", self.modd[layer].rearrange("s j d -> s (j d)"), modv[:], reads=[R_mv], writes=[self.R_modd])
        S.close()

    def prenorm_tile(self, T, gt, A, RA, B, RB):
        P = self.P
        i = T["i"]
        T["i"] += 1
        b = i % 2
        xt, R_xt = T["xt"][b]
        hn, R_hn = T["hn"][b]
        pt, R_pt = T["pt"][b]
        st, R_st = T["st"][b]
        junk, R_junk = T["junk"]
        P.dma("sp", xt[:], self.xrows(gt), reads=[self.R_x[gt]], writes=[R_xt])
        P.op("act", lambda e: e.activation(out=junk[:], in_=xt[:], func=AF.Square, accum_out=st[:, 0:1]),
             [R_xt], [R_junk, R_st])
        P.op("act", lambda e: e.activation(out=st[:, 1:2], in_=st[:, 0:1], func=AF.Sqrt, bias=self.eps_t[:, 0:1], scale=1.0 / D),
             [R_st, self.R_const], [R_st])
        P.op("dve", lambda e: e.reciprocal(out=st[:, 2:3], in_=st[:, 1:2]), [R_st], [R_st])
        P.op("dve", lambda e: e.scalar_tensor_tensor(out=hn[:], in0=xt[:], scalar=st[:, 2:3], in1=A[:],
                                                     op0=ALU.mult, op1=ALU.mult), [R_xt, R_st, RA], [R_hn])
        P.op("pool", lambda e: e.tensor_tensor(out=hn[:], in0=hn[:], in1=B[:], op=ALU.add), [R_hn, RB], [R_hn])
        for k in range(8):
            P.op("pe", lambda e, k=k: e.transpose(out=pt[:, k, :], in_=hn[:, k * 128:(k + 1) * 128],
                                                  identity=self.ident_f[:]), [R_hn, self.R_const], [R_pt])
        return hn, R_hn, pt, R_pt

    def prenorm_tiles(self, S, npt=2):
        T = {"i": 0}
        T["xt"] = [(S.sb([128, D], F32, "xt"), Res()) for _ in range(2)]
        T["hn"] = [(S.sb([128, D], F32, "hn"), Res()) for _ in range(2)]
        T["pt"] = [(S.ps([128, 8, 128], F32, "pt"), Res()) for _ in range(npt)]
        if npt == 1:
            T["pt"] = T["pt"] * 2
        T["st"] = [(S.sb([128, 4], F32, "st"), Res()) for _ in range(2)]
        T["junk"] = (S.sb([128, D], BF16, "junk"), Res())
        return T

    def phase_moe(self, layer):
        nc, P = self.nc, self.P
        with_ctx = layer != DEPTH - 1
        gts = list(range(0 if with_ctx else 2, 34))
        L = ExitStack()
        gates_p = L.enter_context(nc.sbuf_tensor("gates_p%d" % layer, [128, 4, NEXP], F32))
        idx_p = L.enter_context(nc.sbuf_tensor("idx_p%d" % layer, [128, 4, NEXP], I32))
        gates_pc = L.enter_context(nc.sbuf_tensor("gates_pc%d" % layer, [32, NEXP], F32))
        idx_pc = L.enter_context(nc.sbuf_tensor("idx_pc%d" % layer, [32, NEXP], I32))
        R_sel = Res()

        S = Scope(self)
        A_l, RA_l = self.load_bc(S, layer, 0, 4)
        B_l, RB_l = self.load_bc(S, layer, 0, 3, q="act")
        if with_ctx:
            A_c, RA_c = self.load_bc(S, layer, 1, 4)
            B_c, RB_c = self.load_bc(S, layer, 1, 3, q="act")
        T = self.prenorm_tiles(S)
        hb = [(S.sb([128, D], BF16, "hb"), Res()) for _ in range(2)]
        hTf = [(S.sb([128, 8, 128], F32, "hTf"), Res()) for _ in range(2)]
        rt = S.sb([128, 8, NEXP], F32, "router")
        R_rt = Res()
        P.dma("act", rt[:], self.W["moe_router"][layer].rearrange("(k p) e -> p k e", p=128), writes=[R_rt])
        small = S.ps([128, 512], F32, "small")
        lg = [(small[:, 16 * j:16 * j + 16], Res()) for j in range(2)]
        afp = [(small[0:NEXP, 32 + 128 * j:160 + 128 * j], Res()) for j in range(2)]
        ex = [(S.sb([128, NEXP + 2], F32, "ex"), Res()) for _ in range(2)]
        affT = S.sb([NEXP, NLAT], F32, "affT")
        R_aff = Res()
        affTc = S.sb([NEXP, NCTX], F32, "affTc")
        R_affc = Res()
        D1 = Defer()

        def back(b, gt, hn, R_hn, pt, R_pt):
                hbt, R_hb = hb[b]
                P.op("act", lambda e, hbt=hbt, hn=hn: e.activation(out=hbt[:], in_=hn[:], func=AF.Copy), [R_hn], [R_hb])
                P.dma("sp", self.hrows(gt), hbt[:], reads=[R_hb], writes=[self.R_h[gt]])
                hf, R_hf = hTf[b]
                P.op("dve", lambda e, hf=hf, pt=pt: e.tensor_copy(out=hf[:], in_=pt[:]), [R_pt], [R_hf])
                lgt, R_lg = lg[b]
                for k in range(8):
                    P.op("pe", lambda e, k=k, hf=hf, lgt=lgt: e.matmul(lgt, lhsT=hf[:, k, :], rhs=rt[:, k, :],
                                                                        start=(k == 0), stop=(k == 7)),
                         [R_hf, R_rt], [R_lg])
                ext, R_ex = ex[b]
                P.op("act", lambda e, ext=ext, lgt=lgt: e.activation(out=ext[:, 0:NEXP], in_=lgt, func=AF.Exp,
                                                                      accum_out=ext[:, NEXP:NEXP + 1]), [R_lg], [R_ex])
                P.op("dve", lambda e, ext=ext: e.reciprocal(out=ext[:, NEXP + 1:NEXP + 2], in_=ext[:, NEXP:NEXP + 1]),
                     [R_ex], [R_ex])
                P.op("dve", lambda e, ext=ext: e.tensor_scalar(out=ext[:, 0:NEXP], in0=ext[:, 0:NEXP],
                                                               scalar1=ext[:, NEXP + 1:NEXP + 2], scalar2=None, op0=ALU.mult),
                     [R_ex], [R_ex])
                apt, R_ap = afp[b]
                P.op("pe", lambda e, ext=ext, apt=apt: e.transpose(out=apt, in_=ext[:, 0:NEXP], identity=self.ident_f[:]),
                     [R_ex, self.R_const], [R_ap])
                if gt < 2:
                    P.op("act", lambda e, apt=apt, gt=gt: e.activation(out=affTc[:, gt * 128:(gt + 1) * 128], in_=apt,
                                                                        func=AF.Copy), [R_ap], [R_affc])
                else:
                    P.op("act", lambda e, apt=apt, gt=gt: e.activation(out=affT[:, (gt - 2) * 128:(gt - 1) * 128], in_=apt,
                                                                        func=AF.Copy), [R_ap], [R_aff])

        for n_i, gt in enumerate(gts):
            b = n_i % 2
            if gt < 2:
                hn, R_hn, pt, R_pt = self.prenorm_tile(T, gt, A_c, RA_c, B_c, RB_c)
            else:
                hn, R_hn, pt, R_pt = self.prenorm_tile(T, gt, A_l, RA_l, B_l, RB_l)
            D1.push(lambda b=b, gt=gt, hn=hn, R_hn=R_hn, pt=pt, R_pt=R_pt: back(b, gt, hn, R_hn, pt, R_pt))
        D1.flush()
        if self.debug and layer == self.debug_layer:
            P.dma("sp", self.dbg_aff, affT[:], reads=[R_aff])
        vals = S.sb([NEXP, CAP_L], F32, "vals")
        idxu = S.sb([NEXP, CAP_L], U32, "idxu")
        idxf = S.sb([NEXP, CAP_L], F32, "idxf")
        R_v, R_i = Res(), Res()
        for r in range(CAP_L // 8):
            sl = slice(8 * r, 8 * r + 8)
            P.op("dve", lambda e, sl=sl: e.max(out=vals[:, sl], in_=affT[:]), [R_aff], [R_v])
            P.op("dve", lambda e, sl=sl: e.max_index(out=idxu[:, sl], in_max=vals[:, sl], in_values=affT[:]),
                 [R_aff, R_v], [R_i])
            P.op("dve", lambda e, sl=sl: e.match_replace(out=affT[:], in_to_replace=vals[:, sl], in_values=affT[:],
                                                         imm_value=-1.0), [R_aff, R_v], [R_aff])
        P.op("dve", lambda e: e.tensor_copy(out=idxf[:], in_=idxu[:]), [R_i], [R_i])
        tp = small[:, 288:352].rearrange("p (c e) -> p c e", c=4)
        R_tp = Res()
        for c in range(4):
            P.op("pe", lambda e, c=c: e.transpose(out=tp[:, c, :], in_=vals[:, c * 128:(c + 1) * 128],
                                                  identity=self.ident_f[0:NEXP, 0:NEXP]), [R_v, self.R_const], [R_tp])
        P.op("dve", lambda e: e.tensor_copy(out=gates_p[:], in_=tp), [R_tp], [R_sel])
        for c in range(4):
            P.op("pe", lambda e, c=c: e.transpose(out=tp[:, c, :], in_=idxf[:, c * 128:(c + 1) * 128],
                                                  identity=self.ident_f[0:NEXP, 0:NEXP]), [R_i, self.R_const, R_sel], [R_tp])
        P.op("dve", lambda e: e.tensor_copy(out=idx_p[:], in_=tp), [R_tp], [R_sel])
        if with_ctx:
            valsc = S.sb([NEXP, CAP_C], F32, "valsc")
            idxuc = S.sb([NEXP, CAP_C], U32, "idxuc")
            idxfc = S.sb([NEXP, CAP_C], F32, "idxfc")
            R_vc, R_ic = Res(), Res()
            for r in range(CAP_C // 8):
                sl = slice(8 * r, 8 * r + 8)
                P.op("dve", lambda e, sl=sl: e.max(out=valsc[:, sl], in_=affTc[:]), [R_affc], [R_vc])
                P.op("dve", lambda e, sl=sl: e.max_index(out=idxuc[:, sl], in_max=valsc[:, sl], in_values=affTc[:]),
                     [R_affc, R_vc], [R_ic])
                P.op("dve", lambda e, sl=sl: e.match_replace(out=affTc[:], in_to_replace=valsc[:, sl], in_values=affTc[:],
                                                             imm_value=-1.0), [R_affc, R_vc], [R_affc])
            P.op("dve", lambda e: e.tensor_copy(out=idxfc[:], in_=idxuc[:]), [R_ic], [R_ic])
            tpc = small[0:32, 352:384].rearrange("p (c e) -> p c e", c=2)
            R_tpc = Res()
            P.op("pe", lambda e: e.transpose(out=tpc[:, 0, :], in_=valsc[:, :], identity=self.ident_f[0:NEXP, 0:NEXP]),
                 [R_vc, self.R_const], [R_tpc])
            P.op("pe", lambda e: e.transpose(out=tpc[:, 1, :], in_=idxfc[:, :], identity=self.ident_f[0:NEXP, 0:NEXP]),
                 [R_ic, self.R_const], [R_tpc])
            P.op("dve", lambda e: e.tensor_copy(out=gates_pc[:], in_=tpc[:, 0, :]), [R_tpc], [R_sel])
            P.op("dve", lambda e: e.tensor_copy(out=idx_pc[:], in_=tpc[:, 1, :]), [R_tpc], [R_sel])
        if self.debug and layer == self.debug_layer:
            P.dma("sp", self.dbg_idx, idx_p[:].rearrange("p c e -> p (c e)"), reads=[R_sel])
            P.dma("sp", self.dbg_gate, gates_p[:].rearrange("p c e -> p (c e)"), reads=[R_sel])
        S.close()

        import os as _os
        if _os.environ.get("DBG_SKIP_F2"):
            L.close()
            return []
        S = Scope(self)
        G_l, RG_l = self.load_bc(S, layer, 0, 5)
        if with_ctx:
            G_c, RG_c = self.load_bc(S, layer, 1, 5, q="act")
        NS = CAP_L + (CAP_C if with_ctx else 0)
        stage = [(S.sb([128, 8, D], F32, "wst"), Res()) for _ in range(2)]
        wbuf = [(S.sb([128, 8, D], BF16, "wb"), Res()) for _ in range(3)]
        gth = [(S.sb([128, D], BF16, "gth"), Res()) for _ in range(5)]
        tpp = [(S.ps([128, 8, 128], BF16, "tpp"), Res()) for _ in range(2)]
        XeT = [(S.sb([128, 8, NS], BF16, "XeT"), Res()) for _ in range(2)]
        sg = (S.sb([128, 8, NS], F32, "sg"), Res())
        hid = (S.sb([128, 8, NS], BF16, "hid"), Res())
        hps = [(S.ps([128, 512], F32, "hps"), Res()) for _ in range(3)]
        yps = [(S.ps([128, 512], F32, "yps"), Res()) for _ in range(2)]
        hpc = (S.ps([128, 8, CAP_C], F32, "hpc"), Res())
        ysb = [(S.sb([128, D], F32, "ysb"), Res()) for _ in range(2)]
        R_scat_prev = []
        nstage = [0]
        nw = [0]
        cast_eng = ["act", "dve", "act"]
        ng = [0]
        ny = [0]
        nh = [0]
        last_ops = []

        def load_w(name, e, which):
            sb_, R_s = stage[nstage[0] % 2]
            nstage[0] += 1
            wb_, R_wb = wbuf[nw[0] % 3]
            nw[0] += 1
            src = self.W[name][layer, e].rearrange("(k p) n -> p k n", p=128)
            P.dma("sp", sb_[:, 0:4, :], src[:, 0:4, :], writes=[R_s])
            P.dma("sp", sb_[:, 4:8, :], src[:, 4:8, :], writes=[R_s])
            ce = cast_eng[which]
            if ce == "act":
                P.op("act", lambda e_: e_.activation(out=wb_[:], in_=sb_[:], func=AF.Copy), [R_s], [R_wb])
            else:
                P.op(ce, lambda e_: e_.tensor_copy(out=wb_[:], in_=sb_[:]), [R_s], [R_wb])
            return wb_, R_wb

        import os as _os
        scat_box = [[]]

        chunks = [(c, 128) for c in range(4)] + ([(4, CAP_C)] if with_ctx else [])
        NE = int(_os.environ.get("DBG_NEXP", NEXP))

        def do_gather(e):
            xe, R_xe = XeT[e % 2]
            for c, npart in chunks:
                g, R_g = gth[ng[0] % 5]
                ng[0] += 1
                if c < 4:
                    off = bass.IndirectOffsetOnAxis(ap=idx_p[:, c, e:e + 1], axis=0)
                    srcd, rds = self.hl, [self.R_h[i] for i in range(2, 34)]
                else:
                    off = bass.IndirectOffsetOnAxis(ap=idx_pc[:, e:e + 1], axis=0)
                    srcd, rds = self.hc, [self.R_h[0], self.R_h[1]]
                P.op("pool", lambda e_, g=g, npart=npart, srcd=srcd, off=off: e_.indirect_dma_start(
                    out=g[0:npart, :], out_offset=None, in_=srcd, in_offset=off), [R_sel] + rds, [R_g], dma=True)
                tp_, R_tp_ = tpp[ng[0] % 2]
                for k in range(8):
                    P.op("pe", lambda e_, g=g, npart=npart, tp_=tp_, k=k: e_.transpose(
                        out=tp_[:, k, 0:npart], in_=g[0:npart, k * 128:(k + 1) * 128],
                        identity=self.ident_b[0:npart, 0:npart]), [R_g, self.R_const], [R_tp_])
                P.op("dve", lambda e_, tp_=tp_, xe=xe, c=c, npart=npart: e_.tensor_copy(
                    out=xe[:, :, c * 128:c * 128 + npart], in_=tp_[:, :, 0:npart]), [R_tp_], [R_xe])

        def do_expert(e):
            R_scat_prev = scat_box[0]
            xe, R_xe = XeT[e % 2]
            if self.debug and e == 0 and layer == self.debug_layer:
                P.dma("sp", self.dbg_xe, xe[:, :, :].rearrange("p k s -> p (k s)"), reads=[R_xe])
            if int(_os.environ.get("DBG_STAGE", 9)) < 2:
                return
            wg, R_wg = load_w("moe_w_gate", e, 0)
            sgt, R_sg = sg
            hpct, R_hpc = hpc
            for f in range(8):
                hp, R_hp = hps[nh[0] % 3]
                nh[0] += 1
                for k in range(8):
                    P.op("pe", lambda e_, hp=hp, k=k, f=f: e_.matmul(hp[:, :], lhsT=wg[:, k, f * 128:(f + 1) * 128],
                                                                      rhs=xe[:, k, 0:CAP_L], start=(k == 0), stop=(k == 7)),
                         [R_wg, R_xe], [R_hp])
                if with_ctx:
                    for k in range(8):
                        P.op("pe", lambda e_, k=k, f=f: e_.matmul(hpct[:, f, :], lhsT=wg[:, k, f * 128:(f + 1) * 128],
                                                                   rhs=xe[:, k, CAP_L:NS], start=(k == 0), stop=(k == 7)),
                             [R_wg, R_xe], [R_hpc])
                P.op("act", lambda e_, hp=hp, f=f: e_.activation(out=sgt[:, f, 0:CAP_L], in_=hp[:, :], func=AF.Silu),
                     [R_hp], [R_sg])
            if with_ctx:
                P.op("act", lambda e_: e_.activation(out=sgt[:, :, CAP_L:NS], in_=hpct[:, :, :], func=AF.Silu),
                     [R_hpc], [R_sg])
            if int(_os.environ.get("DBG_STAGE", 9)) < 3:
                if self.debug and e == 0:
                    P.dma("sp", self.dbg_sg, sgt[:, :, :].rearrange("p k s -> p (k s)"), reads=[R_sg])
                return
            wu, R_wu = load_w("moe_w_up", e, 1)
            hd_, R_hid = hid
            for f in range(8):
                hp, R_hp = hps[nh[0] % 3]
                nh[0] += 1
                for k in range(8):
                    P.op("pe", lambda e_, hp=hp, k=k, f=f: e_.matmul(hp[:, :], lhsT=wu[:, k, f * 128:(f + 1) * 128],
                                                                      rhs=xe[:, k, 0:CAP_L], start=(k == 0), stop=(k == 7)),
                         [R_wu, R_xe], [R_hp])
                if with_ctx:
                    for k in range(8):
                        P.op("pe", lambda e_, k=k, f=f: e_.matmul(hpct[:, f, :], lhsT=wu[:, k, f * 128:(f + 1) * 128],
                                                                   rhs=xe[:, k, CAP_L:NS], start=(k == 0), stop=(k == 7)),
                             [R_wu, R_xe], [R_hpc])
                P.op("dve", lambda e_, hp=hp, f=f: e_.tensor_tensor(out=hd_[:, f, 0:CAP_L], in0=hp[:, :],
                                                                     in1=sgt[:, f, 0:CAP_L], op=ALU.mult),
                     [R_hp, R_sg], [R_hid])
            if with_ctx:
                P.op("dve", lambda e_: e_.tensor_tensor(out=hd_[:, :, CAP_L:NS], in0=hpct[:, :, :],
                                                        in1=sgt[:, :, CAP_L:NS], op=ALU.mult), [R_hpc, R_sg], [R_hid])
            if self.debug and e == 0 and layer == self.debug_layer:
                P.dma("sp", self.dbg_hid, hd_[:, :, :].rearrange("p k s -> p (k s)"), reads=[R_hid])
                P.dma("sp", self.dbg_sg, sgt[:, :, :].rearrange("p k s -> p (k s)"), reads=[R_sg])
                P.dma("sp", self.dbg_xe2, xe[:, :, :].rearrange("p k s -> p (k s)"), reads=[R_xe])
                P.dma("sp", self.dbg_wg, wg[:, :, :].rearrange("p k s -> p (k s)"), reads=[R_wg])
            if int(_os.environ.get("DBG_STAGE", 9)) < 4:
                return
            if e + 1 < NE:
                do_gather(e + 1)
            wd, R_wd = load_w("moe_w_down", e, 2)
            R_scat_cur = []
            for c, npart in chunks:
                yt, R_y = ysb[ny[0] % 2]
                ny[0] += 1
                for half in range(2):
                    yp, R_yp = yps[half]
                    for f in range(8):
                        P.op("pe", lambda e_, yp=yp, f=f, c=c, npart=npart, half=half: e_.matmul(
                            yp[0:npart, :], lhsT=hd_[:, f, c * 128:c * 128 + npart],
                            rhs=wd[:, f, half * 512:(half + 1) * 512], start=(f == 0), stop=(f == 7)),
                            [R_hid, R_wd], [R_yp])
                    if c < 4:
                        gcol, Gt, RG = gates_p[:, c, e:e + 1], G_l, RG_l
                    else:
                        gcol, Gt, RG = gates_pc[:, e:e + 1], G_c, RG_c
                    P.op("dve", lambda e_, yt=yt, yp=yp, gcol=gcol, Gt=Gt, npart=npart, half=half: e_.scalar_tensor_tensor(
                        out=yt[0:npart, half * 512:(half + 1) * 512], in0=yp[0:npart, :], scalar=gcol,
                        in1=Gt[0:npart, half * 512:(half + 1) * 512], op0=ALU.mult, op1=ALU.mult),
                        [R_yp, R_sel, RG], [R_y])
                R_sc = Res()
                if c < 4:
                    off = bass.IndirectOffsetOnAxis(ap=idx_p[:, c, e:e + 1], axis=0)
                    dst = self.xl
                else:
                    off = bass.IndirectOffsetOnAxis(ap=idx_pc[:, e:e + 1], axis=0)
                    dst = self.xc
                o = P.op("pool", lambda e_, yt=yt, npart=npart, dst=dst, off=off: e_.indirect_dma_start(
                    out=dst, out_offset=off, in_=yt[0:npart, :], in_offset=None, compute_op=ALU.add),
                    [R_y, R_sel] + R_scat_prev, [R_sc], dma=True)
                R_scat_cur.append(R_sc)
                last_ops.append(o)
            scat_box[0] = R_scat_cur

        do_gather(0)
        for e in range(NE):
            do_expert(e)
        R_scat_prev = scat_box[0]
        fo = P.dma("pool", self.fence_d, self.eps_t[:], reads=R_scat_prev + [self.R_const], writes=self.R_x)
        last_ops = [fo]
        S.close()
        L.close()
        return last_ops


    def load_cast(self, S, src_ap, shape, stage, R_stage, q="sp", eng="dve", name="w", into=None):
        P = self.P
        if into is None:
            wt = S.sb(shape, BF16, name)
            R = Res()
        else:
            wt, R = into
        n = 1
        for d_ in shape[1:]:
            n *= d_
        sv = stage[0:shape[0], 0:n]
        if len(shape) == 3:
            sv = sv.rearrange("p (a b) -> p a b", a=shape[1])
        P.dma(q, sv, src_ap, writes=[R_stage])
        wap = wt[:]
        if eng == "act":
            P.op("act", lambda e: e.activation(out=wap, in_=sv, func=AF.Copy), [R_stage], [R])
        else:
            P.op(eng, lambda e: e.tensor_copy(out=wap, in_=sv), [R_stage], [R])
        return wt, R

    def normrope(self, *args):
        for _ in self.normrope_gen(*args):
            pass

    @staticmethod
    def run_zip(gens):
        gens = list(gens)
        while gens:
            for g in list(gens):
                try:
                    next(g)
                except StopIteration:
                    gens.remove(g)

    def normrope_gen(self, Wk, ps, R_ps, M, T, ones_ap, inv_n, gcol, R_g, rm, R_rm, cos_ap, sin_ap, R_tab, out_ap, R_out):
        P = self.P
        sq, R_sq = Wk["sq"]
        sm, R_sm = Wk["sm"]
        r, R_r = Wk["r"]
        xnf, R_xnf = Wk["xnf"]
        xnb, R_xnb = Wk["xnb"]
        rp, R_rp = Wk["rp"]
        t1, R_t1 = Wk["t1"]
        P.op("act", lambda e: e.activation(out=sq[0:M, 0:T], in_=ps, func=AF.Square), [R_ps], [R_sq])
        yield
        P.op("pe", lambda e: e.matmul(sm[0:M, 0:T], lhsT=ones_ap, rhs=sq[0:M, 0:T], start=True, stop=True),
             [R_sq, self.R_const], [R_sm])
        yield
        P.op("act", lambda e: e.activation(out=r[0:M, 0:T], in_=sm[0:M, 0:T], func=AF.Sqrt, bias=self.eps_t[0:M, 0:1],
                                           scale=inv_n), [R_sm, self.R_const], [R_r])
        yield
        P.op("dve", lambda e: e.reciprocal(out=r[0:M, 0:T], in_=r[0:M, 0:T]), [R_r], [R_r])
        P.op("dve", lambda e: e.scalar_tensor_tensor(out=xnf[0:M, 0:T], in0=ps, scalar=gcol, in1=r[0:M, 0:T],
                                                     op0=ALU.mult, op1=ALU.mult), [R_ps, R_g, R_r], [R_xnf])
        yield
        P.op("act", lambda e: e.activation(out=xnb[0:M, 0:T], in_=xnf[0:M, 0:T], func=AF.Copy), [R_xnf], [R_xnb])
        yield
        P.op("pe", lambda e: e.matmul(rp[0:M, 0:T], lhsT=rm, rhs=xnb[0:M, 0:T], start=True, stop=True),
             [R_xnb, R_rm], [R_rp])
        P.op("pool", lambda e: e.tensor_tensor(out=xnf[0:M, 0:T], in0=xnf[0:M, 0:T], in1=cos_ap, op=ALU.mult),
             [R_xnf, R_tab], [R_xnf])
        yield
        P.op("dve", lambda e: e.tensor_tensor(out=t1[0:M, 0:T], in0=rp[0:M, 0:T], in1=sin_ap, op=ALU.mult),
             [R_rp, R_tab], [R_t1])
        P.op("dve", lambda e: e.tensor_tensor(out=out_ap, in0=xnf[0:M, 0:T], in1=t1[0:M, 0:T], op=ALU.add),
             [R_xnf, R_t1], [R_out])
        yield

    def normrope_tiles(self, S):
        Wk = {}
        Wk["sq"] = (S.sb([128, 512], BF16, "sq"), Res())
        Wk["sm"] = (S.ps([128, 512], F32, "sm"), Res())
        Wk["r"] = (S.sb([128, 512], F32, "r"), Res())
        Wk["xnf"] = (S.sb([128, 512], F32, "xnf"), Res())
        Wk["xnb"] = (S.sb([128, 512], BF16, "xnb"), Res())
        Wk["rp"] = (S.ps([128, 512], F32, "rp"), Res())
        Wk["t1"] = (S.sb([128, 512], F32, "t1"), Res())
        return Wk

    def qblocks(self, need_ctx_out):
        qb = []
        if need_ctx_out:
            qb.append((0, 256, [0, 1]))
        for b in range(8):
            qb.append((256 + 512 * b, 512, list(range(34))))
        return qb

    def phase_outproj(self, layer, wname, j, need_ctx_out, src=None, nk=8):
        P = self.P
        S = Scope(self)
        G_l, RG_l = self.load_bc(S, layer, 0, 2)
        if need_ctx_out:
            G_c, RG_c = self.load_bc(S, layer, 1, 2, q="act")
        stage = S.sb([128, 8 * D], F32, "stg")
        R_stage = Res()
        src = self.od if src is None else src
        wo = S.sb([128, nk, D], BF16, "wo")
        R_wo = Res()
        wv_ = self.W[wname][j].rearrange("(k p) n -> p k n", p=128)
        for k0 in range(0, nk, 8):
            self.load_cast(S, wv_[:, k0:k0 + 8, :], [128, 8, D], stage, R_stage, eng="act", into=(wo[:, k0:k0 + 8, :], R_wo))
        ot = [(S.sb([128, nk, 128], BF16, "ot"), Res()) for _ in range(2)]
        xt = [(S.sb([128, D], F32, "xt"), Res()) for _ in range(2)]
        yps = [(S.ps([128, 512], F32, "yps"), Res()) for _ in range(4)]
        odv = src.rearrange("(k p) t -> p k t", p=128)
        gts = list(range(0 if need_ctx_out else 2, 34))
        for n_i, gt in enumerate(gts):
            b = n_i % 2
            o_t, R_o = ot[b]
            x_t, R_xt = xt[b]
            P.dma("sp", o_t[:], odv[:, :, gt * 128:(gt + 1) * 128], reads=[self.R_od], writes=[R_o])
            P.dma("act", x_t[:], self.xrows(gt), reads=[self.R_x[gt]], writes=[R_xt])
            Gt, RG = (G_c, RG_c) if gt < 2 else (G_l, RG_l)
            for half in range(2):
                yp, R_yp = yps[2 * b + half]
                for k in range(nk):
                    P.op("pe", lambda e, yp=yp, k=k, o_t=o_t, half=half: e.matmul(
                        yp[:, :], lhsT=o_t[:, k, :], rhs=wo[:, k, half * 512:(half + 1) * 512], start=(k == 0), stop=(k == nk - 1)),
                        [R_o, R_wo], [R_yp])
                hs = slice(half * 512, (half + 1) * 512)
                P.op("dve", lambda e, yp=yp, Gt=Gt, x_t=x_t, hs=hs: e.tensor_tensor(out=yp[:, :], in0=yp[:, :], in1=Gt[:, hs], op=ALU.mult),
                     [R_yp, RG], [R_yp])
                P.op("dve", lambda e, yp=yp, x_t=x_t, hs=hs: e.tensor_tensor(out=x_t[:, hs], in0=yp[:, :], in1=x_t[:, hs], op=ALU.add),
                     [R_yp, R_xt], [R_xt])
            P.dma("sp", self.xrows(gt), x_t[:], reads=[R_xt], writes=[self.R_x[gt]])
        S.close()

    def phase_mla(self, layer):
        nc, P = self.nc, self.P
        j = layer // 3
        need_ctx_out = layer != DEPTH - 1
        L = ExitStack()
        cqT = L.enter_context(nc.sbuf_tensor("cqT%d" % layer, [128, 4, NTOK], BF16))
        ckvT = L.enter_context(nc.sbuf_tensor("ckvT%d" % layer, [128, 2, NTOK], BF16))
        krT = L.enter_context(nc.sbuf_tensor("krT%d" % layer, [32, NTOK], BF16))
        R_cq = [Res() for _ in range(9)]
        R_ckv = [Res() for _ in range(9)]
        R_kr = [Res() for _ in range(9)]
        blocks = [(0, 256)] + [(256 + 512 * b, 512) for b in range(8)]

        S = Scope(self)
        A_l, RA_l = self.load_bc(S, layer, 0, 1)
        B_l, RB_l = self.load_bc(S, layer, 0, 0, q="act")
        A_c, RA_c = self.load_bc(S, layer, 1, 1)
        B_c, RB_c = self.load_bc(S, layer, 1, 0, q="act")
        T_ = self.prenorm_tiles(S, npt=1)
        stage = S.sb([128, 8 * 800], F32, "stg")
        R_stage = Res()
        win, R_win = self.load_cast(S, self.W["mla_w_in"][j].rearrange("(k p) n -> p k n", p=128), [128, 8, 800],
                                    stage, R_stage, eng="act", name="win")
        gq = S.sb([128, 4], F32, "gq")
        gkv = S.sb([128, 2], F32, "gkv")
        R_gl = Res()
        P.dma("act", gq[:], self.W["mla_q_norm_g"][j].rearrange("(a p) -> p a", p=128), writes=[R_gl], allow_slow_non_contiguous=True)
        P.dma("act", gkv[:], self.W["mla_kv_norm_g"][j].rearrange("(a p) -> p a", p=128), writes=[R_gl], allow_slow_non_contiguous=True)
        hTb = [(S.sb([128, 8, 512], BF16, "hTb"), Res()) for _ in range(2)]
        dps = [(S.ps([128, 512], F32, "dps"), Res()) for _ in range(4)]
        smp = (S.ps([128, 512], F32, "smp"), Res())
        sq = (S.sb([128, 512], BF16, "sq"), Res())
        rr = (S.sb([128, 512], F32, "rr"), Res())
        D2 = Defer()
        for bi, (t0, T) in enumerate(blocks):
            hb_, R_hb = hTb[bi % 2]
            for ti in range(T // 128):
                gt = t0 // 128 + ti
                if gt < 2:
                    hn, R_hn, pt, R_pt = self.prenorm_tile(T_, gt, A_c, RA_c, B_c, RB_c)
                else:
                    hn, R_hn, pt, R_pt = self.prenorm_tile(T_, gt, A_l, RA_l, B_l, RB_l)
                P.op("dve", lambda e, hb_=hb_, pt=pt, ti=ti: e.tensor_copy(out=hb_[:, :, ti * 128:(ti + 1) * 128], in_=pt[:]),
                     [R_pt], [R_hb])
            for grp, chunks, gain, dst, R_dst, nfeat in ((0, [0, 1, 2, 3], gq, cqT, R_cq, 512.0), (1, [4, 5], gkv, ckvT, R_ckv, 256.0)):
                smt, R_smt = smp
                for ci, cj in enumerate(chunks):
                    dp, R_dp = dps[ci]
                    for k in range(8):
                        P.op("pe", lambda e, dp=dp, k=k, cj=cj, hb_=hb_, T=T: e.matmul(
                            dp[:, 0:T], lhsT=win[:, k, cj * 128:(cj + 1) * 128], rhs=hb_[:, k, 0:T], start=(k == 0), stop=(k == 7)),
                            [R_win, R_hb], [R_dp])
                    sqt, R_sq = sq
                    P.op("act", lambda e, dp=dp, sqt=sqt, T=T: e.activation(out=sqt[:, 0:T], in_=dp[:, 0:T], func=AF.Square),
                         [R_dp], [R_sq])
                    P.op("pe", lambda e, sqt=sqt, smt=smt, T=T, ci=ci, n=len(chunks): e.matmul(
                        smt[:, 0:T], lhsT=self.ones_b[:, :], rhs=sqt[:, 0:T], start=(ci == 0), stop=(ci == n - 1)),
                        [R_sq, self.R_const], [R_smt])
                rt_, R_rr = rr
                P.op("act", lambda e, rt_=rt_, smt=smt, T=T, nfeat=nfeat: e.activation(
                    out=rt_[:, 0:T], in_=smt[:, 0:T], func=AF.Sqrt, bias=self.eps_t[:, 0:1], scale=1.0 / nfeat),
                    [R_smt, self.R_const], [R_rr])
                P.op("dve", lambda e, rt_=rt_, T=T: e.reciprocal(out=rt_[:, 0:T], in_=rt_[:, 0:T]), [R_rr], [R_rr])
                for ci, cj in enumerate(chunks):
                    dp, R_dp = dps[ci]
                    P.op("dve", lambda e, dp=dp, ci=ci, gain=gain, rt_=rt_, dst=dst, t0=t0, T=T: e.scalar_tensor_tensor(
                        out=dst[:, ci, t0:t0 + T], in0=dp[:, 0:T], scalar=gain[:, ci:ci + 1], in1=rt_[:, 0:T],
                        op0=ALU.mult, op1=ALU.mult), [R_dp, R_gl, R_rr], [R_dst[bi]])
            dp, R_dp = dps[0]
            for k in range(8):
                P.op("pe", lambda e, dp=dp, k=k, hb_=hb_, T=T: e.matmul(dp[0:32, 0:T], lhsT=win[:, k, 768:800], rhs=hb_[:, k, 0:T],
                                                                         start=(k == 0), stop=(k == 7)), [R_win, R_hb], [R_dp])
            P.op("act", lambda e, dp=dp, t0=t0, T=T: e.activation(out=krT[0:32, t0:t0 + T], in_=dp[0:32, 0:T], func=AF.Copy),
                 [R_dp], [R_kr[bi]])
        S.close()

        if int(_os_environ_get("DBG_MLA", 3)) < 2:
            L.close()
            return []
        S = Scope(self)
        stage = S.sb([128, 4 * 1536], F32, "stg")
        R_stage = Res()
        wuq, R_wuq = self.load_cast(S, self.W["mla_w_uq"][j].rearrange("(k p) n -> p k n", p=128), [128, 4, 1536],
                                    stage, R_stage, eng="act", name="wuq")
        wukv, R_wukv = self.load_cast(S, self.W["mla_w_ukv"][j].rearrange("(k p) n -> p k n", p=128), [128, 2, 2048],
                                      stage, R_stage, eng="act", name="wukv")
        wkp = S.sb([128, 2, 16, 96], BF16, "wkp")
        R_wkp = Res()
        P.op("pool", lambda e: e.memset(wkp[:], 0.0), [], [R_wkp])
        wv4 = wukv[:, :, :].rearrange("p a (h c) -> p a h c", h=16)
        for a in range(2):
            P.op("dve", lambda e, a=a: e.tensor_copy(out=wkp[:, a, :, 0:64], in_=wv4[:, a, :, 0:64]), [R_wukv, R_wkp], [R_wkp])
        rm, R_rm = self.load_cast(S, self.c_rm96, [96, 96], stage, R_stage, name="rm96")
        sel = S.sb([32, 96], BF16, "sel")
        R_sel96 = Res()
        P.op("pool", lambda e: e.memset(sel[:], 0.0), [], [R_sel96])
        P.op("dve", lambda e: e.tensor_copy(out=sel[:, 64:96], in_=self.ident_b[0:32, 0:32]), [self.R_const, R_sel96], [R_sel96])
        gqn = S.sb([96, 1], F32, "gqn")
        gkn = S.sb([96, 1], F32, "gkn")
        R_gh = Res()
        P.dma("act", gqn[:], self.W["mla_qn_g"][j].rearrange("(p o) -> p o", o=1), writes=[R_gh])
        P.dma("act", gkn[:], self.W["mla_kn_g"][j].rearrange("(p o) -> p o", o=1), writes=[R_gh])
        sps = [(S.ps([128, 512], F32, "sps"), Res()) for _ in range(7)]
        Wk = {}
        Wk["sq"] = (S.sb([128, 512], BF16, "sq"), Res())
        Wk["sm"] = sps[1]
        Wk["r"] = (S.sb([128, 512], F32, "r"), Res())
        Wk["xnf"] = (S.sb([128, 512], F32, "xnf"), Res())
        Wk["xnb"] = (S.sb([128, 512], BF16, "xnb"), Res())
        Wk["rp"] = sps[2]
        Wk["t1"] = (S.sb([128, 512], F32, "t1"), Res())
        Wk2 = {"sq": (S.sb([128, 512], BF16, "sq2"), Res()), "sm": sps[4], "r": (S.sb([128, 512], F32, "r2"), Res()),
               "xnf": (S.sb([128, 512], F32, "xnf2"), Res()), "xnb": (S.sb([128, 512], BF16, "xnb2"), Res()), "rp": sps[5],
               "t1": (S.sb([128, 512], F32, "t12"), Res())}
        cs = [(S.sb([96, 512], F32, "cos"), S.sb([96, 512], F32, "sin"), Res()) for _ in range(2)]
        QT = (S.sb([96, NTOK], BF16, "QT"), Res())
        KT = (S.sb([96, NTOK], BF16, "KT"), Res())
        Va = (S.sb([128, 34, 128], BF16, "Va"), Res())
        P.op("pool", lambda e: e.memset(Va[0][:, :, 64:128], 1.0), [], [Va[1]])
        gps = sps[0]
        acc = (S.ps([128, 512], F32, "acc"), Res())
        NB, GRP = 6, 3
        pts = [(S.sb([128, 512], BF16, "pt"), Res()) for _ in range(NB)]
        rec = (S.sb([64, 512], F32, "rec"), Res())
        otl = [(S.sb([64, 512], BF16, "ot"), Res()) for _ in range(2)]
        ncs = [0]
        scale = 96.0 ** -0.5

        def do_head(h):
            qt, R_qt = QT
            kt, R_kt = KT
            va, R_va = Va
            gp, R_gp = gps
            for bi, (t0, T) in enumerate(blocks):
                cos_t, sin_t, R_tab = cs[ncs[0] % 2]
                ncs[0] += 1
                P.dma("sp", cos_t[:, 0:T], self.c_cosA[:, t0:t0 + T], writes=[R_tab])
                P.dma("sp", sin_t[:, 0:T], self.c_sinA[:, t0:t0 + T], writes=[R_tab])
                def kchain(bi=bi, t0=t0, T=T, cos_t=cos_t, sin_t=sin_t, R_tab=R_tab):
                    for a in range(2):
                        P.op("pe", lambda e, a=a: e.matmul(gp[0:96, 0:T], lhsT=wkp[:, a, h, :], rhs=ckvT[:, a, t0:t0 + T],
                                                           start=(a == 0), stop=False), [R_wkp, R_ckv[bi]], [R_gp])
                    P.op("pe", lambda e: e.matmul(gp[0:96, 0:T], lhsT=sel[:, :], rhs=krT[0:32, t0:t0 + T],
                                                  start=False, stop=True), [R_sel96, R_kr[bi]], [R_gp])
                    yield
                    yield from self.normrope_gen(Wk, gp[0:96, 0:T], R_gp, 96, T, self.ones_b[0:96, 0:96], 1.0 / 96, gkn[:, 0:1], R_gh,
                                                 rm[:, :], R_rm, cos_t[:, 0:T], sin_t[:, 0:T], R_tab, kt[:, t0:t0 + T], R_kt)

                def qchain(bi=bi, t0=t0, T=T, cos_t=cos_t, sin_t=sin_t, R_tab=R_tab):
                    gq_, R_gq_ = sps[3]
                    for a in range(4):
                        P.op("pe", lambda e, a=a: e.matmul(gq_[0:96, 0:T], lhsT=wuq[:, a, h * 96:(h + 1) * 96],
                                                           rhs=cqT[:, a, t0:t0 + T], start=(a == 0), stop=(a == 3)),
                             [R_wuq, R_cq[bi]], [R_gq_])
                    yield
                    yield from self.normrope_gen(Wk2, gq_[0:96, 0:T], R_gq_, 96, T, self.ones_b[0:96, 0:96], 1.0 / 96, gqn[:, 0:1], R_gh,
                                                 rm[:, :], R_rm, cos_t[:, 0:T], sin_t[:, 0:T], R_tab, qt[:, t0:t0 + T], R_qt)
                chains = [kchain()]
                if bi > 0 or need_ctx_out:
                    chains.append(qchain())
                self.run_zip(chains)
                for ti in range(T // 128):
                    kc = t0 // 128 + ti
                    for a in range(2):
                        P.op("pe", lambda e, a=a, kc=kc, ti=ti: e.matmul(
                            gp[:, ti * 64:(ti + 1) * 64], lhsT=ckvT[:, a, kc * 128:(kc + 1) * 128], rhs=wv4[:, a, h, 64:128],
                            start=(a == 0), stop=(a == 1)), [R_wukv, R_ckv[bi]], [R_gp])
                nt = T // 128
                P.op("act", lambda e, t0=t0, nt=nt: e.activation(
                    out=va[:, t0 // 128:t0 // 128 + nt, 0:64], in_=gp[:, 0:nt * 64].rearrange("p (t c) -> p t c", t=nt),
                    func=AF.Copy), [R_gp], [R_va])
            if self.debug and h == 0:
                P.dma("sp", self.dbg_qt[0:96, :], qt[:, :], reads=[R_qt])
                P.dma("sp", self.dbg_kt[0:96, :], kt[:, :], reads=[R_kt])
                P.dma("sp", self.dbg_va, va[:, :, :].rearrange("p a b -> p (a b)"), reads=[R_va])
            for qi, (q0, T, kcs) in enumerate(self.qblocks(need_ctx_out)):
                ac, R_ac = acc
                n = len(kcs)

                def score(i):
                    sp_, R_sp = sps[1 + i % NB]
                    kc = kcs[i]
                    P.op("pe", lambda e, sp_=sp_, kc=kc, T=T, q0=q0: e.matmul(sp_[:, 0:T], lhsT=kt[:, kc * 128:(kc + 1) * 128],
                                                                   rhs=qt[:, q0:q0 + T], start=True, stop=True),
                         [R_kt, R_qt], [R_sp])
                    pt_, R_pt_ = pts[i % NB]
                    P.op("act", lambda e, sp_=sp_, pt_=pt_, T=T: e.activation(out=pt_[:, 0:T], in_=sp_[:, 0:T], func=AF.Exp, scale=scale),
                         [R_sp], [R_pt_])

                def pv(i, first, last):
                    pt_, R_pt_ = pts[i % NB]
                    kc = kcs[i]
                    P.op("pe", lambda e, pt_=pt_, kc=kc, T=T, ac=ac, first=first, last=last: e.matmul(
                        ac[:, 0:T], lhsT=va[:, kc, :], rhs=pt_[:, 0:T], start=first, stop=last), [R_va, R_pt_], [R_ac])
                groups = [list(range(a, min(a + GRP, n))) for a in range(0, n, GRP)]
                for i in groups[0]:
                    score(i)
                for gi, grp in enumerate(groups):
                    if gi + 1 < len(groups):
                        for i in groups[gi + 1]:
                            score(i)
                    for i in reversed(grp):
                        pv(i, first=(gi == 0 and i == grp[-1]), last=(gi == len(groups) - 1 and i == grp[0]))
                rc, R_rc = rec
                o_t, R_ot = otl[qi % 2]
                P.op("dve", lambda e, T=T, ac=ac, rc=rc: e.reciprocal(out=rc[:, 0:T], in_=ac[64:128, 0:T]), [R_ac], [R_rc])
                P.op("dve", lambda e, o_t=o_t, T=T, ac=ac, rc=rc: e.tensor_tensor(out=o_t[:, 0:T], in0=ac[0:64, 0:T], in1=rc[:, 0:T], op=ALU.mult),
                     [R_ac, R_rc], [R_ot])
                P.dma("sp", self.od[h * 64:(h + 1) * 64, q0:q0 + T], o_t[:, 0:T], reads=[R_ot], writes=[self.R_od])

        for h in range(int(_os_environ_get("DBG_NHEAD", 16))):
            do_head(h)
        S.close()
        L.close()
        if int(_os_environ_get("DBG_MLA", 3)) < 3:
            return []
        self.phase_outproj(layer, "mla_w_out", j, need_ctx_out)
        return []

    def phase_diff(self, layer):
        nc, P = self.nc, self.P
        j = layer // 3
        need_ctx_out = layer != DEPTH - 1
        lam_init = 0.8 - 0.6 * math.exp(-0.3 * layer)
        L = ExitStack()
        hT = L.enter_context(nc.sbuf_tensor("hT%d" % layer, [128, 8, NTOK], BF16))
        R_hT = [Res() for _ in range(9)]
        blocks = [(0, 256)] + [(256 + 512 * b, 512) for b in range(8)]
        S = Scope(self)
        A_l, RA_l = self.load_bc(S, layer, 0, 1)
        B_l, RB_l = self.load_bc(S, layer, 0, 0, q="act")
        A_c, RA_c = self.load_bc(S, layer, 1, 1)
        B_c, RB_c = self.load_bc(S, layer, 1, 0, q="act")
        T_ = self.prenorm_tiles(S)
        D3 = Defer()
        for gt in range(34):
            if gt < 2:
                hn, R_hn, pt, R_pt = self.prenorm_tile(T_, gt, A_c, RA_c, B_c, RB_c)
                bi = 0
            else:
                hn, R_hn, pt, R_pt = self.prenorm_tile(T_, gt, A_l, RA_l, B_l, RB_l)
                bi = 1 + (gt - 2) // 4
            D3.push(lambda pt=pt, gt=gt, R_pt=R_pt, bi=bi: P.op(
                "dve", lambda e: e.tensor_copy(out=hT[:, :, gt * 128:(gt + 1) * 128], in_=pt[:]), [R_pt], [R_hT[bi]]))
        D3.flush()
        S.close()
        S = Scope(self)
        stage = S.sb([128, 8 * 128], F32, "stg")
        R_stage = Res()
        win = self.W["diff_w_in"][j].rearrange("(k p) n -> p k n", p=128)
        bd = S.sb([128, 128], BF16, "bd")
        R_bd = Res()
        P.op("pool", lambda e: e.memset(bd[:], 0.0), [], [R_bd])
        P.op("dve", lambda e: e.tensor_copy(out=bd[0:64, 0:64], in_=self.ones_b[0:64, 0:64]), [self.R_const, R_bd], [R_bd])
        P.op("dve", lambda e: e.tensor_copy(out=bd[64:128, 64:128], in_=self.ones_b[64:128, 0:64]), [self.R_const, R_bd], [R_bd])
        rm, R_rm = self.load_cast(S, self.c_rm128, [128, 128], stage, R_stage, name="rm128")
        gq = S.sb([128, 1], F32, "gq")
        gk = S.sb([128, 1], F32, "gk")
        gs = S.sb([128, 1], F32, "gs")
        R_gh = Res()
        for half in range(2):
            P.dma("act", gq[half * 64:(half + 1) * 64, :], self.W["diff_qn_g"][j].rearrange("(p o) -> p o", o=1), writes=[R_gh])
            P.dma("act", gk[half * 64:(half + 1) * 64, :], self.W["diff_kn_g"][j].rearrange("(p o) -> p o", o=1), writes=[R_gh])
        P.dma("act", gs[:], self.W["diff_sub_g"][j].rearrange("(p o) -> p o", o=1), writes=[R_gh])
        P.op("dve", lambda e: e.tensor_scalar(out=gs[:], in0=gs[:], scalar1=float(1.0 - lam_init), scalar2=None, op0=ALU.mult),
             [R_gh], [R_gh])
        lv = S.sb([1, 4, 64], F32, "lv")
        lw = S.sb([1, 8], F32, "lw")
        onesf = S.sb([1, 128], F32, "onesf")
        neglam = S.sb([128, 1], F32, "neglam")
        R_lv = Res()
        for n_i, nm in enumerate(("diff_lambda_q1", "diff_lambda_k1", "diff_lambda_q2", "diff_lambda_k2")):
            P.dma("act", lv[0:1, n_i, :], self.W[nm][j:j + 1, :], writes=[R_lv])
        P.op("dve", lambda e: e.memset(onesf[:], 1.0), [], [R_lv])
        P.op("dve", lambda e: e.memset(lw[:], 0.0), [], [R_lv])
        P.op("dve", lambda e: e.tensor_tensor(out=lv[0:1, 0, :], in0=lv[0:1, 0, :], in1=lv[0:1, 1, :], op=ALU.mult), [R_lv], [R_lv])
        P.op("dve", lambda e: e.tensor_tensor(out=lv[0:1, 1, :], in0=lv[0:1, 2, :], in1=lv[0:1, 3, :], op=ALU.mult), [R_lv], [R_lv])
        P.op("dve", lambda e: e.tensor_reduce(out=lw[0:1, 0:2], in_=lv[0:1, 0:2, :], axis=mybir.AxisListType.X, op=ALU.add),
             [R_lv], [R_lv])
        P.op("act", lambda e: e.activation(out=lw[0:1, 2:4], in_=lw[0:1, 0:2], func=AF.Exp), [R_lv], [R_lv])
        P.op("dve", lambda e: e.tensor_tensor(out=lw[0:1, 4:5], in0=lw[0:1, 3:4], in1=lw[0:1, 2:3], op=ALU.subtract), [R_lv], [R_lv])
        P.op("dve", lambda e: e.tensor_scalar(out=lw[0:1, 4:5], in0=lw[0:1, 4:5], scalar1=float(-lam_init), scalar2=None, op0=ALU.add),
             [R_lv], [R_lv])
        Bk = [(S.ps([128, 512], F32, "bk"), Res()) for _ in range(5)]
        sps = [(S.ps([128, 512], F32, "sps"), Res()) for _ in range(3)]
        P.op("pe", lambda e: e.matmul(Bk[4][0][:, 0:2], lhsT=onesf[0:1, :], rhs=lw[0:1, 4:6], start=True, stop=True), [R_lv], [Bk[4][1]])
        P.op("dve", lambda e: e.tensor_copy(out=neglam[:], in_=Bk[4][0][:, 0:1]), [Bk[4][1]], [R_gh])
        Wk = {}
        Wk["sq"] = (S.sb([128, 512], BF16, "sq"), Res())
        Wk["sm"] = Bk[1]
        Wk["r"] = (S.sb([128, 512], F32, "r"), Res())
        Wk["xnf"] = (S.sb([128, 512], F32, "xnf"), Res())
        Wk["xnb"] = (S.sb([128, 512], BF16, "xnb"), Res())
        Wk["rp"] = Bk[2]
        Wk["t1"] = (S.sb([128, 512], F32, "t1"), Res())
        Wk2 = {"sq": (S.sb([128, 512], BF16, "sq2"), Res()), "sm": sps[0], "r": (S.sb([128, 512], F32, "r2"), Res()),
               "xnf": (S.sb([128, 512], F32, "xnf2"), Res()), "xnb": (S.sb([128, 512], BF16, "xnb2"), Res()), "rp": sps[1],
               "t1": (S.sb([128, 512], F32, "t12"), Res())}
        cs = [(S.sb([128, 512], F32, "cos"), S.sb([128, 512], F32, "sin"), Res()) for _ in range(2)]
        QT = (S.sb([128, NTOK], BF16, "QT"), Res())
        KT = (S.sb([128, NTOK], BF16, "KT"), Res())
        Vh = (S.sb([128, 34, 128], BF16, "Vh"), Res())
        NB, GRP = 4, 2
        scb = sps + [Bk[2], Bk[3]]
        pts = [(S.sb([128, 512], BF16, "pt"), Res()) for _ in range(NB)]
        dsum = [(S.sb([128, 512], F32, "dsum"), Res()) for _ in range(2)]
        onesF = S.sb([128, 128], F32, "onesF")
        R_onesF = Res()
        P.op("pool", lambda e: e.memset(onesF[:], 1.0), [], [R_onesF])
        rr = [(S.sb([128, 512], F32, "rr"), Res()) for _ in range(2)]
        uu = [(S.sb([128, 512], F32, "uu"), Res()) for _ in range(2)]
        fin = [(S.sb([128, 512], BF16, "fin"), Res()) for _ in range(2)]
        ncs = [0]
        scale = 64.0 ** -0.5

        wqkv = [(S.sb([128, 8, 128], BF16, "wqkv"), Res()) for _ in range(3)]

        def do_head(h):
            wq, R_wq = self.load_cast(S, win[:, :, h * 128:(h + 1) * 128], [128, 8, 128], stage, R_stage, into=wqkv[0])
            wk, R_wk = self.load_cast(S, win[:, :, D + h * 128:D + (h + 1) * 128], [128, 8, 128], stage, R_stage, into=wqkv[1])
            wv, R_wv = self.load_cast(S, win[:, :, 2 * D + h * 128:2 * D + (h + 1) * 128], [128, 8, 128], stage, R_stage, into=wqkv[2])
            qt, R_qt = QT
            kt, R_kt = KT
            vh, R_vh = Vh
            gp, R_gp = Bk[0]
            for bi, (t0, T) in enumerate(blocks):
                cos_t, sin_t, R_tab = cs[ncs[0] % 2]
                ncs[0] += 1
                P.dma("sp", cos_t[:, 0:T], self.c_cosB[:, t0:t0 + T], writes=[R_tab])
                P.dma("sp", sin_t[:, 0:T], self.c_sinB[:, t0:t0 + T], writes=[R_tab])
                for (wt, R_wt, gcol, dst, R_dst) in ((wk, R_wk, gk, kt, R_kt), (wq, R_wq, gq, qt, R_qt)):
                    for k in range(8):
                        P.op("pe", lambda e, k=k, wt=wt, t0=t0, T=T: e.matmul(gp[:, 0:T], lhsT=wt[:, k, :], rhs=hT[:, k, t0:t0 + T],
                                                                               start=(k == 0), stop=(k == 7)), [R_wt, R_hT[bi]], [R_gp])
                    self.normrope(Wk, gp[:, 0:T], R_gp, 128, T, bd[:, :], 1.0 / 64, gcol[:, 0:1], R_gh,
                                  rm[:, :], R_rm, cos_t[:, 0:T], sin_t[:, 0:T], R_tab, dst[:, t0:t0 + T], R_dst)
                nt = T // 128
                for ti in range(nt):
                    kc = t0 // 128 + ti
                    for k in range(8):
                        P.op("pe", lambda e, k=k, kc=kc, ti=ti: e.matmul(gp[:, ti * 128:(ti + 1) * 128], lhsT=hT[:, k, kc * 128:(kc + 1) * 128],
                                                                          rhs=wv[:, k, :], start=(k == 0), stop=(k == 7)),
                             [R_wv, R_hT[bi]], [R_gp])
                P.op("act", lambda e, t0=t0, nt=nt: e.activation(
                    out=vh[:, t0 // 128:t0 // 128 + nt, :], in_=gp[:, 0:nt * 128].rearrange("p (t c) -> p t c", t=nt),
                    func=AF.Copy), [R_gp], [R_vh])
            for qi, (q0, T, kcs) in enumerate(self.qblocks(need_ctx_out)):
                n = len(kcs)
                seq = [(i, c) for i in range(n) for c in range(2)]

                def score(s_i):
                    i, c = seq[s_i]
                    sp_, R_sp = scb[s_i % NB]
                    kc = kcs[i]
                    P.op("pe", lambda e, sp_=sp_, kc=kc, c=c, T=T, q0=q0: e.matmul(
                        sp_[:, 0:T], lhsT=kt[c * 64:(c + 1) * 64, kc * 128:(kc + 1) * 128], rhs=qt[c * 64:(c + 1) * 64, q0:q0 + T],
                        start=True, stop=True), [R_kt, R_qt], [R_sp])
                    pt_, R_pt_ = pts[s_i % NB]
                    P.op("act", lambda e, sp_=sp_, pt_=pt_, T=T: e.activation(out=pt_[:, 0:T], in_=sp_[:, 0:T], func=AF.Exp, scale=scale),
                         [R_sp], [R_pt_])

                def pv(s_i):
                    i, c = seq[s_i]
                    pt_, R_pt_ = pts[s_i % NB]
                    kc = kcs[i]
                    ao, R_ao = Bk[c]
                    ds_, R_ds = dsum[c]
                    P.op("pe", lambda e, pt_=pt_, kc=kc, i=i, T=T, n=n, ao=ao: e.matmul(
                        ao[:, 0:T], lhsT=vh[:, kc, :], rhs=pt_[:, 0:T], start=(i == 0), stop=(i == n - 1)), [R_vh, R_pt_], [R_ao])
                    eng = "dve" if c == 0 else "pool"
                    if i == 0:
                        P.op(eng, lambda e, pt_=pt_, ds_=ds_, T=T: e.tensor_copy(out=ds_[:, 0:T], in_=pt_[:, 0:T]), [R_pt_], [R_ds])
                    else:
                        P.op(eng, lambda e, pt_=pt_, ds_=ds_, T=T: e.tensor_tensor(out=ds_[:, 0:T], in0=ds_[:, 0:T], in1=pt_[:, 0:T], op=ALU.add),
                             [R_pt_, R_ds], [R_ds])
                ns = len(seq)
                groups = [list(range(a_, min(a_ + GRP, ns))) for a_ in range(0, ns, GRP)]
                for s_i in groups[0]:
                    score(s_i)
                for gi, grp in enumerate(groups):
                    if gi + 1 < len(groups):
                        for s_i in groups[gi + 1]:
                            score(s_i)
                    for s_i in reversed(grp):
                        pv(s_i)
                sm_, R_sm = Bk[4]
                for c in range(2):
                    r_, R_r = rr[c]
                    u_, R_u = uu[c]
                    ao, R_ao = Bk[c]
                    ds_, R_ds = dsum[c]
                    P.op("pe", lambda e, ds_=ds_, T=T: e.matmul(sm_[:, 0:T], lhsT=onesF[:, :], rhs=ds_[:, 0:T], start=True, stop=True),
                         [R_ds, R_onesF], [R_sm])
                    P.op("dve", lambda e, r_=r_, T=T: e.reciprocal(out=r_[:, 0:T], in_=sm_[:, 0:T]), [R_sm], [R_r])
                    P.op("dve", lambda e, u_=u_, ao=ao, r_=r_, T=T: e.tensor_tensor(out=u_[:, 0:T], in0=ao[:, 0:T], in1=r_[:, 0:T], op=ALU.mult),
                         [R_ao, R_r], [R_u])
                u0, R_u0 = uu[0]
                u1, R_u1 = uu[1]
                P.op("dve", lambda e, u0=u0, u1=u1, T=T: e.scalar_tensor_tensor(out=u0[:, 0:T], in0=u1[:, 0:T], scalar=neglam[:, 0:1],
                                                                              in1=u0[:, 0:T], op0=ALU.mult, op1=ALU.add),
                     [R_u0, R_u1, R_gh], [R_u0])
                sq_, R_sq = Wk["sq"]
                sm_, R_sm = Bk[4]
                r_, R_r = rr[0]
                f_, R_f = fin[qi % 2]
                P.op("act", lambda e, u0=u0, sq_=sq_, T=T: e.activation(out=sq_[:, 0:T], in_=u0[:, 0:T], func=AF.Square), [R_u0], [R_sq])
                P.op("pe", lambda e, sq_=sq_, sm_=sm_, T=T: e.matmul(sm_[:, 0:T], lhsT=self.ones_b[:, :], rhs=sq_[:, 0:T], start=True, stop=True),
                     [R_sq, self.R_const], [R_sm])
                P.op("act", lambda e, r_=r_, sm_=sm_, T=T: e.activation(out=r_[:, 0:T], in_=sm_[:, 0:T], func=AF.Sqrt,
                                                                        bias=self.eps_t[:, 0:1], scale=1.0 / 128), [R_sm, self.R_const], [R_r])
                P.op("dve", lambda e, r_=r_, T=T: e.reciprocal(out=r_[:, 0:T], in_=r_[:, 0:T]), [R_r], [R_r])
                P.op("dve", lambda e, u0=u0, r_=r_, f_=f_, T=T: e.scalar_tensor_tensor(out=f_[:, 0:T], in0=u0[:, 0:T], scalar=gs[:, 0:1],
                                                                                    in1=r_[:, 0:T], op0=ALU.mult, op1=ALU.mult),
                     [R_u0, R_r, R_gh], [R_f])
                P.dma("sp", self.od[h * 128:(h + 1) * 128, q0:q0 + T], f_[:, 0:T], reads=[R_f], writes=[self.R_od])

        for h in range(int(_os_environ_get("DBG_NHEAD", 8))):
            do_head(h)
        S.close()
        L.close()
        self.phase_outproj(layer, "diff_w_out", j, need_ctx_out)
        return []

    def gelu_tile(self, G, z, R_z, out_ap, R_out, shape):
        P = self.P
        i = G["i"]
        G["i"] += 1
        s2, R_s2 = G["s2"][i % 2]
        w, R_w = G["w"][i % 2]
        sl = tuple(slice(0, d_) for d_ in shape)
        P.op("act", lambda e: e.activation(out=s2[sl], in_=z, func=AF.Square), [R_z], [R_s2])
        P.op("pool", lambda e: e.tensor_scalar(out=w[sl], in0=s2[sl], scalar1=0.044715, scalar2=1.0, op0=ALU.mult, op1=ALU.add),
             [R_s2], [R_w])
        P.op("dve", lambda e: e.tensor_tensor(out=w[sl], in0=w[sl], in1=z, op=ALU.mult), [R_w, R_z], [R_w])
        P.op("act", lambda e: e.activation(out=s2[sl], in_=w[sl], func=AF.Sigmoid, scale=1.5957691216057308), [R_w, R_s2], [R_s2])
        P.op("dve", lambda e: e.tensor_tensor(out=out_ap, in0=s2[sl], in1=z, op=ALU.mult), [R_s2, R_z], [R_out])

    def phase_chunk(self, layer):
        nc, P = self.nc, self.P
        j = layer // 3
        need_ctx_out = layer != DEPTH - 1
        gts_all = list(range(0 if need_ctx_out else 2, 34))
        blocks = ([(0, 256)] if need_ctx_out else []) + [(256 + 512 * b, 512) for b in range(8)]
        S = Scope(self)
        A_l, RA_l = self.load_bc(S, layer, 0, 1)
        B_l, RB_l = self.load_bc(S, layer, 0, 0, q="act")
        if need_ctx_out:
            A_c, RA_c = self.load_bc(S, layer, 1, 1)
            B_c, RB_c = self.load_bc(S, layer, 1, 0, q="act")
        T_ = self.prenorm_tiles(S, npt=2)
        stage = S.sb([128, 8 * 512], F32, "stg")
        R_stage = Res()
        win = S.sb([128, 8, 4096], BF16, "win")
        R_win = Res()
        wv_ = self.W["sg_w_in"][j].rearrange("(k p) n -> p k n", p=128)
        for c0 in range(0, 4096, 512):
            self.load_cast(S, wv_[:, :, c0:c0 + 512], [128, 8, 512], stage, R_stage, eng=("act" if (c0 // 512) % 2 else "dve"),
                           into=(win[:, :, c0:c0 + 512], R_win))
        wsn = stage[:, 0:1024].rearrange("p (g q) -> p g q", g=8)
        P.dma("sp", wsn, self.W["sg_w_s"][j].rearrange("g p q -> p g q"), writes=[R_stage])
        wsT = S.sb([128, 8, 128], BF16, "wsT")
        R_ws = Res()
        pt0, R_pt0 = T_["pt"][0]
        for g in range(8):
            P.op("pe", lambda e, g=g: e.transpose(out=pt0[:, g, :], in_=wsn[:, g, :], identity=self.ident_f[:]),
                 [R_stage, self.R_const], [R_pt0])
        P.op("dve", lambda e: e.tensor_copy(out=wsT[:], in_=pt0[:]), [R_pt0], [R_ws])
        bsb = S.sb([128, 8, 2, 128], F32, "bsb")
        R_bsb = Res()
        bsrc = self.W["sg_b_s"][j:j + 1].rearrange("o g p -> o (g p)").partition_broadcast(128).rearrange("q o (g p) -> q (o g) p", g=8)
        for r_ in range(2):
            P.dma("act", bsb[:, :, r_, :], bsrc, writes=[R_bsb])
        bsb16 = bsb[:, :, :, :].rearrange("q g r p -> q (g r) p")
        lng = S.sb([128, 2048], F32, "lng")
        lnb = S.sb([128, 2048], F32, "lnb")
        R_ln = Res()
        P.dma("act", lng[:], self.W["sg_ln_g"][j:j + 1, :].partition_broadcast(128), writes=[R_ln])
        P.dma("act", lnb[:], self.W["sg_ln_b"][j:j + 1, :].partition_broadcast(128), writes=[R_ln])
        hTb = (S.sb([128, 8, 512], BF16, "hTb"), Res())
        uT = (S.sb([128, 16, 512], BF16, "uT"), Res())
        vz = (S.sb([128, 2048], F32, "vz"), Res())
        vln = (S.sb([128, 2048], BF16, "vln"), Res())
        G = {"i": 0, "s2": [(S.sb([128, 512], F32, "gs2"), Res()) for _ in range(2)],
             "w": [(S.sb([128, 512], F32, "gw"), Res()) for _ in range(2)]}
        zps = [(S.ps([128, 512], F32, "zp"), Res()) for _ in range(2)]
        spp = [(S.ps([128, 4, 128], F32, "spp"), Res()) for _ in range(2)]
        tt = (S.sb([128, 4, 128], F32, "tt"), Res())
        prodT = [(S.sb([128, 16, 128], BF16, "prodT"), Res()) for _ in range(2)]
        st2 = (S.sb([128, 8], F32, "st2"), Res())
        nz = [0]
        pdv = self.pd.rearrange("(k p) t -> p k t", p=128)
        D4 = Defer()
        for bi, (t0, T) in enumerate(blocks):
            hb_, R_hb = hTb
            nt = T // 128
            for ti in range(nt):
                gt = t0 // 128 + ti
                if gt < 2:
                    hn, R_hn, pt, R_pt = self.prenorm_tile(T_, gt, A_c, RA_c, B_c, RB_c)
                else:
                    hn, R_hn, pt, R_pt = self.prenorm_tile(T_, gt, A_l, RA_l, B_l, RB_l)
                D4.push(lambda pt=pt, ti=ti, R_pt=R_pt: P.op(
                    "dve", lambda e: e.tensor_copy(out=hb_[:, :, ti * 128:(ti + 1) * 128], in_=pt[:]), [R_pt], [R_hb]))
            D4.flush()
            ut, R_ut = uT
            for cc in range(16):
                zp, R_zp = zps[nz[0] % 2]
                nz[0] += 1
                for k in range(8):
                    P.op("pe", lambda e, zp=zp, k=k, cc=cc, T=T: e.matmul(zp[:, 0:T], lhsT=win[:, k, cc * 128:(cc + 1) * 128],
                                                                           rhs=hb_[:, k, 0:T], start=(k == 0), stop=(k == 7)),
                         [R_win, R_hb], [R_zp])
                self.gelu_tile(G, zp[:, 0:T], R_zp, ut[:, cc, 0:T], R_ut, (128, T))
            for ti in range(nt):
                gt = t0 // 128 + ti
                vzt, R_vz = vz
                for nb in range(4):
                    zp, R_zp = zps[nz[0] % 2]
                    nz[0] += 1
                    for k in range(8):
                        P.op("pe", lambda e, zp=zp, k=k, nb=nb, ti=ti: e.matmul(
                            zp[:, :], lhsT=hb_[:, k, ti * 128:(ti + 1) * 128], rhs=win[:, k, 2048 + nb * 512:2048 + (nb + 1) * 512],
                            start=(k == 0), stop=(k == 7)), [R_win, R_hb], [R_zp])
                    self.gelu_tile(G, zp[:, :], R_zp, vzt[:, nb * 512:(nb + 1) * 512], R_vz, (128, 512))
                s_, R_s = st2
                junk, R_junk = T_["junk"]
                P.op("dve", lambda e: e.tensor_reduce(out=s_[:, 0:1], in_=vzt[:, :], axis=mybir.AxisListType.X, op=ALU.add), [R_vz], [R_s])
                for hf in range(2):
                    P.op("act", lambda e, hf=hf: e.activation(out=junk[:, :], in_=vzt[:, hf * 1024:(hf + 1) * 1024], func=AF.Square,
                                                                accum_out=s_[:, 1 + hf:2 + hf]), [R_vz], [R_junk, R_s])
                P.op("dve", lambda e: e.tensor_scalar(out=s_[:, 0:1], in0=s_[:, 0:1], scalar1=1.0 / 2048, scalar2=None, op0=ALU.mult), [R_s], [R_s])
                P.op("dve", lambda e: e.tensor_tensor(out=s_[:, 1:2], in0=s_[:, 1:2], in1=s_[:, 2:3], op=ALU.add), [R_s], [R_s])
                P.op("dve", lambda e: e.tensor_tensor(out=s_[:, 3:4], in0=s_[:, 0:1], in1=s_[:, 0:1], op=ALU.mult), [R_s], [R_s])
                P.op("dve", lambda e: e.scalar_tensor_tensor(out=s_[:, 4:5], in0=s_[:, 1:2], scalar=1.0 / 2048, in1=s_[:, 3:4],
                                                             op0=ALU.mult, op1=ALU.subtract), [R_s], [R_s])
                P.op("act", lambda e: e.activation(out=s_[:, 5:6], in_=s_[:, 4:5], func=AF.Sqrt, bias=self.eps_t[:, 0:1], scale=1.0),
                     [R_s, self.R_const], [R_s])
                P.op("dve", lambda e: e.reciprocal(out=s_[:, 5:6], in_=s_[:, 5:6]), [R_s], [R_s])
                P.op("dve", lambda e: e.scalar_tensor_tensor(out=s_[:, 6:7], in0=s_[:, 0:1], scalar=-1.0, in1=s_[:, 5:6],
                                                             op0=ALU.mult, op1=ALU.mult), [R_s], [R_s])
                P.op("act", lambda e: e.activation(out=vzt[:, :], in_=vzt[:, :], func=AF.Identity, bias=s_[:, 6:7], scale=s_[:, 5:6]),
                     [R_vz, R_s], [R_vz])
                P.op("dve", lambda e: e.tensor_tensor(out=vzt[:, :], in0=vzt[:, :], in1=lng[:, :], op=ALU.mult), [R_vz, R_ln], [R_vz])
                vl, R_vl = vln
                P.op("pool", lambda e: e.tensor_tensor(out=vl[:, :], in0=vzt[:, :], in1=lnb[:, :], op=ALU.add), [R_vz, R_ln], [R_vl])
                pr, R_pr = prodT[gt % 2]
                for m in range(4):
                    sp_, R_sp = spp[m % 2]
                    for q_ in range(4):
                        cc = 4 * m + q_
                        P.op("pe", lambda e, sp_=sp_, q_=q_, cc=cc: e.matmul(sp_[:, q_, :], lhsT=vl[:, cc * 128:(cc + 1) * 128],
                                                                             rhs=wsT[:, cc // 2, :], start=True, stop=True),
                             [R_vl, R_ws], [R_sp])
                    t_, R_t = tt
                    P.op("dve", lambda e, sp_=sp_, m=m: e.tensor_tensor(out=t_[:, :, :], in0=sp_[:, :, :], in1=bsb16[:, 4 * m:4 * m + 4, :], op=ALU.add),
                         [R_sp, R_bsb], [R_t])
                    P.op("pool", lambda e, m=m, ti=ti, pr=pr: e.tensor_tensor(out=pr[:, 4 * m:4 * m + 4, :], in0=t_[:, :, :],
                                                                                in1=ut[:, 4 * m:4 * m + 4, ti * 128:(ti + 1) * 128], op=ALU.mult),
                         [R_t, R_ut], [R_pr])
                P.dma("sp", pdv[:, :, gt * 128:(gt + 1) * 128], pr[:, :, :], reads=[R_pr], writes=[self.R_od])
        S.close()
        self.phase_outproj(layer, "sg_w_out", j, need_ctx_out, src=self.pd, nk=16)
        return []

    def phase_mix(self, layer):
        kind = layer % 3
        if kind == 0:
            return self.phase_mla(layer)
        if kind == 1:
            return self.phase_diff(layer)
        return self.phase_chunk(layer)


_CACHE = {}


def _get_nc(plan):
    key = tuple(plan)
    if key not in _CACHE:
        K = Kern(plan)
        _CACHE[key] = K.build()
    return _CACHE[key]


FULL_PLAN = [(k, l) for l in range(DEPTH) for k in ("mix", "moe")]


def _axial(rot_dim):
    n_rows = NLAT // 64
    rows = np.repeat(np.arange(n_rows, dtype=np.float32), 64)
    cols = np.tile(np.arange(64, dtype=np.float32), n_rows)
    n_freq = rot_dim // 4
    inv_freq = (np.float32(10000.0) ** (-np.arange(n_freq, dtype=np.float32) / np.float32(n_freq))).astype(np.float32)
    ang = np.concatenate([rows[:, None] * inv_freq, cols[:, None] * inv_freq], axis=-1).astype(np.float32)
    return np.cos(ang).astype(np.float32), np.sin(ang).astype(np.float32)


def _host_consts():
    c = {}
    cosa, sina = _axial(32)
    cosA = np.ones((96, NTOK), np.float32)
    sinA = np.zeros((96, NTOK), np.float32)
    cosA[64:80, NCTX:] = cosa.T
    cosA[80:96, NCTX:] = cosa.T
    sinA[64:80, NCTX:] = sina.T
    sinA[80:96, NCTX:] = sina.T
    c["c_cosA"], c["c_sinA"] = cosA, sinA
    rm = np.zeros((96, 96), np.float32)
    for m in range(64, 80):
        rm[m + 16, m] = -1.0
    for m in range(80, 96):
        rm[m - 16, m] = 1.0
    c["c_rm96"] = rm
    cosb, sinb = _axial(64)
    cosB = np.ones((128, NTOK), np.float32)
    sinB = np.zeros((128, NTOK), np.float32)
    rm2 = np.zeros((128, 128), np.float32)
    for blk in range(4):
        cosB[blk * 32:(blk + 1) * 32, NCTX:] = cosb.T
        sinB[blk * 32:(blk + 1) * 32, NCTX:] = sinb.T
    for c0 in (0, 64):
        for m in range(32):
            rm2[c0 + m + 32, c0 + m] = -1.0
            rm2[c0 + m, c0 + m + 32] = 1.0
    c["c_cosB"], c["c_sinB"], c["c_rm128"] = cosB, sinB, rm2
    return c


def make_in_maps(inputs, ncores=8):
    ident = np.eye(128, dtype=np.float32)
    shared = {n: np.ascontiguousarray(np.asarray(inputs[n], dtype=np.float32)) for n, _ in WEIGHT_SPECS}
    shared["c_ident"] = ident
    shared.update(_host_consts())
    maps = []
    c_ctx = np.asarray(inputs["c_ctx"], dtype=np.float32)
    for b in range(ncores):
        c = np.asarray(inputs["c"][b], dtype=np.float32)
        cc = np.stack([c, c_ctx], axis=-1).reshape(8, 128, 2).transpose(1, 0, 2)
        m = dict(shared)
        m["xin"] = np.ascontiguousarray(np.asarray(inputs["x"][b], dtype=np.float32))
        m["cin"] = np.ascontiguousarray(np.asarray(inputs["ctx"][b], dtype=np.float32))
        m["cc"] = np.ascontiguousarray(cc)
        maps.append(m)
    return maps


def kernel(**inputs):
    nc = _get_nc(FULL_PLAN)
    maps = make_in_maps(inputs, 8)
    res = run_bass_kernel_spmd(nc, maps, core_ids=list(range(8)))
    return np.stack([np.asarray(r["xl"], dtype=np.float32) for r in res.results], axis=0)
```

```python
import math
import os


def _os_environ_get(k, d):
    return os.environ.get(k, d)

from contextlib import ExitStack

import numpy as np
import concourse.bass as bass
import concourse.mybir as mybir
from concourse.bass_utils import run_bass_kernel_spmd

F32 = mybir.dt.float32
BF16 = mybir.dt.bfloat16
I32 = mybir.dt.int32
U32 = mybir.dt.uint32
ALU = mybir.AluOpType
AF = mybir.ActivationFunctionType

ENGS = ("pe", "act", "dve", "pool", "sp")
DMA_Q = ("sp", "act", "pool")

D = 1024
NCTX = 256
NLAT = 4096
NTOK = NCTX + NLAT
DEPTH = 4
EPS = 1e-6
NEXP = 16
CAP_L = 512
CAP_C = 32


class Res:
    __slots__ = ("lw", "rd")

    def __init__(self):
        self.lw = None
        self.rd = []


class Op:
    __slots__ = ("eng", "fn", "deps", "signal", "sigval", "is_dma", "dsem", "dval", "phase")

    def __init__(self, eng, fn, is_dma, phase):
        self.eng = eng
        self.fn = fn
        self.deps = []
        self.signal = False
        self.sigval = None
        self.is_dma = is_dma
        self.dsem = None
        self.dval = None
        self.phase = phase


class Prog:
    def __init__(self, nc, n_dma_sems=12):
        self.nc = nc
        self.ops = {e: [] for e in ENGS}
        self.nphase = 0
        self.n_dma_sems = n_dma_sems
        self.max_ops = 3000
        self.inline_wait = os.environ.get("NO_INLINE_WAIT") is None
        self.sems = {}
        self.dma_sems = {}

    def open(self, stack):
        nc = self.nc
        for e in ENGS:
            self.sems[e] = stack.enter_context(nc.semaphore("s_" + e))
        for q in DMA_Q:
            self.dma_sems[q] = [stack.enter_context(nc.semaphore("d_%s%d" % (q, i)))
                                for i in range(self.n_dma_sems)]
        self.sem_cnt = {e: 0 for e in ENGS}
        self.dma_cnt = {q: [0] * self.n_dma_sems for q in DMA_Q}
        self.dma_rr = {q: 0 for q in DMA_Q}
        self.waited = {}

    def op(self, eng, fn, reads=(), writes=(), dma=False):
        if max(len(v) for v in self.ops.values()) >= self.max_ops:
            self.flush()
        o = Op(eng, fn, dma, self.nphase)
        deps = []
        for r in reads:
            if r.lw is not None:
                deps.append(r.lw)
        for w in writes:
            if w.lw is not None:
                deps.append(w.lw)
            deps.extend(w.rd)
        seen = set()
        for d in deps:
            if id(d) in seen or d is o:
                continue
            seen.add(id(d))
            if (not d.is_dma) and d.phase != self.nphase:
                continue
            if (not d.is_dma) and (not dma) and d.eng == eng and eng == "pe":
                continue
            o.deps.append(d)
            d.signal = True
        for r in reads:
            r.rd.append(o)
        for w in writes:
            w.lw = o
            w.rd = []
        self.ops[eng].append(o)
        return o

    def dma(self, q, out, in_, reads=(), writes=(), **kw):
        return self.op(q, lambda e: e.dma_start(out=out, in_=in_, **kw), reads, writes, dma=True)

    def flush(self, final_wait=()):
        nc = self.nc
        ops = self.ops
        for e in ENGS:
            for o in ops[e]:
                if o.is_dma:
                    k = self.dma_rr[e]
                    self.dma_rr[e] = (k + 1) % self.n_dma_sems
                    self.dma_cnt[e][k] += 16
                    o.dsem = (e, k)
                    o.dval = self.dma_cnt[e][k]
                elif o.signal:
                    self.sem_cnt[e] += 1
                    o.sigval = self.sem_cnt[e]

        def emit_engine(e, engobj):
            waited = self.waited
            for o in ops[e]:
                need = {}
                for d in o.deps:
                    if d.is_dma:
                        key = ("d",) + d.dsem
                        val = d.dval
                    else:
                        key = ("c", d.eng)
                        val = d.sigval
                    if val > need.get(key, 0):
                        need[key] = val
                if o.is_dma:
                    key = ("d",) + o.dsem
                    if o.dval - 16 > need.get(key, 0):
                        need[key] = o.dval - 16
                todo = []
                for key, val in need.items():
                    wk = (e, key)
                    if waited.get(wk, 0) >= val:
                        continue
                    waited[wk] = val
                    sem = self.dma_sems[key[1]][key[2]] if key[0] == "d" else self.sems[key[1]]
                    todo.append((sem, val))
                inline = None
                if todo and self.inline_wait and not o.is_dma:
                    inline = todo.pop()
                for sem, val in todo:
                    engobj.wait_ge(sem, val)
                ins = o.fn(engobj)
                if inline is not None:
                    ins._wait_ge(inline[0], inline[1])
                if o.is_dma:
                    ins.then_inc(self.dma_sems[o.dsem[0]][o.dsem[1]], 16)
                elif o.signal:
                    ins.then_inc(self.sems[e], 1)
            if e == "sp":
                for o in final_wait:
                    engobj.wait_ge(self.dma_sems[o.dsem[0]][o.dsem[1]], o.dval)

        with nc.Block() as block:
            if ops["sp"] or final_wait:
                block.sync(lambda eng: emit_engine("sp", eng))
            if ops["act"]:
                block.scalar(lambda eng: emit_engine("act", eng))
            if ops["dve"]:
                block.vector(lambda eng: emit_engine("dve", eng))
            if ops["pool"]:
                block.gpsimd(lambda eng: emit_engine("pool", eng))
            if ops["pe"]:
                block.tensor(lambda eng: emit_engine("pe", eng))
        used = [e for e in ENGS if self.sem_cnt[e] > 0]
        if used and not final_wait:
            with nc.Block() as block:
                reg = {"sp": block.sync, "act": block.scalar, "dve": block.vector, "pool": block.gpsimd, "pe": block.tensor}
                for e in used:
                    reg[e](lambda eng, e=e: eng.sem_clear(self.sems[e]))
            for e in used:
                self.sem_cnt[e] = 0
            for wk in [wk for wk in self.waited if wk[1][0] == "c"]:
                del self.waited[wk]
        self.ops = {e: [] for e in ENGS}
        self.nphase += 1


class Defer:
    def __init__(self):
        self.p = None

    def push(self, fn):
        if self.p is not None:
            self.p()
        self.p = fn

    def flush(self):
        if self.p is not None:
            self.p()
        self.p = None


class Scope:
    def __init__(self, K):
        self.K = K
        self.st = ExitStack()
        self.n = 0

    def sb(self, shape, dt, name=None):
        self.n += 1
        nm = "%s_p%d_%d" % (name or "t", self.K.P.nphase, self.n)
        return self.st.enter_context(self.K.nc.sbuf_tensor(nm, list(shape), dt))

    def ps(self, shape, dt, name=None):
        self.n += 1
        nm = "%s_q%d_%d" % (name or "p", self.K.P.nphase, self.n)
        return self.st.enter_context(self.K.nc.psum_tensor(nm, list(shape), dt))

    def close(self, final_wait=()):
        self.K.P.flush(final_wait=final_wait)
        self.st.close()


WEIGHT_SPECS = [
    ("ada_w", [DEPTH, D, 6 * D]), ("ada_b", [DEPTH, 6 * D]),
    ("norm_mix_g", [DEPTH, D]), ("norm_ffn_g", [DEPTH, D]),
    ("mla_w_in", [2, D, 800]), ("mla_q_norm_g", [2, 512]), ("mla_w_uq", [2, 512, 1536]),
    ("mla_kv_norm_g", [2, 256]), ("mla_w_ukv", [2, 256, 2048]), ("mla_qn_g", [2, 96]),
    ("mla_kn_g", [2, 96]), ("mla_w_out", [2, D, D]),
    ("diff_w_in", [1, D, 3 * D]), ("diff_qn_g", [1, 64]), ("diff_kn_g", [1, 64]),
    ("diff_lambda_q1", [1, 64]), ("diff_lambda_k1", [1, 64]), ("diff_lambda_q2", [1, 64]),
    ("diff_lambda_k2", [1, 64]), ("diff_sub_g", [1, 128]), ("diff_w_out", [1, D, D]),
    ("sg_w_in", [1, D, 4096]), ("sg_ln_g", [1, 2048]), ("sg_ln_b", [1, 2048]),
    ("sg_w_s", [1, 8, 128, 128]), ("sg_b_s", [1, 8, 128]), ("sg_w_out", [1, 2048, D]),
    ("moe_router", [DEPTH, D, NEXP]), ("moe_w_gate", [DEPTH, NEXP, D, D]),
    ("moe_w_up", [DEPTH, NEXP, D, D]), ("moe_w_down", [DEPTH, NEXP, D, D]),
]


class Kern:
    def __init__(self, plan, debug=False, debug_layer=0):
        self.plan = plan
        self.debug = debug
        self.debug_layer = debug_layer
        self.nc = bass.Bass("TRN2", target_bir_lowering=False)
        self.P = Prog(self.nc)

    def xrows(self, gt, n=1):
        if gt < 2:
            return self.xc[gt * 128:(gt + n) * 128, :]
        return self.xl[(gt - 2) * 128:(gt - 2 + n) * 128, :]

    def hrows(self, gt, n=1):
        if gt < 2:
            return self.hc[gt * 128:(gt + n) * 128, :]
        return self.hl[(gt - 2) * 128:(gt - 2 + n) * 128, :]

    def xres(self, gt):
        return self.R_x[gt]

    def load_bc(self, S, layer, stream, slot, q="sp"):
        t = S.sb([128, D], F32, "bc")
        r = Res()
        src = self.modd[layer, stream, slot:slot + 1, :].partition_broadcast(128)
        self.P.dma(q, t[:], src, reads=[self.R_modd], writes=[r])
        return t, r

    def build(self):
        nc, P = self.nc, self.P
        dt_in = lambda name, shape, dt=F32: nc.dram_tensor(name, list(shape), dt, kind="ExternalInput").ap()
        self.xin = dt_in("xin", [NLAT, D])
        self.cin = dt_in("cin", [NCTX, D])
        self.cc = dt_in("cc", [128, 8, 2])
        self.W = {n: dt_in(n, s) for n, s in WEIGHT_SPECS}
        self.c_ident = dt_in("c_ident", [128, 128])
        self.c_rm96 = dt_in("c_rm96", [96, 96])
        self.c_rm128 = dt_in("c_rm128", [128, 128])
        self.c_cosB = dt_in("c_cosB", [128, NTOK])
        self.c_sinB = dt_in("c_sinB", [128, NTOK])
        self.c_cosA = dt_in("c_cosA", [96, NTOK])
        self.c_sinA = dt_in("c_sinA", [96, NTOK])
        self.xl = nc.dram_tensor("xl", [NLAT, D], F32, kind="ExternalOutput").ap()
        if self.debug:
            self.xc = nc.dram_tensor("xc", [NCTX, D], F32, kind="ExternalOutput").ap()
        else:
            self.xc = nc.dram_tensor("xc", [NCTX, D], F32).ap()
        self.hl = nc.dram_tensor("hl", [NLAT, D], BF16).ap()
        self.hc = nc.dram_tensor("hc", [NCTX, D], BF16).ap()
        self.modd = nc.dram_tensor("modd", [DEPTH, 2, 6, D], F32).ap()
        if self.debug:
            self.od = nc.dram_tensor("od", [D, NTOK], BF16, kind="ExternalOutput").ap()
            self.dbg_qt = nc.dram_tensor("dbg_qt", [128, NTOK], BF16, kind="ExternalOutput").ap()
            self.dbg_kt = nc.dram_tensor("dbg_kt", [128, NTOK], BF16, kind="ExternalOutput").ap()
            self.dbg_va = nc.dram_tensor("dbg_va", [128, 34 * 128], BF16, kind="ExternalOutput").ap()
        else:
            self.od = nc.dram_tensor("od", [D, NTOK], BF16).ap()
        self.R_x = [Res() for _ in range(34)]
        self.R_h = [Res() for _ in range(34)]
        self.R_modd = Res()
        self.R_od = Res()
        self.pd = nc.dram_tensor("pd", [2048, NTOK], BF16).ap()
        self.R_xl_all = Res()
        self.R_xc_all = Res()
        self.fence_d = nc.dram_tensor("fence_d", [128, 4], F32).ap()
        if self.debug:
            self.dbg_xe = nc.dram_tensor("dbg_xe", [128, 8 * 544], BF16, kind="ExternalOutput").ap()
            self.dbg_hid = nc.dram_tensor("dbg_hid", [128, 8 * 544], BF16, kind="ExternalOutput").ap()
            self.dbg_sg = nc.dram_tensor("dbg_sg", [128, 8 * 544], F32, kind="ExternalOutput").ap()
            self.dbg_xe2 = nc.dram_tensor("dbg_xe2", [128, 8 * 544], BF16, kind="ExternalOutput").ap()
            self.dbg_wg = nc.dram_tensor("dbg_wg", [128, 8 * 1024], BF16, kind="ExternalOutput").ap()
            self.dbg_aff = nc.dram_tensor("dbg_aff", [NEXP, NLAT], F32, kind="ExternalOutput").ap()
            self.dbg_idx = nc.dram_tensor("dbg_idx", [128, 4 * NEXP], I32, kind="ExternalOutput").ap()
            self.dbg_gate = nc.dram_tensor("dbg_gate", [128, 4 * NEXP], F32, kind="ExternalOutput").ap()

        with ExitStack() as top:
            P.open(top)
            self.top = top
            self.ident_f = top.enter_context(nc.sbuf_tensor("ident_f", [128, 128], F32))
            self.ident_b = top.enter_context(nc.sbuf_tensor("ident_b", [128, 128], BF16))
            self.ones_b = top.enter_context(nc.sbuf_tensor("ones_b", [128, 128], BF16))
            self.eps_t = top.enter_context(nc.sbuf_tensor("eps_t", [128, 4], F32))
            self.R_const = Res()
            P.op("dve", lambda e: e.memset(self.eps_t[:], EPS), [], [self.R_const])
            P.dma("sp", self.ident_f[:], self.c_ident, writes=[self.R_const])
            P.op("dve", lambda e: e.tensor_copy(out=self.ident_b[:], in_=self.ident_f[:]), [self.R_const], [self.R_const])
            P.op("dve", lambda e: e.memset(self.ones_b[:], 1.0), [], [self.R_const])
            for i in range(8):
                P.dma("sp", self.xl[i * 512:(i + 1) * 512, :], self.xin[i * 512:(i + 1) * 512, :],
                      writes=[self.R_x[2 + 4 * i + j] for j in range(4)])
            P.dma("sp", self.xc[:, :], self.cin[:, :], writes=[self.R_x[0], self.R_x[1]])
            self.phase_adaln()
            last_ops = []
            for step in self.plan:
                kind, layer = step
                if kind == "moe":
                    last_ops = self.phase_moe(layer)
                elif kind == "mix":
                    last_ops = self.phase_mix(layer)
            fin = []
            for r in self.R_x[2:]:
                if r.lw is not None and r.lw.is_dma:
                    fin.append(r.lw)
            fin = list({id(o): o for o in fin + [o for o in last_ops if o.is_dma]}.values())
            P.flush(final_wait=fin)
        return nc

    def phase_adaln(self):
        nc, P = self.nc, self.P
        S = Scope(self)
        ccs = S.sb([128, 8, 2], F32, "ccs")
        R_cc = Res()
        P.dma("sp", ccs[:], self.cc, writes=[R_cc])
        P.op("act", lambda e: e.activation(out=ccs[:], in_=ccs[:], func=AF.Silu), [R_cc], [R_cc])
        wst = [S.sb([128, 8, 512], F32, "adaw") for _ in range(2)]
        R_w = [Res(), Res()]
        pss = [S.ps([2, 512], F32, "adaps") for _ in range(2)]
        R_ps = [Res(), Res()]
        modv = S.sb([2, 6 * D], F32, "modv")
        R_mv = Res()
        bias = S.sb([2, 6 * D], F32, "adab")
        gm = S.sb([2, D], F32, "gm")
        gf = S.sb([2, D], F32, "gf")
        R_b = Res()
        n = 0
        for layer in range(DEPTH):
            P.dma("act", bias[:], self.W["ada_b"][layer:layer + 1, :].partition_broadcast(2), writes=[R_b])
            P.dma("act", gm[:], self.W["norm_mix_g"][layer:layer + 1, :].partition_broadcast(2), writes=[R_b])
            P.dma("act", gf[:], self.W["norm_ffn_g"][layer:layer + 1, :].partition_broadcast(2), writes=[R_b])
            for cb in range(12):
                b = n % 2
                n += 1
                src = self.W["ada_w"][layer, :, cb * 512:(cb + 1) * 512].rearrange("(k p) n -> p k n", p=128)
                P.dma("sp", wst[b][:], src, writes=[R_w[b]])
                for k in range(8):
                    P.op("pe", lambda e, b=b, k=k: e.matmul(pss[b][:, :], lhsT=ccs[:, k, :], rhs=wst[b][:, k, :],
                                                            start=(k == 0), stop=(k == 7)),
                         [R_cc, R_w[b]], [R_ps[b]])
                P.op("dve", lambda e, b=b, cb=cb: e.tensor_tensor(out=modv[:, cb * 512:(cb + 1) * 512], in0=pss[b][:, :],
                                                                   in1=bias[:, cb * 512:(cb + 1) * 512], op=ALU.add),
                     [R_ps[b], R_b], [R_mv])
            P.op("dve", lambda e: e.scalar_tensor_tensor(out=modv[:, D:2 * D], in0=modv[:, D:2 * D], scalar=1.0,
                                                         in1=gm[:], op0=ALU.add, op1=ALU.mult), [R_mv, R_b], [R_mv])
            P.op("dve", lambda e: e.scalar_tensor_tensor(out=modv[:, 4 * D:5 * D], in0=modv[:, 4 * D:5 * D], scalar=1.0,
                                                         in1=gf[:], op0=ALU.add, op1=ALU.mult), [R_mv, R_b], [R_mv])
            P.dma("sp", self.modd[layer].rearrange("s j d -> s (j d)"), modv[:], reads=[R_mv], writes=[self.R_modd])
        S.close()

    def prenorm_tile(self, T, gt, A, RA, B, RB):
        P = self.P
        i = T["i"]
        T["i"] += 1
        b = i % 2
        xt, R_xt = T["xt"][b]
        hn, R_hn = T["hn"][b]
        pt, R_pt = T["pt"][b]
        st, R_st = T["st"][b]
        junk, R_junk = T["junk"]
        P.dma("sp", xt[:], self.xrows(gt), reads=[self.R_x[gt]], writes=[R_xt])
        P.op("act", lambda e: e.activation(out=junk[:], in_=xt[:], func=AF.Square, accum_out=st[:, 0:1]),
             [R_xt], [R_junk, R_st])
        P.op("act", lambda e: e.activation(out=st[:, 1:2], in_=st[:, 0:1], func=AF.Sqrt, bias=self.eps_t[:, 0:1], scale=1.0 / D),
             [R_st, self.R_const], [R_st])
        P.op("dve", lambda e: e.reciprocal(out=st[:, 2:3], in_=st[:, 1:2]), [R_st], [R_st])
        P.op("dve", lambda e: e.scalar_tensor_tensor(out=hn[:], in0=xt[:], scalar=st[:, 2:3], in1=A[:],
                                                     op0=ALU.mult, op1=ALU.mult), [R_xt, R_st, RA], [R_hn])
        P.op("pool", lambda e: e.tensor_tensor(out=hn[:], in0=hn[:], in1=B[:], op=ALU.add), [R_hn, RB], [R_hn])
        for k in range(8):
            P.op("pe", lambda e, k=k: e.transpose(out=pt[:, k, :], in_=hn[:, k * 128:(k + 1) * 128],
                                                  identity=self.ident_f[:]), [R_hn, self.R_const], [R_pt])
        return hn, R_hn, pt, R_pt

    def prenorm_tiles(self, S, npt=2):
        T = {"i": 0}
        T["xt"] = [(S.sb([128, D], F32, "xt"), Res()) for _ in range(2)]
        T["hn"] = [(S.sb([128, D], F32, "hn"), Res()) for _ in range(2)]
        T["pt"] = [(S.ps([128, 8, 128], F32, "pt"), Res()) for _ in range(npt)]
        if npt == 1:
            T["pt"] = T["pt"] * 2
        T["st"] = [(S.sb([128, 4], F32, "st"), Res()) for _ in range(2)]
        T["junk"] = (S.sb([128, D], BF16, "junk"), Res())
        return T

    def phase_moe(self, layer):
        nc, P = self.nc, self.P
        with_ctx = layer != DEPTH - 1
        gts = list(range(0 if with_ctx else 2, 34))
        L = ExitStack()
        gates_p = L.enter_context(nc.sbuf_tensor("gates_p%d" % layer, [128, 4, NEXP], F32))
        idx_p = L.enter_context(nc.sbuf_tensor("idx_p%d" % layer, [128, 4, NEXP], I32))
        gates_pc = L.enter_context(nc.sbuf_tensor("gates_pc%d" % layer, [32, NEXP], F32))
        idx_pc = L.enter_context(nc.sbuf_tensor("idx_pc%d" % layer, [32, NEXP], I32))
        R_sel = Res()

        S = Scope(self)
        A_l, RA_l = self.load_bc(S, layer, 0, 4)
        B_l, RB_l = self.load_bc(S, layer, 0, 3, q="act")
        if with_ctx:
            A_c, RA_c = self.load_bc(S, layer, 1, 4)
            B_c, RB_c = self.load_bc(S, layer, 1, 3, q="act")
        T = self.prenorm_tiles(S)
        hb = [(S.sb([128, D], BF16, "hb"), Res()) for _ in range(2)]
        hTf = [(S.sb([128, 8, 128], F32, "hTf"), Res()) for _ in range(2)]
        rt = S.sb([128, 8, NEXP], F32, "router")
        R_rt = Res()
        P.dma("act", rt[:], self.W["moe_router"][layer].rearrange("(k p) e -> p k e", p=128), writes=[R_rt])
        small = S.ps([128, 512], F32, "small")
        lg = [(small[:, 16 * j:16 * j + 16], Res()) for j in range(2)]
        afp = [(small[0:NEXP, 32 + 128 * j:160 + 128 * j], Res()) for j in range(2)]
        ex = [(S.sb([128, NEXP + 2], F32, "ex"), Res()) for _ in range(2)]
        affT = S.sb([NEXP, NLAT], F32, "affT")
        R_aff = Res()
        affTc = S.sb([NEXP, NCTX], F32, "affTc")
        R_affc = Res()
        D1 = Defer()

        def back(b, gt, hn, R_hn, pt, R_pt):
                hbt, R_hb = hb[b]
                P.op("act", lambda e, hbt=hbt, hn=hn: e.activation(out=hbt[:], in_=hn[:], func=AF.Copy), [R_hn], [R_hb])
                P.dma("sp", self.hrows(gt), hbt[:], reads=[R_hb], writes=[self.R_h[gt]])
                hf, R_hf = hTf[b]
                P.op("dve", lambda e, hf=hf, pt=pt: e.tensor_copy(out=hf[:], in_=pt[:]), [R_pt], [R_hf])
                lgt, R_lg = lg[b]
                for k in range(8):
                    P.op("pe", lambda e, k=k, hf=hf, lgt=lgt: e.matmul(lgt, lhsT=hf[:, k, :], rhs=rt[:, k, :],
                                                                        start=(k == 0), stop=(k == 7)),
                         [R_hf, R_rt], [R_lg])
                ext, R_ex = ex[b]
                P.op("act", lambda e, ext=ext, lgt=lgt: e.activation(out=ext[:, 0:NEXP], in_=lgt, func=AF.Exp,
                                                                      accum_out=ext[:, NEXP:NEXP + 1]), [R_lg], [R_ex])
                P.op("dve", lambda e, ext=ext: e.reciprocal(out=ext[:, NEXP + 1:NEXP + 2], in_=ext[:, NEXP:NEXP + 1]),
                     [R_ex], [R_ex])
                P.op("dve", lambda e, ext=ext: e.tensor_scalar(out=ext[:, 0:NEXP], in0=ext[:, 0:NEXP],
                                                               scalar1=ext[:, NEXP + 1:NEXP + 2], scalar2=None, op0=ALU.mult),
                     [R_ex], [R_ex])
                apt, R_ap = afp[b]
                P.op("pe", lambda e, ext=ext, apt=apt: e.transpose(out=apt, in_=ext[:, 0:NEXP], identity=self.ident_f[:]),
                     [R_ex, self.R_const], [R_ap])
                if gt < 2:
                    P.op("act", lambda e, apt=apt, gt=gt: e.activation(out=affTc[:, gt * 128:(gt + 1) * 128], in_=apt,
                                                                        func=AF.Copy), [R_ap], [R_affc])
                else:
                    P.op("act", lambda e, apt=apt, gt=gt: e.activation(out=affT[:, (gt - 2) * 128:(gt - 1) * 128], in_=apt,
                                                                        func=AF.Copy), [R_ap], [R_aff])

        for n_i, gt in enumerate(gts):
            b = n_i % 2
            if gt < 2:
                hn, R_hn, pt, R_pt = self.prenorm_tile(T, gt, A_c, RA_c, B_c, RB_c)
            else:
                hn, R_hn, pt, R_pt = self.prenorm_tile(T, gt, A_l, RA_l, B_l, RB_l)
            D1.push(lambda b=b, gt=gt, hn=hn, R_hn=R_hn, pt=pt, R_pt=R_pt: back(b, gt, hn, R_hn, pt, R_pt))
        D1.flush()
        if self.debug and layer == self.debug_layer:
            P.dma("sp", self.dbg_aff, affT[:], reads=[R_aff])
        vals = S.sb([NEXP, CAP_L], F32, "vals")
        idxu = S.sb([NEXP, CAP_L], U32, "idxu")
        idxf = S.sb([NEXP, CAP_L], F32, "idxf")
        R_v, R_i = Res(), Res()
        for r in range(CAP_L // 8):
            sl = slice(8 * r, 8 * r + 8)
            P.op("dve", lambda e, sl=sl: e.max(out=vals[:, sl], in_=affT[:]), [R_aff], [R_v])
            P.op("dve", lambda e, sl=sl: e.max_index(out=idxu[:, sl], in_max=vals[:, sl], in_values=affT[:]),
                 [R_aff, R_v], [R_i])
            P.op("dve", lambda e, sl=sl: e.match_replace(out=affT[:], in_to_replace=vals[:, sl], in_values=affT[:],
                                                         imm_value=-1.0), [R_aff, R_v], [R_aff])
        P.op("dve", lambda e: e.tensor_copy(out=idxf[:], in_=idxu[:]), [R_i], [R_i])
        tp = small[:, 288:352].rearrange("p (c e) -> p c e", c=4)
        R_tp = Res()
        for c in range(4):
            P.op("pe", lambda e, c=c: e.transpose(out=tp[:, c, :], in_=vals[:, c * 128:(c + 1) * 128],
                                                  identity=self.ident_f[0:NEXP, 0:NEXP]), [R_v, self.R_const], [R_tp])
        P.op("dve", lambda e: e.tensor_copy(out=gates_p[:], in_=tp), [R_tp], [R_sel])
        for c in range(4):
            P.op("pe", lambda e, c=c: e.transpose(out=tp[:, c, :], in_=idxf[:, c * 128:(c + 1) * 128],
                                                  identity=self.ident_f[0:NEXP, 0:NEXP]), [R_i, self.R_const, R_sel], [R_tp])
        P.op("dve", lambda e: e.tensor_copy(out=idx_p[:], in_=tp), [R_tp], [R_sel])
        if with_ctx:
            valsc = S.sb([NEXP, CAP_C], F32, "valsc")
            idxuc = S.sb([NEXP, CAP_C], U32, "idxuc")
            idxfc = S.sb([NEXP, CAP_C], F32, "idxfc")
            R_vc, R_ic = Res(), Res()
            for r in range(CAP_C // 8):
                sl = slice(8 * r, 8 * r + 8)
                P.op("dve", lambda e, sl=sl: e.max(out=valsc[:, sl], in_=affTc[:]), [R_affc], [R_vc])
                P.op("dve", lambda e, sl=sl: e.max_index(out=idxuc[:, sl], in_max=valsc[:, sl], in_values=affTc[:]),
                     [R_affc, R_vc], [R_ic])
                P.op("dve", lambda e, sl=sl: e.match_replace(out=affTc[:], in_to_replace=valsc[:, sl], in_values=affTc[:],
                                                             imm_value=-1.0), [R_affc, R_vc], [R_affc])
            P.op("dve", lambda e: e.tensor_copy(out=idxfc[:], in_=idxuc[:]), [R_ic], [R_ic])
            tpc = small[0:32, 352:384].rearrange("p (c e) -> p c e", c=2)
            R_tpc = Res()
            P.op("pe", lambda e: e.transpose(out=tpc[:, 0, :], in_=valsc[:, :], identity=self.ident_f[0:NEXP, 0:NEXP]),
                 [R_vc, self.R_const], [R_tpc])
            P.op("pe", lambda e: e.transpose(out=tpc[:, 1, :], in_=idxfc[:, :], identity=self.ident_f[0:NEXP, 0:NEXP]),
                 [R_ic, self.R_const], [R_tpc])
            P.op("dve", lambda e: e.tensor_copy(out=gates_pc[:], in_=tpc[:, 0, :]), [R_tpc], [R_sel])
            P.op("dve", lambda e: e.tensor_copy(out=idx_pc[:], in_=tpc[:, 1, :]), [R_tpc], [R_sel])
        if self.debug and layer == self.debug_layer:
            P.dma("sp", self.dbg_idx, idx_p[:].rearrange("p c e -> p (c e)"), reads=[R_sel])
            P.dma("sp", self.dbg_gate, gates_p[:].rearrange("p c e -> p (c e)"), reads=[R_sel])
        S.close()

        import os as _os
        if _os.environ.get("DBG_SKIP_F2"):
            L.close()
            return []
        S = Scope(self)
        G_l, RG_l = self.load_bc(S, layer, 0, 5)
        if with_ctx:
            G_c, RG_c = self.load_bc(S, layer, 1, 5, q="act")
        NS = CAP_L + (CAP_C if with_ctx else 0)
        stage = [(S.sb([128, 8, D], F32, "wst"), Res()) for _ in range(2)]
        wbuf = [(S.sb([128, 8, D], BF16, "wb"), Res()) for _ in range(3)]
        gth = [(S.sb([128, D], BF16, "gth"), Res()) for _ in range(5)]
        tpp = [(S.ps([128, 8, 128], BF16, "tpp"), Res()) for _ in range(2)]
        XeT = [(S.sb([128, 8, NS], BF16, "XeT"), Res()) for _ in range(2)]
        sg = (S.sb([128, 8, NS], F32, "sg"), Res())
        hid = (S.sb([128, 8, NS], BF16, "hid"), Res())
        hps = [(S.ps([128, 512], F32, "hps"), Res()) for _ in range(3)]
        yps = [(S.ps([128, 512], F32, "yps"), Res()) for _ in range(2)]
        hpc = (S.ps([128, 8, CAP_C], F32, "hpc"), Res())
        ysb = [(S.sb([128, D], F32, "ysb"), Res()) for _ in range(2)]
        R_scat_prev = []
        nstage = [0]
        nw = [0]
        cast_eng = ["act", "dve", "act"]
        ng = [0]
        ny = [0]
        nh = [0]
        last_ops = []

        def load_w(name, e, which):
            sb_, R_s = stage[nstage[0] % 2]
            nstage[0] += 1
            wb_, R_wb = wbuf[nw[0] % 3]
            nw[0] += 1
            src = self.W[name][layer, e].rearrange("(k p) n -> p k n", p=128)
            P.dma("sp", sb_[:, 0:4, :], src[:, 0:4, :], writes=[R_s])
            P.dma("sp", sb_[:, 4:8, :], src[:, 4:8, :], writes=[R_s])
            ce = cast_eng[which]
            if ce == "act":
                P.op("act", lambda e_: e_.activation(out=wb_[:], in_=sb_[:], func=AF.Copy), [R_s], [R_wb])
            else:
                P.op(ce, lambda e_: e_.tensor_copy(out=wb_[:], in_=sb_[:]), [R_s], [R_wb])
            return wb_, R_wb

        import os as _os
        scat_box = [[]]

        chunks = [(c, 128) for c in range(4)] + ([(4, CAP_C)] if with_ctx else [])
        NE = int(_os.environ.get("DBG_NEXP", NEXP))

        def do_gather(e):
            xe, R_xe = XeT[e % 2]
            for c, npart in chunks:
                g, R_g = gth[ng[0] % 5]
                ng[0] += 1
                if c < 4:
                    off = bass.IndirectOffsetOnAxis(ap=idx_p[:, c, e:e + 1], axis=0)
                    srcd, rds = self.hl, [self.R_h[i] for i in range(2, 34)]
                else:
                    off = bass.IndirectOffsetOnAxis(ap=idx_pc[:, e:e + 1], axis=0)
                    srcd, rds = self.hc, [self.R_h[0], self.R_h[1]]
                P.op("pool", lambda e_, g=g, npart=npart, srcd=srcd, off=off: e_.indirect_dma_start(
                    out=g[0:npart, :], out_offset=None, in_=srcd, in_offset=off), [R_sel] + rds, [R_g], dma=True)
                tp_, R_tp_ = tpp[ng[0] % 2]
                for k in range(8):
                    P.op("pe", lambda e_, g=g, npart=npart, tp_=tp_, k=k: e_.transpose(
                        out=tp_[:, k, 0:npart], in_=g[0:npart, k * 128:(k + 1) * 128],
                        identity=self.ident_b[0:npart, 0:npart]), [R_g, self.R_const], [R_tp_])
                P.op("dve", lambda e_, tp_=tp_, xe=xe, c=c, npart=npart: e_.tensor_copy(
                    out=xe[:, :, c * 128:c * 128 + npart], in_=tp_[:, :, 0:npart]), [R_tp_], [R_xe])

        def do_expert(e):
            R_scat_prev = scat_box[0]
            xe, R_xe = XeT[e % 2]
            if self.debug and e == 0 and layer == self.debug_layer:
                P.dma("sp", self.dbg_xe, xe[:, :, :].rearrange("p k s -> p (k s)"), reads=[R_xe])
            if int(_os.environ.get("DBG_STAGE", 9)) < 2:
                return
            wg, R_wg = load_w("moe_w_gate", e, 0)
            sgt, R_sg = sg
            hpct, R_hpc = hpc
            for f in range(8):
                hp, R_hp = hps[nh[0] % 3]
                nh[0] += 1
                for k in range(8):
                    P.op("pe", lambda e_, hp=hp, k=k, f=f: e_.matmul(hp[:, :], lhsT=wg[:, k, f * 128:(f + 1) * 128],
                                                                      rhs=xe[:, k, 0:CAP_L], start=(k == 0), stop=(k == 7)),
                         [R_wg, R_xe], [R_hp])
                if with_ctx:
                    for k in range(8):
                        P.op("pe", lambda e_, k=k, f=f: e_.matmul(hpct[:, f, :], lhsT=wg[:, k, f * 128:(f + 1) * 128],
                                                                   rhs=xe[:, k, CAP_L:NS], start=(k == 0), stop=(k == 7)),
                             [R_wg, R_xe], [R_hpc])
                P.op("act", lambda e_, hp=hp, f=f: e_.activation(out=sgt[:, f, 0:CAP_L], in_=hp[:, :], func=AF.Silu),
                     [R_hp], [R_sg])
            if with_ctx:
                P.op("act", lambda e_: e_.activation(out=sgt[:, :, CAP_L:NS], in_=hpct[:, :, :], func=AF.Silu),
                     [R_hpc], [R_sg])
            if int(_os.environ.get("DBG_STAGE", 9)) < 3:
                if self.debug and e == 0:
                    P.dma("sp", self.dbg_sg, sgt[:, :, :].rearrange("p k s -> p (k s)"), reads=[R_sg])
                return
            wu, R_wu = load_w("moe_w_up", e, 1)
            hd_, R_hid = hid
            for f in range(8):
                hp, R_hp = hps[nh[0] % 3]
                nh[0] += 1
                for k in range(8):
                    P.op("pe", lambda e_, hp=hp, k=k, f=f: e_.matmul(hp[:, :], lhsT=wu[:, k, f * 128:(f + 1) * 128],
                                                                      rhs=xe[:, k, 0:CAP_L], start=(k == 0), stop=(k == 7)),
                         [R_wu, R_xe], [R_hp])
                if with_ctx:
                    for k in range(8):
                        P.op("pe", lambda e_, k=k, f=f: e_.matmul(hpct[:, f, :], lhsT=wu[:, k, f * 128:(f + 1) * 128],
                                                                   rhs=xe[:, k, CAP_L:NS], start=(k == 0), stop=(k == 7)),
                             [R_wu, R_xe], [R_hpc])
                P.op("dve", lambda e_, hp=hp, f=f: e_.tensor_tensor(out=hd_[:, f, 0:CAP_L], in0=hp[:, :],
                                                                     in1=sgt[:, f, 0:CAP_L], op=ALU.mult),
                     [R_hp, R_sg], [R_hid])
            if with_ctx:
                P.op("dve", lambda e_: e_.tensor_tensor(out=hd_[:, :, CAP_L:NS], in0=hpct[:, :, :],
                                                        in1=sgt[:, :, CAP_L:NS], op=ALU.mult), [R_hpc, R_sg], [R_hid])
            if self.debug and e == 0 and layer == self.debug_layer:
                P.dma("sp", self.dbg_hid, hd_[:, :, :].rearrange("p k s -> p (k s)"), reads=[R_hid])
                P.dma("sp", self.dbg_sg, sgt[:, :, :].rearrange("p k s -> p (k s)"), reads=[R_sg])
                P.dma("sp", self.dbg_xe2, xe[:, :, :].rearrange("p k s -> p (k s)"), reads=[R_xe])
                P.dma("sp", self.dbg_wg, wg[:, :, :].rearrange("p k s -> p (k s)"), reads=[R_wg])
            if int(_os.environ.get("DBG_STAGE", 9)) < 4:
                return
            if e + 1 < NE:
                do_gather(e + 1)
            wd, R_wd = load_w("moe_w_down", e, 2)
            R_scat_cur = []
            for c, npart in chunks:
                yt, R_y = ysb[ny[0] % 2]
                ny[0] += 1
                for half in range(2):
                    yp, R_yp = yps[half]
                    for f in range(8):
                        P.op("pe", lambda e_, yp=yp, f=f, c=c, npart=npart, half=half: e_.matmul(
                            yp[0:npart, :], lhsT=hd_[:, f, c * 128:c * 128 + npart],
                            rhs=wd[:, f, half * 512:(half + 1) * 512], start=(f == 0), stop=(f == 7)),
                            [R_hid, R_wd], [R_yp])
                    if c < 4:
                        gcol, Gt, RG = gates_p[:, c, e:e + 1], G_l, RG_l
                    else:
                        gcol, Gt, RG = gates_pc[:, e:e + 1], G_c, RG_c
                    P.op("dve", lambda e_, yt=yt, yp=yp, gcol=gcol, Gt=Gt, npart=npart, half=half: e_.scalar_tensor_tensor(
                        out=yt[0:npart, half * 512:(half + 1) * 512], in0=yp[0:npart, :], scalar=gcol,
                        in1=Gt[0:npart, half * 512:(half + 1) * 512], op0=ALU.mult, op1=ALU.mult),
                        [R_yp, R_sel, RG], [R_y])
                R_sc = Res()
                if c < 4:
                    off = bass.IndirectOffsetOnAxis(ap=idx_p[:, c, e:e + 1], axis=0)
                    dst = self.xl
                else:
                    off = bass.IndirectOffsetOnAxis(ap=idx_pc[:, e:e + 1], axis=0)
                    dst = self.xc
                o = P.op("pool", lambda e_, yt=yt, npart=npart, dst=dst, off=off: e_.indirect_dma_start(
                    out=dst, out_offset=off, in_=yt[0:npart, :], in_offset=None, compute_op=ALU.add),
                    [R_y, R_sel] + R_scat_prev, [R_sc], dma=True)
                R_scat_cur.append(R_sc)
                last_ops.append(o)
            scat_box[0] = R_scat_cur

        do_gather(0)
        for e in range(NE):
            do_expert(e)
        R_scat_prev = scat_box[0]
        fo = P.dma("pool", self.fence_d, self.eps_t[:], reads=R_scat_prev + [self.R_const], writes=self.R_x)
        last_ops = [fo]
        S.close()
        L.close()
        return last_ops


    def load_cast(self, S, src_ap, shape, stage, R_stage, q="sp", eng="dve", name="w", into=None):
        P = self.P
        if into is None:
            wt = S.sb(shape, BF16, name)
            R = Res()
        else:
            wt, R = into
        n = 1
        for d_ in shape[1:]:
            n *= d_
        sv = stage[0:shape[0], 0:n]
        if len(shape) == 3:
            sv = sv.rearrange("p (a b) -> p a b", a=shape[1])
        P.dma(q, sv, src_ap, writes=[R_stage])
        wap = wt[:]
        if eng == "act":
            P.op("act", lambda e: e.activation(out=wap, in_=sv, func=AF.Copy), [R_stage], [R])
        else:
            P.op(eng, lambda e: e.tensor_copy(out=wap, in_=sv), [R_stage], [R])
        return wt, R

    def normrope(self, *args):
        for _ in self.normrope_gen(*args):
            pass

    @staticmethod
    def run_zip(gens):
        gens = list(gens)
        while gens:
            for g in list(gens):
                try:
                    next(g)
                except StopIteration:
                    gens.remove(g)

    def normrope_gen(self, Wk, ps, R_ps, M, T, ones_ap, inv_n, gcol, R_g, rm, R_rm, cos_ap, sin_ap, R_tab, out_ap, R_out):
        P = self.P
        sq, R_sq = Wk["sq"]
        sm, R_sm = Wk["sm"]
        r, R_r = Wk["r"]
        xnf, R_xnf = Wk["xnf"]
        xnb, R_xnb = Wk["xnb"]
        rp, R_rp = Wk["rp"]
        t1, R_t1 = Wk["t1"]
        P.op("act", lambda e: e.activation(out=sq[0:M, 0:T], in_=ps, func=AF.Square), [R_ps], [R_sq])
        yield
        P.op("pe", lambda e: e.matmul(sm[0:M, 0:T], lhsT=ones_ap, rhs=sq[0:M, 0:T], start=True, stop=True),
             [R_sq, self.R_const], [R_sm])
        yield
        P.op("act", lambda e: e.activation(out=r[0:M, 0:T], in_=sm[0:M, 0:T], func=AF.Sqrt, bias=self.eps_t[0:M, 0:1],
                                           scale=inv_n), [R_sm, self.R_const], [R_r])
        yield
        P.op("dve", lambda e: e.reciprocal(out=r[0:M, 0:T], in_=r[0:M, 0:T]), [R_r], [R_r])
        P.op("dve", lambda e: e.scalar_tensor_tensor(out=xnf[0:M, 0:T], in0=ps, scalar=gcol, in1=r[0:M, 0:T],
                                                     op0=ALU.mult, op1=ALU.mult), [R_ps, R_g, R_r], [R_xnf])
        yield
        P.op("act", lambda e: e.activation(out=xnb[0:M, 0:T], in_=xnf[0:M, 0:T], func=AF.Copy), [R_xnf], [R_xnb])
        yield
        P.op("pe", lambda e: e.matmul(rp[0:M, 0:T], lhsT=rm, rhs=xnb[0:M, 0:T], start=True, stop=True),
             [R_xnb, R_rm], [R_rp])
        P.op("pool", lambda e: e.tensor_tensor(out=xnf[0:M, 0:T], in0=xnf[0:M, 0:T], in1=cos_ap, op=ALU.mult),
             [R_xnf, R_tab], [R_xnf])
        yield
        P.op("dve", lambda e: e.tensor_tensor(out=t1[0:M, 0:T], in0=rp[0:M, 0:T], in1=sin_ap, op=ALU.mult),
             [R_rp, R_tab], [R_t1])
        P.op("dve", lambda e: e.tensor_tensor(out=out_ap, in0=xnf[0:M, 0:T], in1=t1[0:M, 0:T], op=ALU.add),
             [R_xnf, R_t1], [R_out])
        yield

    def normrope_tiles(self, S):
        Wk = {}
        Wk["sq"] = (S.sb([128, 512], BF16, "sq"), Res())
        Wk["sm"] = (S.ps([128, 512], F32, "sm"), Res())
        Wk["r"] = (S.sb([128, 512], F32, "r"), Res())
        Wk["xnf"] = (S.sb([128, 512], F32, "xnf"), Res())
        Wk["xnb"] = (S.sb([128, 512], BF16, "xnb"), Res())
        Wk["rp"] = (S.ps([128, 512], F32, "rp"), Res())
        Wk["t1"] = (S.sb([128, 512], F32, "t1"), Res())
        return Wk

    def qblocks(self, need_ctx_out):
        qb = []
        if need_ctx_out:
            qb.append((0, 256, [0, 1]))
        for b in range(8):
            qb.append((256 + 512 * b, 512, list(range(34))))
        return qb

    def phase_outproj(self, layer, wname, j, need_ctx_out, src=None, nk=8):
        P = self.P
        S = Scope(self)
        G_l, RG_l = self.load_bc(S, layer, 0, 2)
        if need_ctx_out:
            G_c, RG_c = self.load_bc(S, layer, 1, 2, q="act")
        stage = S.sb([128, 8 * D], F32, "stg")
        R_stage = Res()
        src = self.od if src is None else src
        wo = S.sb([128, nk, D], BF16, "wo")
        R_wo = Res()
        wv_ = self.W[wname][j].rearrange("(k p) n -> p k n", p=128)
        for k0 in range(0, nk, 8):
            self.load_cast(S, wv_[:, k0:k0 + 8, :], [128, 8, D], stage, R_stage, eng="act", into=(wo[:, k0:k0 + 8, :], R_wo))
        ot = [(S.sb([128, nk, 128], BF16, "ot"), Res()) for _ in range(2)]
        xt = [(S.sb([128, D], F32, "xt"), Res()) for _ in range(2)]
        yps = [(S.ps([128, 512], F32, "yps"), Res()) for _ in range(4)]
        odv = src.rearrange("(k p) t -> p k t", p=128)
        gts = list(range(0 if need_ctx_out else 2, 34))
        for n_i, gt in enumerate(gts):
            b = n_i % 2
            o_t, R_o = ot[b]
            x_t, R_xt = xt[b]
            P.dma("sp", o_t[:], odv[:, :, gt * 128:(gt + 1) * 128], reads=[self.R_od], writes=[R_o])
            P.dma("act", x_t[:], self.xrows(gt), reads=[self.R_x[gt]], writes=[R_xt])
            Gt, RG = (G_c, RG_c) if gt < 2 else (G_l, RG_l)
            for half in range(2):
                yp, R_yp = yps[2 * b + half]
                for k in range(nk):
                    P.op("pe", lambda e, yp=yp, k=k, o_t=o_t, half=half: e.matmul(
                        yp[:, :], lhsT=o_t[:, k, :], rhs=wo[:, k, half * 512:(half + 1) * 512], start=(k == 0), stop=(k == nk - 1)),
                        [R_o, R_wo], [R_yp])
                hs = slice(half * 512, (half + 1) * 512)
                P.op("dve", lambda e, yp=yp, Gt=Gt, x_t=x_t, hs=hs: e.tensor_tensor(out=yp[:, :], in0=yp[:, :], in1=Gt[:, hs], op=ALU.mult),
                     [R_yp, RG], [R_yp])
                P.op("dve", lambda e, yp=yp, x_t=x_t, hs=hs: e.tensor_tensor(out=x_t[:, hs], in0=yp[:, :], in1=x_t[:, hs], op=ALU.add),
                     [R_yp, R_xt], [R_xt])
            P.dma("sp", self.xrows(gt), x_t[:], reads=[R_xt], writes=[self.R_x[gt]])
        S.close()

    def phase_mla(self, layer):
        nc, P = self.nc, self.P
        j = layer // 3
        need_ctx_out = layer != DEPTH - 1
        L = ExitStack()
        cqT = L.enter_context(nc.sbuf_tensor("cqT%d" % layer, [128, 4, NTOK], BF16))
        ckvT = L.enter_context(nc.sbuf_tensor("ckvT%d" % layer, [128, 2, NTOK], BF16))
        krT = L.enter_context(nc.sbuf_tensor("krT%d" % layer, [32, NTOK], BF16))
        R_cq = [Res() for _ in range(9)]
        R_ckv = [Res() for _ in range(9)]
        R_kr = [Res() for _ in range(9)]
        blocks = [(0, 256)] + [(256 + 512 * b, 512) for b in range(8)]

        S = Scope(self)
        A_l, RA_l = self.load_bc(S, layer, 0, 1)
        B_l, RB_l = self.load_bc(S, layer, 0, 0, q="act")
        A_c, RA_c = self.load_bc(S, layer, 1, 1)
        B_c, RB_c = self.load_bc(S, layer, 1, 0, q="act")
        T_ = self.prenorm_tiles(S, npt=1)
        stage = S.sb([128, 8 * 800], F32, "stg")
        R_stage = Res()
        win, R_win = self.load_cast(S, self.W["mla_w_in"][j].rearrange("(k p) n -> p k n", p=128), [128, 8, 800],
                                    stage, R_stage, eng="act", name="win")
        gq = S.sb([128, 4], F32, "gq")
        gkv = S.sb([128, 2], F32, "gkv")
        R_gl = Res()
        P.dma("act", gq[:], self.W["mla_q_norm_g"][j].rearrange("(a p) -> p a", p=128), writes=[R_gl], allow_slow_non_contiguous=True)
        P.dma("act", gkv[:], self.W["mla_kv_norm_g"][j].rearrange("(a p) -> p a", p=128), writes=[R_gl], allow_slow_non_contiguous=True)
        hTb = [(S.sb([128, 8, 512], BF16, "hTb"), Res()) for _ in range(2)]
        dps = [(S.ps([128, 512], F32, "dps"), Res()) for _ in range(4)]
        smp = (S.ps([128, 512], F32, "smp"), Res())
        sq = (S.sb([128, 512], BF16, "sq"), Res())
        rr = (S.sb([128, 512], F32, "rr"), Res())
        D2 = Defer()
        for bi, (t0, T) in enumerate(blocks):
            hb_, R_hb = hTb[bi % 2]
            for ti in range(T // 128):
                gt = t0 // 128 + ti
                if gt < 2:
                    hn, R_hn, pt, R_pt = self.prenorm_tile(T_, gt, A_c, RA_c, B_c, RB_c)
                else:
                    hn, R_hn, pt, R_pt = self.prenorm_tile(T_, gt, A_l, RA_l, B_l, RB_l)
                P.op("dve", lambda e, hb_=hb_, pt=pt, ti=ti: e.tensor_copy(out=hb_[:, :, ti * 128:(ti + 1) * 128], in_=pt[:]),
                     [R_pt], [R_hb])
            for grp, chunks, gain, dst, R_dst, nfeat in ((0, [0, 1, 2, 3], gq, cqT, R_cq, 512.0), (1, [4, 5], gkv, ckvT, R_ckv, 256.0)):
                smt, R_smt = smp
                for ci, cj in enumerate(chunks):
                    dp, R_dp = dps[ci]
                    for k in range(8):
                        P.op("pe", lambda e, dp=dp, k=k, cj=cj, hb_=hb_, T=T: e.matmul(
                            dp[:, 0:T], lhsT=win[:, k, cj * 128:(cj + 1) * 128], rhs=hb_[:, k, 0:T], start=(k == 0), stop=(k == 7)),
                            [R_win, R_hb], [R_dp])
                    sqt, R_sq = sq
                    P.op("act", lambda e, dp=dp, sqt=sqt, T=T: e.activation(out=sqt[:, 0:T], in_=dp[:, 0:T], func=AF.Square),
                         [R_dp], [R_sq])
                    P.op("pe", lambda e, sqt=sqt, smt=smt, T=T, ci=ci, n=len(chunks): e.matmul(
                        smt[:, 0:T], lhsT=self.ones_b[:, :], rhs=sqt[:, 0:T], start=(ci == 0), stop=(ci == n - 1)),
                        [R_sq, self.R_const], [R_smt])
                rt_, R_rr = rr
                P.op("act", lambda e, rt_=rt_, smt=smt, T=T, nfeat=nfeat: e.activation(
                    out=rt_[:, 0:T], in_=smt[:, 0:T], func=AF.Sqrt, bias=self.eps_t[:, 0:1], scale=1.0 / nfeat),
                    [R_smt, self.R_const], [R_rr])
                P.op("dve", lambda e, rt_=rt_, T=T: e.reciprocal(out=rt_[:, 0:T], in_=rt_[:, 0:T]), [R_rr], [R_rr])
                for ci, cj in enumerate(chunks):
                    dp, R_dp = dps[ci]
                    P.op("dve", lambda e, dp=dp, ci=ci, gain=gain, rt_=rt_, dst=dst, t0=t0, T=T: e.scalar_tensor_tensor(
                        out=dst[:, ci, t0:t0 + T], in0=dp[:, 0:T], scalar=gain[:, ci:ci + 1], in1=rt_[:, 0:T],
                        op0=ALU.mult, op1=ALU.mult), [R_dp, R_gl, R_rr], [R_dst[bi]])
            dp, R_dp = dps[0]
            for k in range(8):
                P.op("pe", lambda e, dp=dp, k=k, hb_=hb_, T=T: e.matmul(dp[0:32, 0:T], lhsT=win[:, k, 768:800], rhs=hb_[:, k, 0:T],
                                                                         start=(k == 0), stop=(k == 7)), [R_win, R_hb], [R_dp])
            P.op("act", lambda e, dp=dp, t0=t0, T=T: e.activation(out=krT[0:32, t0:t0 + T], in_=dp[0:32, 0:T], func=AF.Copy),
                 [R_dp], [R_kr[bi]])
        S.close()

        if int(_os_environ_get("DBG_MLA", 3)) < 2:
            L.close()
            return []
        S = Scope(self)
        stage = S.sb([128, 4 * 1536], F32, "stg")
        R_stage = Res()
        wuq, R_wuq = self.load_cast(S, self.W["mla_w_uq"][j].rearrange("(k p) n -> p k n", p=128), [128, 4, 1536],
                                    stage, R_stage, eng="act", name="wuq")
        wukv, R_wukv = self.load_cast(S, self.W["mla_w_ukv"][j].rearrange("(k p) n -> p k n", p=128), [128, 2, 2048],
                                      stage, R_stage, eng="act", name="wukv")
        wkp = S.sb([128, 2, 16, 96], BF16, "wkp")
        R_wkp = Res()
        P.op("pool", lambda e: e.memset(wkp[:], 0.0), [], [R_wkp])
        wv4 = wukv[:, :, :].rearrange("p a (h c) -> p a h c", h=16)
        for a in range(2):
            P.op("dve", lambda e, a=a: e.tensor_copy(out=wkp[:, a, :, 0:64], in_=wv4[:, a, :, 0:64]), [R_wukv, R_wkp], [R_wkp])
        rm, R_rm = self.load_cast(S, self.c_rm96, [96, 96], stage, R_stage, name="rm96")
        sel = S.sb([32, 96], BF16, "sel")
        R_sel96 = Res()
        P.op("pool", lambda e: e.memset(sel[:], 0.0), [], [R_sel96])
        P.op("dve", lambda e: e.tensor_copy(out=sel[:, 64:96], in_=self.ident_b[0:32, 0:32]), [self.R_const, R_sel96], [R_sel96])
        gqn = S.sb([96, 1], F32, "gqn")
        gkn = S.sb([96, 1], F32, "gkn")
        R_gh = Res()
        P.dma("act", gqn[:], self.W["mla_qn_g"][j].rearrange("(p o) -> p o", o=1), writes=[R_gh])
        P.dma("act", gkn[:], self.W["mla_kn_g"][j].rearrange("(p o) -> p o", o=1), writes=[R_gh])
        sps = [(S.ps([128, 512], F32, "sps"), Res()) for _ in range(7)]
        Wk = {}
        Wk["sq"] = (S.sb([128, 512], BF16, "sq"), Res())
        Wk["sm"] = sps[1]
        Wk["r"] = (S.sb([128, 512], F32, "r"), Res())
        Wk["xnf"] = (S.sb([128, 512], F32, "xnf"), Res())
        Wk["xnb"] = (S.sb([128, 512], BF16, "xnb"), Res())
        Wk["rp"] = sps[2]
        Wk["t1"] = (S.sb([128, 512], F32, "t1"), Res())
        Wk2 = {"sq": (S.sb([128, 512], BF16, "sq2"), Res()), "sm": sps[4], "r": (S.sb([128, 512], F32, "r2"), Res()),
               "xnf": (S.sb([128, 512], F32, "xnf2"), Res()), "xnb": (S.sb([128, 512], BF16, "xnb2"), Res()), "rp": sps[5],
               "t1": (S.sb([128, 512], F32, "t12"), Res())}
        cs = [(S.sb([96, 512], F32, "cos"), S.sb([96, 512], F32, "sin"), Res()) for _ in range(2)]
        QT = (S.sb([96, NTOK], BF16, "QT"), Res())
        KT = (S.sb([96, NTOK], BF16, "KT"), Res())
        Va = (S.sb([128, 34, 128], BF16, "Va"), Res())
        P.op("pool", lambda e: e.memset(Va[0][:, :, 64:128], 1.0), [], [Va[1]])
        gps = sps[0]
        acc = (S.ps([128, 512], F32, "acc"), Res())
        NB, GRP = 6, 3
        pts = [(S.sb([128, 512], BF16, "pt"), Res()) for _ in range(NB)]
        rec = (S.sb([64, 512], F32, "rec"), Res())
        otl = [(S.sb([64, 512], BF16, "ot"), Res()) for _ in range(2)]
        ncs = [0]
        scale = 96.0 ** -0.5

        def do_head(h):
            qt, R_qt = QT
            kt, R_kt = KT
            va, R_va = Va
            gp, R_gp = gps
            for bi, (t0, T) in enumerate(blocks):
                cos_t, sin_t, R_tab = cs[ncs[0] % 2]
                ncs[0] += 1
                P.dma("sp", cos_t[:, 0:T], self.c_cosA[:, t0:t0 + T], writes=[R_tab])
                P.dma("sp", sin_t[:, 0:T], self.c_sinA[:, t0:t0 + T], writes=[R_tab])
                def kchain(bi=bi, t0=t0, T=T, cos_t=cos_t, sin_t=sin_t, R_tab=R_tab):
                    for a in range(2):
                        P.op("pe", lambda e, a=a: e.matmul(gp[0:96, 0:T], lhsT=wkp[:, a, h, :], rhs=ckvT[:, a, t0:t0 + T],
                                                           start=(a == 0), stop=False), [R_wkp, R_ckv[bi]], [R_gp])
                    P.op("pe", lambda e: e.matmul(gp[0:96, 0:T], lhsT=sel[:, :], rhs=krT[0:32, t0:t0 + T],
                                                  start=False, stop=True), [R_sel96, R_kr[bi]], [R_gp])
                    yield
                    yield from self.normrope_gen(Wk, gp[0:96, 0:T], R_gp, 96, T, self.ones_b[0:96, 0:96], 1.0 / 96, gkn[:, 0:1], R_gh,
                                                 rm[:, :], R_rm, cos_t[:, 0:T], sin_t[:, 0:T], R_tab, kt[:, t0:t0 + T], R_kt)

                def qchain(bi=bi, t0=t0, T=T, cos_t=cos_t, sin_t=sin_t, R_tab=R_tab):
                    gq_, R_gq_ = sps[3]
                    for a in range(4):
                        P.op("pe", lambda e, a=a: e.matmul(gq_[0:96, 0:T], lhsT=wuq[:, a, h * 96:(h + 1) * 96],
                                                           rhs=cqT[:, a, t0:t0 + T], start=(a == 0), stop=(a == 3)),
                             [R_wuq, R_cq[bi]], [R_gq_])
                    yield
                    yield from self.normrope_gen(Wk2, gq_[0:96, 0:T], R_gq_, 96, T, self.ones_b[0:96, 0:96], 1.0 / 96, gqn[:, 0:1], R_gh,
                                                 rm[:, :], R_rm, cos_t[:, 0:T], sin_t[:, 0:T], R_tab, qt[:, t0:t0 + T], R_qt)
                chains = [kchain()]
                if bi > 0 or need_ctx_out:
                    chains.append(qchain())
                self.run_zip(chains)
                for ti in range(T // 128):
                    kc = t0 // 128 + ti
                    for a in range(2):
                        P.op("pe", lambda e, a=a, kc=kc, ti=ti: e.matmul(
                            gp[:, ti * 64:(ti + 1) * 64], lhsT=ckvT[:, a, kc * 128:(kc + 1) * 128], rhs=wv4[:, a, h, 64:128],
                            start=(a == 0), stop=(a == 1)), [R_wukv, R_ckv[bi]], [R_gp])
                nt = T // 128
                P.op("act", lambda e, t0=t0, nt=nt: e.activation(
                    out=va[:, t0 // 128:t0 // 128 + nt, 0:64], in_=gp[:, 0:nt * 64].rearrange("p (t c) -> p t c", t=nt),
                    func=AF.Copy), [R_gp], [R_va])
            if self.debug and h == 0:
                P.dma("sp", self.dbg_qt[0:96, :], qt[:, :], reads=[R_qt])
                P.dma("sp", self.dbg_kt[0:96, :], kt[:, :], reads=[R_kt])
                P.dma("sp", self.dbg_va, va[:, :, :].rearrange("p a b -> p (a b)"), reads=[R_va])
            for qi, (q0, T, kcs) in enumerate(self.qblocks(need_ctx_out)):
                ac, R_ac = (acc, sps[0])[qi % 2]
                n = len(kcs)

                def score(i):
                    sp_, R_sp = sps[1 + i % NB]
                    kc = kcs[i]
                    P.op("pe", lambda e, sp_=sp_, kc=kc, T=T, q0=q0: e.matmul(sp_[:, 0:T], lhsT=kt[:, kc * 128:(kc + 1) * 128],
                                                                   rhs=qt[:, q0:q0 + T], start=True, stop=True),
                         [R_kt, R_qt], [R_sp])
                    pt_, R_pt_ = pts[i % NB]
                    P.op("act", lambda e, sp_=sp_, pt_=pt_, T=T: e.activation(out=pt_[:, 0:T], in_=sp_[:, 0:T], func=AF.Exp, scale=scale),
                         [R_sp], [R_pt_])

                def pv(i, first, last):
                    pt_, R_pt_ = pts[i % NB]
                    kc = kcs[i]
                    P.op("pe", lambda e, pt_=pt_, kc=kc, T=T, ac=ac, first=first, last=last: e.matmul(
                        ac[:, 0:T], lhsT=va[:, kc, :], rhs=pt_[:, 0:T], start=first, stop=last), [R_va, R_pt_], [R_ac])
                groups = [list(range(a, min(a + GRP, n))) for a in range(0, n, GRP)]
                for i in groups[0]:
                    score(i)
                for gi, grp in enumerate(groups):
                    if gi + 1 < len(groups):
                        for i in groups[gi + 1]:
                            score(i)
                    for i in reversed(grp):
                        pv(i, first=(gi == 0 and i == grp[-1]), last=(gi == len(groups) - 1 and i == grp[0]))
                rc, R_rc = rec
                o_t, R_ot = otl[qi % 2]
                P.op("dve", lambda e, T=T, ac=ac, rc=rc: e.reciprocal(out=rc[:, 0:T], in_=ac[64:128, 0:T]), [R_ac], [R_rc])
                P.op("dve", lambda e, o_t=o_t, T=T, ac=ac, rc=rc: e.tensor_tensor(out=o_t[:, 0:T], in0=ac[0:64, 0:T], in1=rc[:, 0:T], op=ALU.mult),
                     [R_ac, R_rc], [R_ot])
                P.dma("sp", self.od[h * 64:(h + 1) * 64, q0:q0 + T], o_t[:, 0:T], reads=[R_ot], writes=[self.R_od])

        for h in range(int(_os_environ_get("DBG_NHEAD", 16))):
            do_head(h)
        S.close()
        L.close()
        if int(_os_environ_get("DBG_MLA", 3)) < 3:
            return []
        self.phase_outproj(layer, "mla_w_out", j, need_ctx_out)
        return []

    def phase_diff(self, layer):
        nc, P = self.nc, self.P
        j = layer // 3
        need_ctx_out = layer != DEPTH - 1
        lam_init = 0.8 - 0.6 * math.exp(-0.3 * layer)
        L = ExitStack()
        hT = L.enter_context(nc.sbuf_tensor("hT%d" % layer, [128, 8, NTOK], BF16))
        R_hT = [Res() for _ in range(9)]
        blocks = [(0, 256)] + [(256 + 512 * b, 512) for b in range(8)]
        S = Scope(self)
        A_l, RA_l = self.load_bc(S, layer, 0, 1)
        B_l, RB_l = self.load_bc(S, layer, 0, 0, q="act")
        A_c, RA_c = self.load_bc(S, layer, 1, 1)
        B_c, RB_c = self.load_bc(S, layer, 1, 0, q="act")
        T_ = self.prenorm_tiles(S)
        D3 = Defer()
        for gt in range(34):
            if gt < 2:
                hn, R_hn, pt, R_pt = self.prenorm_tile(T_, gt, A_c, RA_c, B_c, RB_c)
                bi = 0
            else:
                hn, R_hn, pt, R_pt = self.prenorm_tile(T_, gt, A_l, RA_l, B_l, RB_l)
                bi = 1 + (gt - 2) // 4
            D3.push(lambda pt=pt, gt=gt, R_pt=R_pt, bi=bi: P.op(
                "dve", lambda e: e.tensor_copy(out=hT[:, :, gt * 128:(gt + 1) * 128], in_=pt[:]), [R_pt], [R_hT[bi]]))
        D3.flush()
        S.close()
        S = Scope(self)
        stage = S.sb([128, 8 * 128], F32, "stg")
        R_stage = Res()
        win = self.W["diff_w_in"][j].rearrange("(k p) n -> p k n", p=128)
        bd = S.sb([128, 128], BF16, "bd")
        R_bd = Res()
        P.op("pool", lambda e: e.memset(bd[:], 0.0), [], [R_bd])
        P.op("dve", lambda e: e.tensor_copy(out=bd[0:64, 0:64], in_=self.ones_b[0:64, 0:64]), [self.R_const, R_bd], [R_bd])
        P.op("dve", lambda e: e.tensor_copy(out=bd[64:128, 64:128], in_=self.ones_b[64:128, 0:64]), [self.R_const, R_bd], [R_bd])
        rm, R_rm = self.load_cast(S, self.c_rm128, [128, 128], stage, R_stage, name="rm128")
        gq = S.sb([128, 1], F32, "gq")
        gk = S.sb([128, 1], F32, "gk")
        gs = S.sb([128, 1], F32, "gs")
        R_gh = Res()
        for half in range(2):
            P.dma("act", gq[half * 64:(half + 1) * 64, :], self.W["diff_qn_g"][j].rearrange("(p o) -> p o", o=1), writes=[R_gh])
            P.dma("act", gk[half * 64:(half + 1) * 64, :], self.W["diff_kn_g"][j].rearrange("(p o) -> p o", o=1), writes=[R_gh])
        P.dma("act", gs[:], self.W["diff_sub_g"][j].rearrange("(p o) -> p o", o=1), writes=[R_gh])
        P.op("dve", lambda e: e.tensor_scalar(out=gs[:], in0=gs[:], scalar1=float(1.0 - lam_init), scalar2=None, op0=ALU.mult),
             [R_gh], [R_gh])
        lv = S.sb([1, 4, 64], F32, "lv")
        lw = S.sb([1, 8], F32, "lw")
        onesf = S.sb([1, 128], F32, "onesf")
        neglam = S.sb([128, 1], F32, "neglam")
        R_lv = Res()
        for n_i, nm in enumerate(("diff_lambda_q1", "diff_lambda_k1", "diff_lambda_q2", "diff_lambda_k2")):
            P.dma("act", lv[0:1, n_i, :], self.W[nm][j:j + 1, :], writes=[R_lv])
        P.op("dve", lambda e: e.memset(onesf[:], 1.0), [], [R_lv])
        P.op("dve", lambda e: e.memset(lw[:], 0.0), [], [R_lv])
        P.op("dve", lambda e: e.tensor_tensor(out=lv[0:1, 0, :], in0=lv[0:1, 0, :], in1=lv[0:1, 1, :], op=ALU.mult), [R_lv], [R_lv])
        P.op("dve", lambda e: e.tensor_tensor(out=lv[0:1, 1, :], in0=lv[0:1, 2, :], in1=lv[0:1, 3, :], op=ALU.mult), [R_lv], [R_lv])
        P.op("dve", lambda e: e.tensor_reduce(out=lw[0:1, 0:2], in_=lv[0:1, 0:2, :], axis=mybir.AxisListType.X, op=ALU.add),
             [R_lv], [R_lv])
        P.op("act", lambda e: e.activation(out=lw[0:1, 2:4], in_=lw[0:1, 0:2], func=AF.Exp), [R_lv], [R_lv])
        P.op("dve", lambda e: e.tensor_tensor(out=lw[0:1, 4:5], in0=lw[0:1, 3:4], in1=lw[0:1, 2:3], op=ALU.subtract), [R_lv], [R_lv])
        P.op("dve", lambda e: e.tensor_scalar(out=lw[0:1, 4:5], in0=lw[0:1, 4:5], scalar1=float(-lam_init), scalar2=None, op0=ALU.add),
             [R_lv], [R_lv])
        Bk = [(S.ps([128, 512], F32, "bk"), Res()) for _ in range(5)]
        sps = [(S.ps([128, 512], F32, "sps"), Res()) for _ in range(3)]
        P.op("pe", lambda e: e.matmul(Bk[4][0][:, 0:2], lhsT=onesf[0:1, :], rhs=lw[0:1, 4:6], start=True, stop=True), [R_lv], [Bk[4][1]])
        P.op("dve", lambda e: e.tensor_copy(out=neglam[:], in_=Bk[4][0][:, 0:1]), [Bk[4][1]], [R_gh])
        Wk = {}
        Wk["sq"] = (S.sb([128, 512], BF16, "sq"), Res())
        Wk["sm"] = Bk[1]
        Wk["r"] = (S.sb([128, 512], F32, "r"), Res())
        Wk["xnf"] = (S.sb([128, 512], F32, "xnf"), Res())
        Wk["xnb"] = (S.sb([128, 512], BF16, "xnb"), Res())
        Wk["rp"] = Bk[2]
        Wk["t1"] = (S.sb([128, 512], F32, "t1"), Res())
        Wk2 = {"sq": (S.sb([128, 512], BF16, "sq2"), Res()), "sm": sps[0], "r": (S.sb([128, 512], F32, "r2"), Res()),
               "xnf": (S.sb([128, 512], F32, "xnf2"), Res()), "xnb": (S.sb([128, 512], BF16, "xnb2"), Res()), "rp": sps[1],
               "t1": (S.sb([128, 512], F32, "t12"), Res())}
        cs = [(S.sb([128, 512], F32, "cos"), S.sb([128, 512], F32, "sin"), Res()) for _ in range(2)]
        QT = (S.sb([128, NTOK], BF16, "QT"), Res())
        KT = (S.sb([128, NTOK], BF16, "KT"), Res())
        Vh = (S.sb([128, 34, 128], BF16, "Vh"), Res())
        NB, GRP = 4, 2
        scb = sps + [Bk[2], Bk[3]]
        pts = [(S.sb([128, 512], BF16, "pt"), Res()) for _ in range(NB)]
        dsum = [(S.sb([128, 512], F32, "dsum"), Res()) for _ in range(2)]
        onesF = S.sb([128, 128], F32, "onesF")
        R_onesF = Res()
        P.op("pool", lambda e: e.memset(onesF[:], 1.0), [], [R_onesF])
        rr = [(S.sb([128, 512], F32, "rr"), Res()) for _ in range(2)]
        uu = [(S.sb([128, 512], F32, "uu"), Res()) for _ in range(2)]
        fin = [(S.sb([128, 512], BF16, "fin"), Res()) for _ in range(2)]
        ncs = [0]
        scale = 64.0 ** -0.5

        wqkv = [(S.sb([128, 8, 128], BF16, "wqkv"), Res()) for _ in range(3)]

        def do_head(h):
            wq, R_wq = self.load_cast(S, win[:, :, h * 128:(h + 1) * 128], [128, 8, 128], stage, R_stage, into=wqkv[0])
            wk, R_wk = self.load_cast(S, win[:, :, D + h * 128:D + (h + 1) * 128], [128, 8, 128], stage, R_stage, into=wqkv[1])
            wv, R_wv = self.load_cast(S, win[:, :, 2 * D + h * 128:2 * D + (h + 1) * 128], [128, 8, 128], stage, R_stage, into=wqkv[2])
            qt, R_qt = QT
            kt, R_kt = KT
            vh, R_vh = Vh
            gp, R_gp = Bk[0]
            for bi, (t0, T) in enumerate(blocks):
                cos_t, sin_t, R_tab = cs[ncs[0] % 2]
                ncs[0] += 1
                P.dma("sp", cos_t[:, 0:T], self.c_cosB[:, t0:t0 + T], writes=[R_tab])
                P.dma("sp", sin_t[:, 0:T], self.c_sinB[:, t0:t0 + T], writes=[R_tab])
                for (wt, R_wt, gcol, dst, R_dst) in ((wk, R_wk, gk, kt, R_kt), (wq, R_wq, gq, qt, R_qt)):
                    for k in range(8):
                        P.op("pe", lambda e, k=k, wt=wt, t0=t0, T=T: e.matmul(gp[:, 0:T], lhsT=wt[:, k, :], rhs=hT[:, k, t0:t0 + T],
                                                                               start=(k == 0), stop=(k == 7)), [R_wt, R_hT[bi]], [R_gp])
                    self.normrope(Wk, gp[:, 0:T], R_gp, 128, T, bd[:, :], 1.0 / 64, gcol[:, 0:1], R_gh,
                                  rm[:, :], R_rm, cos_t[:, 0:T], sin_t[:, 0:T], R_tab, dst[:, t0:t0 + T], R_dst)
                nt = T // 128
                for ti in range(nt):
                    kc = t0 // 128 + ti
                    for k in range(8):
                        P.op("pe", lambda e, k=k, kc=kc, ti=ti: e.matmul(gp[:, ti * 128:(ti + 1) * 128], lhsT=hT[:, k, kc * 128:(kc + 1) * 128],
                                                                          rhs=wv[:, k, :], start=(k == 0), stop=(k == 7)),
                             [R_wv, R_hT[bi]], [R_gp])
                P.op("act", lambda e, t0=t0, nt=nt: e.activation(
                    out=vh[:, t0 // 128:t0 // 128 + nt, :], in_=gp[:, 0:nt * 128].rearrange("p (t c) -> p t c", t=nt),
                    func=AF.Copy), [R_gp], [R_vh])
            for qi, (q0, T, kcs) in enumerate(self.qblocks(need_ctx_out)):
                n = len(kcs)
                seq = [(i, c) for i in range(n) for c in range(2)]

                def score(s_i):
                    i, c = seq[s_i]
                    sp_, R_sp = scb[s_i % NB]
                    kc = kcs[i]
                    P.op("pe", lambda e, sp_=sp_, kc=kc, c=c, T=T, q0=q0: e.matmul(
                        sp_[:, 0:T], lhsT=kt[c * 64:(c + 1) * 64, kc * 128:(kc + 1) * 128], rhs=qt[c * 64:(c + 1) * 64, q0:q0 + T],
                        start=True, stop=True), [R_kt, R_qt], [R_sp])
                    pt_, R_pt_ = pts[s_i % NB]
                    P.op("act", lambda e, sp_=sp_, pt_=pt_, T=T: e.activation(out=pt_[:, 0:T], in_=sp_[:, 0:T], func=AF.Exp, scale=scale),
                         [R_sp], [R_pt_])

                def pv(s_i):
                    i, c = seq[s_i]
                    pt_, R_pt_ = pts[s_i % NB]
                    kc = kcs[i]
                    ao, R_ao = (Bk[3] if (c == 0 and qi % 2 == 1) else Bk[c])
                    ds_, R_ds = dsum[c]
                    P.op("pe", lambda e, pt_=pt_, kc=kc, i=i, T=T, n=n, ao=ao: e.matmul(
                        ao[:, 0:T], lhsT=vh[:, kc, :], rhs=pt_[:, 0:T], start=(i == 0), stop=(i == n - 1)), [R_vh, R_pt_], [R_ao])
                    eng = "dve" if c == 0 else "pool"
                    if i == 0:
                        P.op(eng, lambda e, pt_=pt_, ds_=ds_, T=T: e.tensor_copy(out=ds_[:, 0:T], in_=pt_[:, 0:T]), [R_pt_], [R_ds])
                    else:
                        P.op(eng, lambda e, pt_=pt_, ds_=ds_, T=T: e.tensor_tensor(out=ds_[:, 0:T], in0=ds_[:, 0:T], in1=pt_[:, 0:T], op=ALU.add),
                             [R_pt_, R_ds], [R_ds])
                ns = len(seq)
                groups = [list(range(a_, min(a_ + GRP, ns))) for a_ in range(0, ns, GRP)]
                for s_i in groups[0]:
                    score(s_i)
                for gi, grp in enumerate(groups):
                    if gi + 1 < len(groups):
                        for s_i in groups[gi + 1]:
                            score(s_i)
                    for s_i in reversed(grp):
                        pv(s_i)
                sm_, R_sm = Bk[4]
                for c in (1, 0):
                    r_, R_r = rr[c]
                    u_, R_u = uu[c]
                    ao, R_ao = (Bk[3] if (c == 0 and qi % 2 == 1) else Bk[c])
                    ds_, R_ds = dsum[c]
                    P.op("pe", lambda e, ds_=ds_, T=T: e.matmul(sm_[:, 0:T], lhsT=onesF[:, :], rhs=ds_[:, 0:T], start=True, stop=True),
                         [R_ds, R_onesF], [R_sm])
                    P.op("dve", lambda e, r_=r_, T=T: e.reciprocal(out=r_[:, 0:T], in_=sm_[:, 0:T]), [R_sm], [R_r])
                    P.op("dve", lambda e, u_=u_, ao=ao, r_=r_, T=T: e.tensor_tensor(out=u_[:, 0:T], in0=ao[:, 0:T], in1=r_[:, 0:T], op=ALU.mult),
                         [R_ao, R_r], [R_u])
                u0, R_u0 = uu[0]
                u1, R_u1 = uu[1]
                P.op("dve", lambda e, u0=u0, u1=u1, T=T: e.scalar_tensor_tensor(out=u0[:, 0:T], in0=u1[:, 0:T], scalar=neglam[:, 0:1],
                                                                              in1=u0[:, 0:T], op0=ALU.mult, op1=ALU.add),
                     [R_u0, R_u1, R_gh], [R_u0])
                sq_, R_sq = Wk["sq"]
                sm_, R_sm = Bk[4]
                r_, R_r = rr[0]
                f_, R_f = fin[qi % 2]
                P.op("act", lambda e, u0=u0, sq_=sq_, T=T: e.activation(out=sq_[:, 0:T], in_=u0[:, 0:T], func=AF.Square), [R_u0], [R_sq])
                P.op("pe", lambda e, sq_=sq_, sm_=sm_, T=T: e.matmul(sm_[:, 0:T], lhsT=self.ones_b[:, :], rhs=sq_[:, 0:T], start=True, stop=True),
                     [R_sq, self.R_const], [R_sm])
                P.op("act", lambda e, r_=r_, sm_=sm_, T=T: e.activation(out=r_[:, 0:T], in_=sm_[:, 0:T], func=AF.Sqrt,
                                                                        bias=self.eps_t[:, 0:1], scale=1.0 / 128), [R_sm, self.R_const], [R_r])
                P.op("dve", lambda e, r_=r_, T=T: e.reciprocal(out=r_[:, 0:T], in_=r_[:, 0:T]), [R_r], [R_r])
                P.op("dve", lambda e, u0=u0, r_=r_, f_=f_, T=T: e.scalar_tensor_tensor(out=f_[:, 0:T], in0=u0[:, 0:T], scalar=gs[:, 0:1],
                                                                                    in1=r_[:, 0:T], op0=ALU.mult, op1=ALU.mult),
                     [R_u0, R_r, R_gh], [R_f])
                P.dma("sp", self.od[h * 128:(h + 1) * 128, q0:q0 + T], f_[:, 0:T], reads=[R_f], writes=[self.R_od])

        for h in range(int(_os_environ_get("DBG_NHEAD", 8))):
            do_head(h)
        S.close()
        L.close()
        self.phase_outproj(layer, "diff_w_out", j, need_ctx_out)
        return []

    def gelu_tile(self, G, z, R_z, out_ap, R_out, shape):
        P = self.P
        i = G["i"]
        G["i"] += 1
        s2, R_s2 = G["s2"][i % 2]
        w, R_w = G["w"][i % 2]
        sl = tuple(slice(0, d_) for d_ in shape)
        P.op("act", lambda e: e.activation(out=s2[sl], in_=z, func=AF.Square), [R_z], [R_s2])
        P.op("pool", lambda e: e.tensor_scalar(out=w[sl], in0=s2[sl], scalar1=0.044715, scalar2=1.0, op0=ALU.mult, op1=ALU.add),
             [R_s2], [R_w])
        P.op("dve", lambda e: e.tensor_tensor(out=w[sl], in0=w[sl], in1=z, op=ALU.mult), [R_w, R_z], [R_w])
        P.op("act", lambda e: e.activation(out=s2[sl], in_=w[sl], func=AF.Sigmoid, scale=1.5957691216057308), [R_w, R_s2], [R_s2])
        P.op("dve", lambda e: e.tensor_tensor(out=out_ap, in0=s2[sl], in1=z, op=ALU.mult), [R_s2, R_z], [R_out])

    def phase_chunk(self, layer):
        nc, P = self.nc, self.P
        j = layer // 3
        need_ctx_out = layer != DEPTH - 1
        gts_all = list(range(0 if need_ctx_out else 2, 34))
        blocks = ([(0, 256)] if need_ctx_out else []) + [(256 + 512 * b, 512) for b in range(8)]
        S = Scope(self)
        A_l, RA_l = self.load_bc(S, layer, 0, 1)
        B_l, RB_l = self.load_bc(S, layer, 0, 0, q="act")
        if need_ctx_out:
            A_c, RA_c = self.load_bc(S, layer, 1, 1)
            B_c, RB_c = self.load_bc(S, layer, 1, 0, q="act")
        T_ = self.prenorm_tiles(S, npt=2)
        stage = S.sb([128, 8 * 512], F32, "stg")
        R_stage = Res()
        win = S.sb([128, 8, 4096], BF16, "win")
        R_win = Res()
        wv_ = self.W["sg_w_in"][j].rearrange("(k p) n -> p k n", p=128)
        for c0 in range(0, 4096, 512):
            self.load_cast(S, wv_[:, :, c0:c0 + 512], [128, 8, 512], stage, R_stage, eng=("act" if (c0 // 512) % 2 else "dve"),
                           into=(win[:, :, c0:c0 + 512], R_win))
        wsn = stage[:, 0:1024].rearrange("p (g q) -> p g q", g=8)
        P.dma("sp", wsn, self.W["sg_w_s"][j].rearrange("g p q -> p g q"), writes=[R_stage])
        wsT = S.sb([128, 8, 128], BF16, "wsT")
        R_ws = Res()
        pt0, R_pt0 = T_["pt"][0]
        for g in range(8):
            P.op("pe", lambda e, g=g: e.transpose(out=pt0[:, g, :], in_=wsn[:, g, :], identity=self.ident_f[:]),
                 [R_stage, self.R_const], [R_pt0])
        P.op("dve", lambda e: e.tensor_copy(out=wsT[:], in_=pt0[:]), [R_pt0], [R_ws])
        bsb = S.sb([128, 8, 2, 128], F32, "bsb")
        R_bsb = Res()
        bsrc = self.W["sg_b_s"][j:j + 1].rearrange("o g p -> o (g p)").partition_broadcast(128).rearrange("q o (g p) -> q (o g) p", g=8)
        for r_ in range(2):
            P.dma("act", bsb[:, :, r_, :], bsrc, writes=[R_bsb])
        bsb16 = bsb[:, :, :, :].rearrange("q g r p -> q (g r) p")
        lng = S.sb([128, 2048], F32, "lng")
        lnb = S.sb([128, 2048], F32, "lnb")
        R_ln = Res()
        P.dma("act", lng[:], self.W["sg_ln_g"][j:j + 1, :].partition_broadcast(128), writes=[R_ln])
        P.dma("act", lnb[:], self.W["sg_ln_b"][j:j + 1, :].partition_broadcast(128), writes=[R_ln])
        hTb = (S.sb([128, 8, 512], BF16, "hTb"), Res())
        uT = (S.sb([128, 16, 512], BF16, "uT"), Res())
        vz = (S.sb([128, 2048], F32, "vz"), Res())
        vln = (S.sb([128, 2048], BF16, "vln"), Res())
        G = {"i": 0, "s2": [(S.sb([128, 512], F32, "gs2"), Res()) for _ in range(2)],
             "w": [(S.sb([128, 512], F32, "gw"), Res()) for _ in range(2)]}
        zps = [(S.ps([128, 512], F32, "zp"), Res()) for _ in range(2)]
        spp = [(S.ps([128, 4, 128], F32, "spp"), Res()) for _ in range(2)]
        tt = (S.sb([128, 4, 128], F32, "tt"), Res())
        prodT = [(S.sb([128, 16, 128], BF16, "prodT"), Res()) for _ in range(2)]
        st2 = (S.sb([128, 8], F32, "st2"), Res())
        nz = [0]
        pdv = self.pd.rearrange("(k p) t -> p k t", p=128)
        D4 = Defer()
        for bi, (t0, T) in enumerate(blocks):
            hb_, R_hb = hTb
            nt = T // 128
            for ti in range(nt):
                gt = t0 // 128 + ti
                if gt < 2:
                    hn, R_hn, pt, R_pt = self.prenorm_tile(T_, gt, A_c, RA_c, B_c, RB_c)
                else:
                    hn, R_hn, pt, R_pt = self.prenorm_tile(T_, gt, A_l, RA_l, B_l, RB_l)
                D4.push(lambda pt=pt, ti=ti, R_pt=R_pt: P.op(
                    "dve", lambda e: e.tensor_copy(out=hb_[:, :, ti * 128:(ti + 1) * 128], in_=pt[:]), [R_pt], [R_hb]))
            D4.flush()
            ut, R_ut = uT
            for cc in range(16):
                zp, R_zp = zps[nz[0] % 2]
                nz[0] += 1
                for k in range(8):
                    P.op("pe", lambda e, zp=zp, k=k, cc=cc, T=T: e.matmul(zp[:, 0:T], lhsT=win[:, k, cc * 128:(cc + 1) * 128],
                                                                           rhs=hb_[:, k, 0:T], start=(k == 0), stop=(k == 7)),
                         [R_win, R_hb], [R_zp])
                self.gelu_tile(G, zp[:, 0:T], R_zp, ut[:, cc, 0:T], R_ut, (128, T))
            for ti in range(nt):
                gt = t0 // 128 + ti
                vzt, R_vz = vz
                for nb in range(4):
                    zp, R_zp = zps[nz[0] % 2]
                    nz[0] += 1
                    for k in range(8):
                        P.op("pe", lambda e, zp=zp, k=k, nb=nb, ti=ti: e.matmul(
                            zp[:, :], lhsT=hb_[:, k, ti * 128:(ti + 1) * 128], rhs=win[:, k, 2048 + nb * 512:2048 + (nb + 1) * 512],
                            start=(k == 0), stop=(k == 7)), [R_win, R_hb], [R_zp])
                    self.gelu_tile(G, zp[:, :], R_zp, vzt[:, nb * 512:(nb + 1) * 512], R_vz, (128, 512))
                s_, R_s = st2
                junk, R_junk = T_["junk"]
                P.op("dve", lambda e: e.tensor_reduce(out=s_[:, 0:1], in_=vzt[:, :], axis=mybir.AxisListType.X, op=ALU.add), [R_vz], [R_s])
                for hf in range(2):
                    P.op("act", lambda e, hf=hf: e.activation(out=junk[:, :], in_=vzt[:, hf * 1024:(hf + 1) * 1024], func=AF.Square,
                                                                accum_out=s_[:, 1 + hf:2 + hf]), [R_vz], [R_junk, R_s])
                P.op("dve", lambda e: e.tensor_scalar(out=s_[:, 0:1], in0=s_[:, 0:1], scalar1=1.0 / 2048, scalar2=None, op0=ALU.mult), [R_s], [R_s])
                P.op("dve", lambda e: e.tensor_tensor(out=s_[:, 1:2], in0=s_[:, 1:2], in1=s_[:, 2:3], op=ALU.add), [R_s], [R_s])
                P.op("dve", lambda e: e.tensor_tensor(out=s_[:, 3:4], in0=s_[:, 0:1], in1=s_[:, 0:1], op=ALU.mult), [R_s], [R_s])
                P.op("dve", lambda e: e.scalar_tensor_tensor(out=s_[:, 4:5], in0=s_[:, 1:2], scalar=1.0 / 2048, in1=s_[:, 3:4],
                                                             op0=ALU.mult, op1=ALU.subtract), [R_s], [R_s])
                P.op("act", lambda e: e.activation(out=s_[:, 5:6], in_=s_[:, 4:5], func=AF.Sqrt, bias=self.eps_t[:, 0:1], scale=1.0),
                     [R_s, self.R_const], [R_s])
                P.op("dve", lambda e: e.reciprocal(out=s_[:, 5:6], in_=s_[:, 5:6]), [R_s], [R_s])
                P.op("dve", lambda e: e.scalar_tensor_tensor(out=s_[:, 6:7], in0=s_[:, 0:1], scalar=-1.0, in1=s_[:, 5:6],
                                                             op0=ALU.mult, op1=ALU.mult), [R_s], [R_s])
                P.op("act", lambda e: e.activation(out=vzt[:, :], in_=vzt[:, :], func=AF.Identity, bias=s_[:, 6:7], scale=s_[:, 5:6]),
                     [R_vz, R_s], [R_vz])
                P.op("dve", lambda e: e.tensor_tensor(out=vzt[:, :], in0=vzt[:, :], in1=lng[:, :], op=ALU.mult), [R_vz, R_ln], [R_vz])
                vl, R_vl = vln
                P.op("pool", lambda e: e.tensor_tensor(out=vl[:, :], in0=vzt[:, :], in1=lnb[:, :], op=ALU.add), [R_vz, R_ln], [R_vl])
                pr, R_pr = prodT[gt % 2]
                for m in range(4):
                    sp_, R_sp = spp[m % 2]
                    for q_ in range(4):
                        cc = 4 * m + q_
                        P.op("pe", lambda e, sp_=sp_, q_=q_, cc=cc: e.matmul(sp_[:, q_, :], lhsT=vl[:, cc * 128:(cc + 1) * 128],
                                                                             rhs=wsT[:, cc // 2, :], start=True, stop=True),
                             [R_vl, R_ws], [R_sp])
                    t_, R_t = tt
                    P.op("dve", lambda e, sp_=sp_, m=m: e.tensor_tensor(out=t_[:, :, :], in0=sp_[:, :, :], in1=bsb16[:, 4 * m:4 * m + 4, :], op=ALU.add),
                         [R_sp, R_bsb], [R_t])
                    P.op("pool", lambda e, m=m, ti=ti, pr=pr: e.tensor_tensor(out=pr[:, 4 * m:4 * m + 4, :], in0=t_[:, :, :],
                                                                                in1=ut[:, 4 * m:4 * m + 4, ti * 128:(ti + 1) * 128], op=ALU.mult),
                         [R_t, R_ut], [R_pr])
                P.dma("sp", pdv[:, :, gt * 128:(gt + 1) * 128], pr[:, :, :], reads=[R_pr], writes=[self.R_od])
        S.close()
        self.phase_outproj(layer, "sg_w_out", j, need_ctx_out, src=self.pd, nk=16)
        return []

    def phase_mix(self, layer):
        kind = layer % 3
        if kind == 0:
            return self.phase_mla(layer)
        if kind == 1:
            return self.phase_diff(layer)
        return self.phase_chunk(layer)


_CACHE = {}


def _get_nc(plan):
    key = tuple(plan)
    if key not in _CACHE:
        K = Kern(plan)
        _CACHE[key] = K.build()
    return _CACHE[key]


FULL_PLAN = [(k, l) for l in range(DEPTH) for k in ("mix", "moe")]


def _axial(rot_dim):
    n_rows = NLAT // 64
    rows = np.repeat(np.arange(n_rows, dtype=np.float32), 64)
    cols = np.tile(np.arange(64, dtype=np.float32), n_rows)
    n_freq = rot_dim // 4
    inv_freq = (np.float32(10000.0) ** (-np.arange(n_freq, dtype=np.float32) / np.float32(n_freq))).astype(np.float32)
    ang = np.concatenate([rows[:, None] * inv_freq, cols[:, None] * inv_freq], axis=-1).astype(np.float32)
    return np.cos(ang).astype(np.float32), np.sin(ang).astype(np.float32)


def _host_consts():
    c = {}
    cosa, sina = _axial(32)
    cosA = np.ones((96, NTOK), np.float32)
    sinA = np.zeros((96, NTOK), np.float32)
    cosA[64:80, NCTX:] = cosa.T
    cosA[80:96, NCTX:] = cosa.T
    sinA[64:80, NCTX:] = sina.T
    sinA[80:96, NCTX:] = sina.T
    c["c_cosA"], c["c_sinA"] = cosA, sinA
    rm = np.zeros((96, 96), np.float32)
    for m in range(64, 80):
        rm[m + 16, m] = -1.0
    for m in range(80, 96):
        rm[m - 16, m] = 1.0
    c["c_rm96"] = rm
    cosb, sinb = _axial(64)
    cosB = np.ones((128, NTOK), np.float32)
    sinB = np.zeros((128, NTOK), np.float32)
    rm2 = np.zeros((128, 128), np.float32)
    for blk in range(4):
        cosB[blk * 32:(blk + 1) * 32, NCTX:] = cosb.T
        sinB[blk * 32:(blk + 1) * 32, NCTX:] = sinb.T
    for c0 in (0, 64):
        for m in range(32):
            rm2[c0 + m + 32, c0 + m] = -1.0
            rm2[c0 + m, c0 + m + 32] = 1.0
    c["c_cosB"], c["c_sinB"], c["c_rm128"] = cosB, sinB, rm2
    return c


def make_in_maps(inputs, ncores=8):
    ident = np.eye(128, dtype=np.float32)
    shared = {n: np.ascontiguousarray(np.asarray(inputs[n], dtype=np.float32)) for n, _ in WEIGHT_SPECS}
    shared["c_ident"] = ident
    shared.update(_host_consts())
    maps = []
    c_ctx = np.asarray(inputs["c_ctx"], dtype=np.float32)
    for b in range(ncores):
        c = np.asarray(inputs["c"][b], dtype=np.float32)
        cc = np.stack([c, c_ctx], axis=-1).reshape(8, 128, 2).transpose(1, 0, 2)
        m = dict(shared)
        m["xin"] = np.ascontiguousarray(np.asarray(inputs["x"][b], dtype=np.float32))
        m["cin"] = np.ascontiguousarray(np.asarray(inputs["ctx"][b], dtype=np.float32))
        m["cc"] = np.ascontiguousarray(cc)
        maps.append(m)
    return maps


def kernel(**inputs):
    nc = _get_nc(FULL_PLAN)
    maps = make_in_maps(inputs, 8)
    res = run_bass_kernel_spmd(nc, maps, core_ids=list(range(8)))
    return np.stack([np.asarray(r["xl"], dtype=np.float32) for r in res.results], axis=0)
```

```python
import math
import os


def _os_environ_get(k, d):
    return os.environ.get(k, d)

from contextlib import ExitStack

import numpy as np
import concourse.bass as bass
import concourse.mybir as mybir
from concourse.bass_utils import run_bass_kernel_spmd

F32 = mybir.dt.float32
BF16 = mybir.dt.bfloat16
I32 = mybir.dt.int32
U32 = mybir.dt.uint32
ALU = mybir.AluOpType
AF = mybir.ActivationFunctionType

ENGS = ("pe", "act", "dve", "pool", "sp")
DMA_Q = ("sp", "act", "pool")

D = 1024
NCTX = 256
NLAT = 4096
NTOK = NCTX + NLAT
DEPTH = 4
EPS = 1e-6
NEXP = 16
CAP_L = 512
CAP_C = 32


class Res:
    __slots__ = ("lw", "rd")

    def __init__(self):
        self.lw = None
        self.rd = []


class Op:
    __slots__ = ("eng", "fn", "deps", "signal", "sigval", "is_dma", "dsem", "dval", "phase")

    def __init__(self, eng, fn, is_dma, phase):
        self.eng = eng
        self.fn = fn
        self.deps = []
        self.signal = False
        self.sigval = None
        self.is_dma = is_dma
        self.dsem = None
        self.dval = None
        self.phase = phase


class Prog:
    def __init__(self, nc, n_dma_sems=12):
        self.nc = nc
        self.ops = {e: [] for e in ENGS}
        self.nphase = 0
        self.n_dma_sems = n_dma_sems
        self.max_ops = 3000
        self.inline_wait = os.environ.get("NO_INLINE_WAIT") is None
        self.sems = {}
        self.dma_sems = {}

    def open(self, stack):
        nc = self.nc
        for e in ENGS:
            self.sems[e] = stack.enter_context(nc.semaphore("s_" + e))
        for q in DMA_Q:
            self.dma_sems[q] = [stack.enter_context(nc.semaphore("d_%s%d" % (q, i)))
                                for i in range(self.n_dma_sems)]
        self.sem_cnt = {e: 0 for e in ENGS}
        self.dma_cnt = {q: [0] * self.n_dma_sems for q in DMA_Q}
        self.dma_rr = {q: 0 for q in DMA_Q}
        self.waited = {}

    def op(self, eng, fn, reads=(), writes=(), dma=False):
        if max(len(v) for v in self.ops.values()) >= self.max_ops:
            self.flush()
        o = Op(eng, fn, dma, self.nphase)
        deps = []
        for r in reads:
            if r.lw is not None:
                deps.append(r.lw)
        for w in writes:
            if w.lw is not None:
                deps.append(w.lw)
            deps.extend(w.rd)
        seen = set()
        for d in deps:
            if id(d) in seen or d is o:
                continue
            seen.add(id(d))
            if (not d.is_dma) and d.phase != self.nphase:
                continue
            if (not d.is_dma) and (not dma) and d.eng == eng and eng == "pe":
                continue
            o.deps.append(d)
            d.signal = True
        for r in reads:
            r.rd.append(o)
        for w in writes:
            w.lw = o
            w.rd = []
        self.ops[eng].append(o)
        return o

    def dma(self, q, out, in_, reads=(), writes=(), **kw):
        return self.op(q, lambda e: e.dma_start(out=out, in_=in_, **kw), reads, writes, dma=True)

    def flush(self, final_wait=()):
        nc = self.nc
        ops = self.ops
        for e in ENGS:
            for o in ops[e]:
                if o.is_dma:
                    k = self.dma_rr[e]
                    self.dma_rr[e] = (k + 1) % self.n_dma_sems
                    self.dma_cnt[e][k] += 16
                    o.dsem = (e, k)
                    o.dval = self.dma_cnt[e][k]
                elif o.signal:
                    self.sem_cnt[e] += 1
                    o.sigval = self.sem_cnt[e]

        def emit_engine(e, engobj):
            waited = self.waited
            for o in ops[e]:
                need = {}
                for d in o.deps:
                    if d.is_dma:
                        key = ("d",) + d.dsem
                        val = d.dval
                    else:
                        key = ("c", d.eng)
                        val = d.sigval
                    if val > need.get(key, 0):
                        need[key] = val
                if o.is_dma:
                    key = ("d",) + o.dsem
                    if o.dval - 16 > need.get(key, 0):
                        need[key] = o.dval - 16
                todo = []
                for key, val in need.items():
                    wk = (e, key)
                    if waited.get(wk, 0) >= val:
                        continue
                    waited[wk] = val
                    sem = self.dma_sems[key[1]][key[2]] if key[0] == "d" else self.sems[key[1]]
                    todo.append((sem, val))
                inline = None
                if todo and self.inline_wait and not o.is_dma:
                    inline = todo.pop()
                for sem, val in todo:
                    engobj.wait_ge(sem, val)
                ins = o.fn(engobj)
                if inline is not None:
                    ins._wait_ge(inline[0], inline[1])
                if o.is_dma:
                    ins.then_inc(self.dma_sems[o.dsem[0]][o.dsem[1]], 16)
                elif o.signal:
                    ins.then_inc(self.sems[e], 1)
            if e == "sp":
                for o in final_wait:
                    engobj.wait_ge(self.dma_sems[o.dsem[0]][o.dsem[1]], o.dval)

        with nc.Block() as block:
            if ops["sp"] or final_wait:
                block.sync(lambda eng: emit_engine("sp", eng))
            if ops["act"]:
                block.scalar(lambda eng: emit_engine("act", eng))
            if ops["dve"]:
                block.vector(lambda eng: emit_engine("dve", eng))
            if ops["pool"]:
                block.gpsimd(lambda eng: emit_engine("pool", eng))
            if ops["pe"]:
                block.tensor(lambda eng: emit_engine("pe", eng))
        used = [e for e in ENGS if self.sem_cnt[e] > 0]
        if used and not final_wait:
            with nc.Block() as block:
                reg = {"sp": block.sync, "act": block.scalar, "dve": block.vector, "pool": block.gpsimd, "pe": block.tensor}
                for e in used:
                    reg[e](lambda eng, e=e: eng.sem_clear(self.sems[e]))
            for e in used:
                self.sem_cnt[e] = 0
            for wk in [wk for wk in self.waited if wk[1][0] == "c"]:
                del self.waited[wk]
        self.ops = {e: [] for e in ENGS}
        self.nphase += 1


class Defer:
    def __init__(self):
        self.p = None

    def push(self, fn):
        if self.p is not None:
            self.p()
        self.p = fn

    def flush(self):
        if self.p is not None:
            self.p()
        self.p = None


class Scope:
    def __init__(self, K):
        self.K = K
        self.st = ExitStack()
        self.n = 0

    def sb(self, shape, dt, name=None):
        self.n += 1
        nm = "%s_p%d_%d" % (name or "t", self.K.P.nphase, self.n)
        return self.st.enter_context(self.K.nc.sbuf_tensor(nm, list(shape), dt))

    def ps(self, shape, dt, name=None):
        self.n += 1
        nm = "%s_q%d_%d" % (name or "p", self.K.P.nphase, self.n)
        return self.st.enter_context(self.K.nc.psum_tensor(nm, list(shape), dt))

    def close(self, final_wait=()):
        self.K.P.flush(final_wait=final_wait)
        self.st.close()


WEIGHT_SPECS = [
    ("ada_w", [DEPTH, D, 6 * D]), ("ada_b", [DEPTH, 6 * D]),
    ("norm_mix_g", [DEPTH, D]), ("norm_ffn_g", [DEPTH, D]),
    ("mla_w_in", [2, D, 800]), ("mla_q_norm_g", [2, 512]), ("mla_w_uq", [2, 512, 1536]),
    ("mla_kv_norm_g", [2, 256]), ("mla_w_ukv", [2, 256, 2048]), ("mla_qn_g", [2, 96]),
    ("mla_kn_g", [2, 96]), ("mla_w_out", [2, D, D]),
    ("diff_w_in", [1, D, 3 * D]), ("diff_qn_g", [1, 64]), ("diff_kn_g", [1, 64]),
    ("diff_lambda_q1", [1, 64]), ("diff_lambda_k1", [1, 64]), ("diff_lambda_q2", [1, 64]),
    ("diff_lambda_k2", [1, 64]), ("diff_sub_g", [1, 128]), ("diff_w_out", [1, D, D]),
    ("sg_w_in", [1, D, 4096]), ("sg_ln_g", [1, 2048]), ("sg_ln_b", [1, 2048]),
    ("sg_w_s", [1, 8, 128, 128]), ("sg_b_s", [1, 8, 128]), ("sg_w_out", [1, 2048, D]),
    ("moe_router", [DEPTH, D, NEXP]), ("moe_w_gate", [DEPTH, NEXP, D, D]),
    ("moe_w_up", [DEPTH, NEXP, D, D]), ("moe_w_down", [DEPTH, NEXP, D, D]),
]


class Kern:
    def __init__(self, plan, debug=False, debug_layer=0):
        self.plan = plan
        self.debug = debug
        self.debug_layer = debug_layer
        self.nc = bass.Bass("TRN2", target_bir_lowering=False)
        self.P = Prog(self.nc)

    def xrows(self, gt, n=1):
        if gt < 2:
            return self.xc[gt * 128:(gt + n) * 128, :]
        return self.xl[(gt - 2) * 128:(gt - 2 + n) * 128, :]

    def hrows(self, gt, n=1):
        if gt < 2:
            return self.hc[gt * 128:(gt + n) * 128, :]
        return self.hl[(gt - 2) * 128:(gt - 2 + n) * 128, :]

    def xres(self, gt):
        return self.R_x[gt]

    def load_bc(self, S, layer, stream, slot, q="sp"):
        t = S.sb([128, D], F32, "bc")
        r = Res()
        src = self.modd[layer, stream, slot:slot + 1, :].partition_broadcast(128)
        self.P.dma(q, t[:], src, reads=[self.R_modd], writes=[r])
        return t, r

    def build(self):
        nc, P = self.nc, self.P
        dt_in = lambda name, shape, dt=F32: nc.dram_tensor(name, list(shape), dt, kind="ExternalInput").ap()
        self.xin = dt_in("xin", [NLAT, D])
        self.cin = dt_in("cin", [NCTX, D])
        self.cc = dt_in("cc", [128, 8, 2])
        self.W = {n: dt_in(n, s) for n, s in WEIGHT_SPECS}
        self.c_ident = dt_in("c_ident", [128, 128])
        self.c_rm96 = dt_in("c_rm96", [96, 96])
        self.c_rm128 = dt_in("c_rm128", [128, 128])
        self.c_cosB = dt_in("c_cosB", [128, NTOK])
        self.c_sinB = dt_in("c_sinB", [128, NTOK])
        self.c_cosA = dt_in("c_cosA", [96, NTOK])
        self.c_sinA = dt_in("c_sinA", [96, NTOK])
        self.xl = nc.dram_tensor("xl", [NLAT, D], F32, kind="ExternalOutput").ap()
        if self.debug:
            self.xc = nc.dram_tensor("xc", [NCTX, D], F32, kind="ExternalOutput").ap()
        else:
            self.xc = nc.dram_tensor("xc", [NCTX, D], F32).ap()
        self.hl = nc.dram_tensor("hl", [NLAT, D], BF16).ap()
        self.hc = nc.dram_tensor("hc", [NCTX, D], BF16).ap()
        self.modd = nc.dram_tensor("modd", [DEPTH, 2, 6, D], F32).ap()
        if self.debug:
            self.od = nc.dram_tensor("od", [D, NTOK], BF16, kind="ExternalOutput").ap()
            self.dbg_qt = nc.dram_tensor("dbg_qt", [128, NTOK], BF16, kind="ExternalOutput").ap()
            self.dbg_kt = nc.dram_tensor("dbg_kt", [128, NTOK], BF16, kind="ExternalOutput").ap()
            self.dbg_va = nc.dram_tensor("dbg_va", [128, 34 * 128], BF16, kind="ExternalOutput").ap()
        else:
            self.od = nc.dram_tensor("od", [D, NTOK], BF16).ap()
        self.R_x = [Res() for _ in range(34)]
        self.R_h = [Res() for _ in range(34)]
        self.R_modd = Res()
        self.R_od = Res()
        self.pd = nc.dram_tensor("pd", [2048, NTOK], BF16).ap()
        self.R_xl_all = Res()
        self.R_xc_all = Res()
        self.fence_d = nc.dram_tensor("fence_d", [128, 4], F32).ap()
        if self.debug:
            self.dbg_xe = nc.dram_tensor("dbg_xe", [128, 8 * 544], BF16, kind="ExternalOutput").ap()
            self.dbg_hid = nc.dram_tensor("dbg_hid", [128, 8 * 544], BF16, kind="ExternalOutput").ap()
            self.dbg_sg = nc.dram_tensor("dbg_sg", [128, 8 * 544], F32, kind="ExternalOutput").ap()
            self.dbg_xe2 = nc.dram_tensor("dbg_xe2", [128, 8 * 544], BF16, kind="ExternalOutput").ap()
            self.dbg_wg = nc.dram_tensor("dbg_wg", [128, 8 * 1024], BF16, kind="ExternalOutput").ap()
            self.dbg_aff = nc.dram_tensor("dbg_aff", [NEXP, NLAT], F32, kind="ExternalOutput").ap()
            self.dbg_idx = nc.dram_tensor("dbg_idx", [128, 4 * NEXP], I32, kind="ExternalOutput").ap()
            self.dbg_gate = nc.dram_tensor("dbg_gate", [128, 4 * NEXP], F32, kind="ExternalOutput").ap()

        with ExitStack() as top:
            P.open(top)
            self.top = top
            self.ident_f = top.enter_context(nc.sbuf_tensor("ident_f", [128, 128], F32))
            self.ident_b = top.enter_context(nc.sbuf_tensor("ident_b", [128, 128], BF16))
            self.ones_b = top.enter_context(nc.sbuf_tensor("ones_b", [128, 128], BF16))
            self.eps_t = top.enter_context(nc.sbuf_tensor("eps_t", [128, 4], F32))
            self.R_const = Res()
            P.op("dve", lambda e: e.memset(self.eps_t[:], EPS), [], [self.R_const])
            P.dma("sp", self.ident_f[:], self.c_ident, writes=[self.R_const])
            P.op("dve", lambda e: e.tensor_copy(out=self.ident_b[:], in_=self.ident_f[:]), [self.R_const], [self.R_const])
            P.op("dve", lambda e: e.memset(self.ones_b[:], 1.0), [], [self.R_const])
            for i in range(8):
                P.dma("sp", self.xl[i * 512:(i + 1) * 512, :], self.xin[i * 512:(i + 1) * 512, :],
                      writes=[self.R_x[2 + 4 * i + j] for j in range(4)])
            P.dma("sp", self.xc[:, :], self.cin[:, :], writes=[self.R_x[0], self.R_x[1]])
            self.phase_adaln()
            last_ops = []
            for step in self.plan:
                kind, layer = step
                if kind == "moe":
                    last_ops = self.phase_moe(layer)
                elif kind == "mix":
                    last_ops = self.phase_mix(layer)
            fin = []
            for r in self.R_x[2:]:
                if r.lw is not None and r.lw.is_dma:
                    fin.append(r.lw)
            fin = list({id(o): o for o in fin + [o for o in last_ops if o.is_dma]}.values())
            P.flush(final_wait=fin)
        return nc

    def phase_adaln(self):
        nc, P = self.nc, self.P
        S = Scope(self)
        ccs = S.sb([128, 8, 2], F32, "ccs")
        R_cc = Res()
        P.dma("sp", ccs[:], self.cc, writes=[R_cc])
        P.op("act", lambda e: e.activation(out=ccs[:], in_=ccs[:], func=AF.Silu), [R_cc], [R_cc])
        wst = [S.sb([128, 8, 512], F32, "adaw") for _ in range(2)]
        R_w = [Res(), Res()]
        pss = [S.ps([2, 512], F32, "adaps") for _ in range(2)]
        R_ps = [Res(), Res()]
        modv = S.sb([2, 6 * D], F32, "modv")
        R_mv = Res()
        bias = S.sb([2, 6 * D], F32, "adab")
        gm = S.sb([2, D], F32, "gm")
        gf = S.sb([2, D], F32, "gf")
        R_b = Res()
        n = 0
        for layer in range(DEPTH):
            P.dma("act", bias[:], self.W["ada_b"][layer:layer + 1, :].partition_broadcast(2), writes=[R_b])
            P.dma("act", gm[:], self.W["norm_mix_g"][layer:layer + 1, :].partition_broadcast(2), writes=[R_b])
            P.dma("act", gf[:], self.W["norm_ffn_g"][layer:layer + 1, :].partition_broadcast(2), writes=[R_b])
            for cb in range(12):
                b = n % 2
                n += 1
                src = self.W["ada_w"][layer, :, cb * 512:(cb + 1) * 512].rearrange("(k p) n -> p k n", p=128)
                P.dma("sp", wst[b][:], src, writes=[R_w[b]])
                for k in range(8):
                    P.op("pe", lambda e, b=b, k=k: e.matmul(pss[b][:, :], lhsT=ccs[:, k, :], rhs=wst[b][:, k, :],
                                                            start=(k == 0), stop=(k == 7)),
                         [R_cc, R_w[b]], [R_ps[b]])
                P.op("dve", lambda e, b=b, cb=cb: e.tensor_tensor(out=modv[:, cb * 512:(cb + 1) * 512], in0=pss[b][:, :],
                                                                   in1=bias[:, cb * 512:(cb + 1) * 512], op=ALU.add),
                     [R_ps[b], R_b], [R_mv])
            P.op("dve", lambda e: e.scalar_tensor_tensor(out=modv[:, D:2 * D], in0=modv[:, D:2 * D], scalar=1.0,
                                                         in1=gm[:], op0=ALU.add, op1=ALU.mult), [R_mv, R_b], [R_mv])
            P.op("dve", lambda e: e.scalar_tensor_tensor(out=modv[:, 4 * D:5 * D], in0=modv[:, 4 * D:5 * D], scalar=1.0,
                                                         in1=gf[:], op0=ALU.add, op1=ALU.mult), [R_mv, R_b], [R_mv])
            P.dma("sp", self.modd[layer].rearrange("s j d -> s (j d)"), modv[:], reads=[R_mv], writes=[self.R_modd])
        S.close()

    def prenorm_tile(self, T, gt, A, RA, B, RB):
        P = self.P
        i = T["i"]
        T["i"] += 1
        b = i % 2
        xt, R_xt = T["xt"][b]
        hn, R_hn = T["hn"][b]
        pt, R_pt = T["pt"][b]
        st, R_st = T["st"][b]
        junk, R_junk = T["junk"]
        P.dma("sp", xt[:], self.xrows(gt), reads=[self.R_x[gt]], writes=[R_xt])
        P.op("act", lambda e: e.activation(out=junk[:], in_=xt[:], func=AF.Square, accum_out=st[:, 0:1]),
             [R_xt], [R_junk, R_st])
        P.op("act", lambda e: e.activation(out=st[:, 1:2], in_=st[:, 0:1], func=AF.Sqrt, bias=self.eps_t[:, 0:1], scale=1.0 / D),
             [R_st, self.R_const], [R_st])
        P.op("dve", lambda e: e.reciprocal(out=st[:, 2:3], in_=st[:, 1:2]), [R_st], [R_st])
        P.op("dve", lambda e: e.scalar_tensor_tensor(out=hn[:], in0=xt[:], scalar=st[:, 2:3], in1=A[:],
                                                     op0=ALU.mult, op1=ALU.mult), [R_xt, R_st, RA], [R_hn])
        if T.get("bf"):
            hnb, R_hnb = T["hnb"][b]
            P.op("pool", lambda e: e.tensor_tensor(out=hnb[:], in0=hn[:], in1=B[:], op=ALU.add), [R_hn, RB], [R_hnb])
            for k in range(8):
                P.op("pe", lambda e, k=k: e.transpose(out=pt[:, k, :], in_=hnb[:, k * 128:(k + 1) * 128],
                                                      identity=self.ident_b[:]), [R_hnb, self.R_const], [R_pt])
            return hnb, R_hnb, pt, R_pt
        P.op("pool", lambda e: e.tensor_tensor(out=hn[:], in0=hn[:], in1=B[:], op=ALU.add), [R_hn, RB], [R_hn])
        for k in range(8):
            P.op("pe", lambda e, k=k: e.transpose(out=pt[:, k, :], in_=hn[:, k * 128:(k + 1) * 128],
                                                  identity=self.ident_f[:]), [R_hn, self.R_const], [R_pt])
        return hn, R_hn, pt, R_pt

    def prenorm_tiles(self, S, npt=2, bf=False):
        T = {"i": 0, "bf": bf}
        if bf:
            T["hnb"] = [(S.sb([128, D], BF16, "hnb"), Res()) for _ in range(2)]
        T["xt"] = [(S.sb([128, D], F32, "xt"), Res()) for _ in range(2)]
        T["hn"] = [(S.sb([128, D], F32, "hn"), Res()) for _ in range(2)]
        T["pt"] = [(S.ps([128, 8, 128], BF16 if bf else F32, "pt"), Res()) for _ in range(npt)]
        if npt == 1:
            T["pt"] = T["pt"] * 2
        T["st"] = [(S.sb([128, 4], F32, "st"), Res()) for _ in range(2)]
        T["junk"] = (S.sb([128, D], BF16, "junk"), Res())
        return T

    def phase_moe(self, layer):
        nc, P = self.nc, self.P
        with_ctx = layer != DEPTH - 1
        gts = list(range(0 if with_ctx else 2, 34))
        L = ExitStack()
        gates_p = L.enter_context(nc.sbuf_tensor("gates_p%d" % layer, [128, 4, NEXP], F32))
        idx_p = L.enter_context(nc.sbuf_tensor("idx_p%d" % layer, [128, 4, NEXP], I32))
        gates_pc = L.enter_context(nc.sbuf_tensor("gates_pc%d" % layer, [32, NEXP], F32))
        idx_pc = L.enter_context(nc.sbuf_tensor("idx_pc%d" % layer, [32, NEXP], I32))
        R_sel = Res()

        S = Scope(self)
        A_l, RA_l = self.load_bc(S, layer, 0, 4)
        B_l, RB_l = self.load_bc(S, layer, 0, 3, q="act")
        if with_ctx:
            A_c, RA_c = self.load_bc(S, layer, 1, 4)
            B_c, RB_c = self.load_bc(S, layer, 1, 3, q="act")
        T = self.prenorm_tiles(S)
        hb = [(S.sb([128, D], BF16, "hb"), Res()) for _ in range(2)]
        hTf = [(S.sb([128, 8, 128], F32, "hTf"), Res()) for _ in range(2)]
        rt = S.sb([128, 8, NEXP], F32, "router")
        R_rt = Res()
        P.dma("act", rt[:], self.W["moe_router"][layer].rearrange("(k p) e -> p k e", p=128), writes=[R_rt])
        small = S.ps([128, 512], F32, "small")
        lg = [(small[:, 16 * j:16 * j + 16], Res()) for j in range(2)]
        afp = [(small[0:NEXP, 32 + 128 * j:160 + 128 * j], Res()) for j in range(2)]
        ex = [(S.sb([128, NEXP + 2], F32, "ex"), Res()) for _ in range(2)]
        affT = S.sb([NEXP, NLAT], F32, "affT")
        R_aff = Res()
        affTc = S.sb([NEXP, NCTX], F32, "affTc")
        R_affc = Res()
        D1 = Defer()

        def back(b, gt, hn, R_hn, pt, R_pt):
                hbt, R_hb = hb[b]
                P.op("act", lambda e, hbt=hbt, hn=hn: e.activation(out=hbt[:], in_=hn[:], func=AF.Copy), [R_hn], [R_hb])
                P.dma("sp", self.hrows(gt), hbt[:], reads=[R_hb], writes=[self.R_h[gt]])
                hf, R_hf = hTf[b]
                P.op("dve", lambda e, hf=hf, pt=pt: e.tensor_copy(out=hf[:], in_=pt[:]), [R_pt], [R_hf])
                lgt, R_lg = lg[b]
                for k in range(8):
                    P.op("pe", lambda e, k=k, hf=hf, lgt=lgt: e.matmul(lgt, lhsT=hf[:, k, :], rhs=rt[:, k, :],
                                                                        start=(k == 0), stop=(k == 7)),
                         [R_hf, R_rt], [R_lg])
                ext, R_ex = ex[b]
                P.op("act", lambda e, ext=ext, lgt=lgt: e.activation(out=ext[:, 0:NEXP], in_=lgt, func=AF.Exp,
                                                                      accum_out=ext[:, NEXP:NEXP + 1]), [R_lg], [R_ex])
                P.op("dve", lambda e, ext=ext: e.reciprocal(out=ext[:, NEXP + 1:NEXP + 2], in_=ext[:, NEXP:NEXP + 1]),
                     [R_ex], [R_ex])
                P.op("dve", lambda e, ext=ext: e.tensor_scalar(out=ext[:, 0:NEXP], in0=ext[:, 0:NEXP],
                                                               scalar1=ext[:, NEXP + 1:NEXP + 2], scalar2=None, op0=ALU.mult),
                     [R_ex], [R_ex])
                apt, R_ap = afp[b]
                P.op("pe", lambda e, ext=ext, apt=apt: e.transpose(out=apt, in_=ext[:, 0:NEXP], identity=self.ident_f[:]),
                     [R_ex, self.R_const], [R_ap])
                if gt < 2:
                    P.op("act", lambda e, apt=apt, gt=gt: e.activation(out=affTc[:, gt * 128:(gt + 1) * 128], in_=apt,
                                                                        func=AF.Copy), [R_ap], [R_affc])
                else:
                    P.op("act", lambda e, apt=apt, gt=gt: e.activation(out=affT[:, (gt - 2) * 128:(gt - 1) * 128], in_=apt,
                                                                        func=AF.Copy), [R_ap], [R_aff])

        for n_i, gt in enumerate(gts):
            b = n_i % 2
            if gt < 2:
                hn, R_hn, pt, R_pt = self.prenorm_tile(T, gt, A_c, RA_c, B_c, RB_c)
            else:
                hn, R_hn, pt, R_pt = self.prenorm_tile(T, gt, A_l, RA_l, B_l, RB_l)
            D1.push(lambda b=b, gt=gt, hn=hn, R_hn=R_hn, pt=pt, R_pt=R_pt: back(b, gt, hn, R_hn, pt, R_pt))
        D1.flush()
        if self.debug and layer == self.debug_layer:
            P.dma("sp", self.dbg_aff, affT[:], reads=[R_aff])
        vals = S.sb([NEXP, CAP_L], F32, "vals")
        idxu = S.sb([NEXP, CAP_L], U32, "idxu")
        idxf = S.sb([NEXP, CAP_L], F32, "idxf")
        R_v, R_i = Res(), Res()
        for r in range(CAP_L // 8):
            sl = slice(8 * r, 8 * r + 8)
            P.op("dve", lambda e, sl=sl: e.max(out=vals[:, sl], in_=affT[:]), [R_aff], [R_v])
            P.op("dve", lambda e, sl=sl: e.max_index(out=idxu[:, sl], in_max=vals[:, sl], in_values=affT[:]),
                 [R_aff, R_v], [R_i])
            P.op("dve", lambda e, sl=sl: e.match_replace(out=affT[:], in_to_replace=vals[:, sl], in_values=affT[:],
                                                         imm_value=-1.0), [R_aff, R_v], [R_aff])
        P.op("dve", lambda e: e.tensor_copy(out=idxf[:], in_=idxu[:]), [R_i], [R_i])
        tp = small[:, 288:352].rearrange("p (c e) -> p c e", c=4)
        R_tp = Res()
        for c in range(4):
            P.op("pe", lambda e, c=c: e.transpose(out=tp[:, c, :], in_=vals[:, c * 128:(c + 1) * 128],
                                                  identity=self.ident_f[0:NEXP, 0:NEXP]), [R_v, self.R_const], [R_tp])
        P.op("dve", lambda e: e.tensor_copy(out=gates_p[:], in_=tp), [R_tp], [R_sel])
        for c in range(4):
            P.op("pe", lambda e, c=c: e.transpose(out=tp[:, c, :], in_=idxf[:, c * 128:(c + 1) * 128],
                                                  identity=self.ident_f[0:NEXP, 0:NEXP]), [R_i, self.R_const, R_sel], [R_tp])
        P.op("dve", lambda e: e.tensor_copy(out=idx_p[:], in_=tp), [R_tp], [R_sel])
        if with_ctx:
            valsc = S.sb([NEXP, CAP_C], F32, "valsc")
            idxuc = S.sb([NEXP, CAP_C], U32, "idxuc")
            idxfc = S.sb([NEXP, CAP_C], F32, "idxfc")
            R_vc, R_ic = Res(), Res()
            for r in range(CAP_C // 8):
                sl = slice(8 * r, 8 * r + 8)
                P.op("dve", lambda e, sl=sl: e.max(out=valsc[:, sl], in_=affTc[:]), [R_affc], [R_vc])
                P.op("dve", lambda e, sl=sl: e.max_index(out=idxuc[:, sl], in_max=valsc[:, sl], in_values=affTc[:]),
                     [R_affc, R_vc], [R_ic])
                P.op("dve", lambda e, sl=sl: e.match_replace(out=affTc[:], in_to_replace=valsc[:, sl], in_values=affTc[:],
                                                             imm_value=-1.0), [R_affc, R_vc], [R_affc])
            P.op("dve", lambda e: e.tensor_copy(out=idxfc[:], in_=idxuc[:]), [R_ic], [R_ic])
            tpc = small[0:32, 352:384].rearrange("p (c e) -> p c e", c=2)
            R_tpc = Res()
            P.op("pe", lambda e: e.transpose(out=tpc[:, 0, :], in_=valsc[:, :], identity=self.ident_f[0:NEXP, 0:NEXP]),
                 [R_vc, self.R_const], [R_tpc])
            P.op("pe", lambda e: e.transpose(out=tpc[:, 1, :], in_=idxfc[:, :], identity=self.ident_f[0:NEXP, 0:NEXP]),
                 [R_ic, self.R_const], [R_tpc])
            P.op("dve", lambda e: e.tensor_copy(out=gates_pc[:], in_=tpc[:, 0, :]), [R_tpc], [R_sel])
            P.op("dve", lambda e: e.tensor_copy(out=idx_pc[:], in_=tpc[:, 1, :]), [R_tpc], [R_sel])
        if self.debug and layer == self.debug_layer:
            P.dma("sp", self.dbg_idx, idx_p[:].rearrange("p c e -> p (c e)"), reads=[R_sel])
            P.dma("sp", self.dbg_gate, gates_p[:].rearrange("p c e -> p (c e)"), reads=[R_sel])
        S.close()

        import os as _os
        if _os.environ.get("DBG_SKIP_F2"):
            L.close()
            return []
        S = Scope(self)
        G_l, RG_l = self.load_bc(S, layer, 0, 5)
        if with_ctx:
            G_c, RG_c = self.load_bc(S, layer, 1, 5, q="act")
        NS = CAP_L + (CAP_C if with_ctx else 0)
        stage = [(S.sb([128, 8, D], F32, "wst"), Res()) for _ in range(2)]
        wbuf = [(S.sb([128, 8, D], BF16, "wb"), Res()) for _ in range(3)]
        gth = [(S.sb([128, D], BF16, "gth"), Res()) for _ in range(5)]
        tpp = [(S.ps([128, 8, 128], BF16, "tpp"), Res()) for _ in range(2)]
        XeT = [(S.sb([128, 8, NS], BF16, "XeT"), Res()) for _ in range(2)]
        sg = (S.sb([128, 8, NS], F32, "sg"), Res())
        hid = (S.sb([128, 8, NS], BF16, "hid"), Res())
        hps = [(S.ps([128, 512], F32, "hps"), Res()) for _ in range(3)]
        yps = [(S.ps([128, 512], F32, "yps"), Res()) for _ in range(2)]
        hpc = (S.ps([128, 8, CAP_C], F32, "hpc"), Res())
        ysb = [(S.sb([128, D], F32, "ysb"), Res()) for _ in range(2)]
        R_scat_prev = []
        nstage = [0]
        nw = [0]
        cast_eng = ["act", "dve", "act"]
        ng = [0]
        ny = [0]
        nh = [0]
        last_ops = []

        def load_w(name, e, which):
            sb_, R_s = stage[nstage[0] % 2]
            nstage[0] += 1
            wb_, R_wb = wbuf[nw[0] % 3]
            nw[0] += 1
            src = self.W[name][layer, e].rearrange("(k p) n -> p k n", p=128)
            P.dma("sp", sb_[:, 0:4, :], src[:, 0:4, :], writes=[R_s])
            P.dma("sp", sb_[:, 4:8, :], src[:, 4:8, :], writes=[R_s])
            ce = cast_eng[which]
            if ce == "act":
                P.op("act", lambda e_: e_.activation(out=wb_[:], in_=sb_[:], func=AF.Copy), [R_s], [R_wb])
            else:
                P.op(ce, lambda e_: e_.tensor_copy(out=wb_[:], in_=sb_[:]), [R_s], [R_wb])
            return wb_, R_wb

        import os as _os
        scat_box = [[]]

        chunks = [(c, 128) for c in range(4)] + ([(4, CAP_C)] if with_ctx else [])
        NE = int(_os.environ.get("DBG_NEXP", NEXP))

        def do_gather(e):
            xe, R_xe = XeT[e % 2]
            for c, npart in chunks:
                g, R_g = gth[ng[0] % 5]
                ng[0] += 1
                if c < 4:
                    off = bass.IndirectOffsetOnAxis(ap=idx_p[:, c, e:e + 1], axis=0)
                    srcd, rds = self.hl, [self.R_h[i] for i in range(2, 34)]
                else:
                    off = bass.IndirectOffsetOnAxis(ap=idx_pc[:, e:e + 1], axis=0)
                    srcd, rds = self.hc, [self.R_h[0], self.R_h[1]]
                P.op("pool", lambda e_, g=g, npart=npart, srcd=srcd, off=off: e_.indirect_dma_start(
                    out=g[0:npart, :], out_offset=None, in_=srcd, in_offset=off), [R_sel] + rds, [R_g], dma=True)
                tp_, R_tp_ = tpp[ng[0] % 2]
                for k in range(8):
                    P.op("pe", lambda e_, g=g, npart=npart, tp_=tp_, k=k: e_.transpose(
                        out=tp_[:, k, 0:npart], in_=g[0:npart, k * 128:(k + 1) * 128],
                        identity=self.ident_b[0:npart, 0:npart]), [R_g, self.R_const], [R_tp_])
                P.op("dve", lambda e_, tp_=tp_, xe=xe, c=c, npart=npart: e_.tensor_copy(
                    out=xe[:, :, c * 128:c * 128 + npart], in_=tp_[:, :, 0:npart]), [R_tp_], [R_xe])

        def do_expert(e):
            R_scat_prev = scat_box[0]
            xe, R_xe = XeT[e % 2]
            if self.debug and e == 0 and layer == self.debug_layer:
                P.dma("sp", self.dbg_xe, xe[:, :, :].rearrange("p k s -> p (k s)"), reads=[R_xe])
            if int(_os.environ.get("DBG_STAGE", 9)) < 2:
                return
            wg, R_wg = load_w("moe_w_gate", e, 0)
            sgt, R_sg = sg
            hpct, R_hpc = hpc
            for f in range(8):
                hp, R_hp = hps[nh[0] % 3]
                nh[0] += 1
                for k in range(8):
                    P.op("pe", lambda e_, hp=hp, k=k, f=f: e_.matmul(hp[:, :], lhsT=wg[:, k, f * 128:(f + 1) * 128],
                                                                      rhs=xe[:, k, 0:CAP_L], start=(k == 0), stop=(k == 7)),
                         [R_wg, R_xe], [R_hp])
                if with_ctx:
                    for k in range(8):
                        P.op("pe", lambda e_, k=k, f=f: e_.matmul(hpct[:, f, :], lhsT=wg[:, k, f * 128:(f + 1) * 128],
                                                                   rhs=xe[:, k, CAP_L:NS], start=(k == 0), stop=(k == 7)),
                             [R_wg, R_xe], [R_hpc])
                P.op("act", lambda e_, hp=hp, f=f: e_.activation(out=sgt[:, f, 0:CAP_L], in_=hp[:, :], func=AF.Silu),
                     [R_hp], [R_sg])
            if with_ctx:
                P.op("act", lambda e_: e_.activation(out=sgt[:, :, CAP_L:NS], in_=hpct[:, :, :], func=AF.Silu),
                     [R_hpc], [R_sg])
            if int(_os.environ.get("DBG_STAGE", 9)) < 3:
                if self.debug and e == 0:
                    P.dma("sp", self.dbg_sg, sgt[:, :, :].rearrange("p k s -> p (k s)"), reads=[R_sg])
                return
            wu, R_wu = load_w("moe_w_up", e, 1)
            hd_, R_hid = hid
            for f in range(8):
                hp, R_hp = hps[nh[0] % 3]
                nh[0] += 1
                for k in range(8):
                    P.op("pe", lambda e_, hp=hp, k=k, f=f: e_.matmul(hp[:, :], lhsT=wu[:, k, f * 128:(f + 1) * 128],
                                                                      rhs=xe[:, k, 0:CAP_L], start=(k == 0), stop=(k == 7)),
                         [R_wu, R_xe], [R_hp])
                if with_ctx:
                    for k in range(8):
                        P.op("pe", lambda e_, k=k, f=f: e_.matmul(hpct[:, f, :], lhsT=wu[:, k, f * 128:(f + 1) * 128],
                                                                   rhs=xe[:, k, CAP_L:NS], start=(k == 0), stop=(k == 7)),
                             [R_wu, R_xe], [R_hpc])
                P.op("dve", lambda e_, hp=hp, f=f: e_.tensor_tensor(out=hd_[:, f, 0:CAP_L], in0=hp[:, :],
                                                                     in1=sgt[:, f, 0:CAP_L], op=ALU.mult),
                     [R_hp, R_sg], [R_hid])
            if with_ctx:
                P.op("dve", lambda e_: e_.tensor_tensor(out=hd_[:, :, CAP_L:NS], in0=hpct[:, :, :],
                                                        in1=sgt[:, :, CAP_L:NS], op=ALU.mult), [R_hpc, R_sg], [R_hid])
            if self.debug and e == 0 and layer == self.debug_layer:
                P.dma("sp", self.dbg_hid, hd_[:, :, :].rearrange("p k s -> p (k s)"), reads=[R_hid])
                P.dma("sp", self.dbg_sg, sgt[:, :, :].rearrange("p k s -> p (k s)"), reads=[R_sg])
                P.dma("sp", self.dbg_xe2, xe[:, :, :].rearrange("p k s -> p (k s)"), reads=[R_xe])
                P.dma("sp", self.dbg_wg, wg[:, :, :].rearrange("p k s -> p (k s)"), reads=[R_wg])
            if int(_os.environ.get("DBG_STAGE", 9)) < 4:
                return
            if e + 1 < NE:
                do_gather(e + 1)
            wd, R_wd = load_w("moe_w_down", e, 2)
            R_scat_cur = []
            for c, npart in chunks:
                yt, R_y = ysb[ny[0] % 2]
                ny[0] += 1
                for half in range(2):
                    yp, R_yp = yps[half]
                    for f in range(8):
                        P.op("pe", lambda e_, yp=yp, f=f, c=c, npart=npart, half=half: e_.matmul(
                            yp[0:npart, :], lhsT=hd_[:, f, c * 128:c * 128 + npart],
                            rhs=wd[:, f, half * 512:(half + 1) * 512], start=(f == 0), stop=(f == 7)),
                            [R_hid, R_wd], [R_yp])
                    if c < 4:
                        gcol, Gt, RG = gates_p[:, c, e:e + 1], G_l, RG_l
                    else:
                        gcol, Gt, RG = gates_pc[:, e:e + 1], G_c, RG_c
                    P.op("dve", lambda e_, yt=yt, yp=yp, gcol=gcol, Gt=Gt, npart=npart, half=half: e_.scalar_tensor_tensor(
                        out=yt[0:npart, half * 512:(half + 1) * 512], in0=yp[0:npart, :], scalar=gcol,
                        in1=Gt[0:npart, half * 512:(half + 1) * 512], op0=ALU.mult, op1=ALU.mult),
                        [R_yp, R_sel, RG], [R_y])
                R_sc = Res()
                if c < 4:
                    off = bass.IndirectOffsetOnAxis(ap=idx_p[:, c, e:e + 1], axis=0)
                    dst = self.xl
                else:
                    off = bass.IndirectOffsetOnAxis(ap=idx_pc[:, e:e + 1], axis=0)
                    dst = self.xc
                o = P.op("pool", lambda e_, yt=yt, npart=npart, dst=dst, off=off: e_.indirect_dma_start(
                    out=dst, out_offset=off, in_=yt[0:npart, :], in_offset=None, compute_op=ALU.add),
                    [R_y, R_sel] + R_scat_prev, [R_sc], dma=True)
                R_scat_cur.append(R_sc)
                last_ops.append(o)
            scat_box[0] = R_scat_cur

        do_gather(0)
        for e in range(NE):
            do_expert(e)
        R_scat_prev = scat_box[0]
        fo = P.dma("pool", self.fence_d, self.eps_t[:], reads=R_scat_prev + [self.R_const], writes=self.R_x)
        last_ops = [fo]
        S.close()
        L.close()
        return last_ops


    def load_cast(self, S, src_ap, shape, stage, R_stage, q="sp", eng="dve", name="w", into=None):
        P = self.P
        if into is None:
            wt = S.sb(shape, BF16, name)
            R = Res()
        else:
            wt, R = into
        n = 1
        for d_ in shape[1:]:
            n *= d_
        sv = stage[0:shape[0], 0:n]
        if len(shape) == 3:
            sv = sv.rearrange("p (a b) -> p a b", a=shape[1])
        P.dma(q, sv, src_ap, writes=[R_stage])
        wap = wt[:]
        if eng == "act":
            P.op("act", lambda e: e.activation(out=wap, in_=sv, func=AF.Copy), [R_stage], [R])
        else:
            P.op(eng, lambda e: e.tensor_copy(out=wap, in_=sv), [R_stage], [R])
        return wt, R

    def normrope(self, *args):
        for _ in self.normrope_gen(*args):
            pass

    @staticmethod
    def run_zip(gens):
        gens = list(gens)
        while gens:
            for g in list(gens):
                try:
                    next(g)
                except StopIteration:
                    gens.remove(g)

    def normrope_gen(self, Wk, ps, R_ps, M, T, ones_ap, inv_n, gcol, R_g, rm, R_rm, cos_ap, sin_ap, R_tab, out_ap, R_out):
        P = self.P
        sq, R_sq = Wk["sq"]
        sm, R_sm = Wk["sm"]
        r, R_r = Wk["r"]
        xnf, R_xnf = Wk["xnf"]
        xnb, R_xnb = Wk["xnb"]
        rp, R_rp = Wk["rp"]
        t1, R_t1 = Wk["t1"]
        P.op("act", lambda e: e.activation(out=sq[0:M, 0:T], in_=ps, func=AF.Square), [R_ps], [R_sq])
        yield
        P.op("pe", lambda e: e.matmul(sm[0:M, 0:T], lhsT=ones_ap, rhs=sq[0:M, 0:T], start=True, stop=True),
             [R_sq, self.R_const], [R_sm])
        yield
        P.op("act", lambda e: e.activation(out=r[0:M, 0:T], in_=sm[0:M, 0:T], func=AF.Sqrt, bias=self.eps_t[0:M, 0:1],
                                           scale=inv_n), [R_sm, self.R_const], [R_r])
        yield
        P.op("dve", lambda e: e.reciprocal(out=r[0:M, 0:T], in_=r[0:M, 0:T]), [R_r], [R_r])
        P.op("dve", lambda e: e.scalar_tensor_tensor(out=xnf[0:M, 0:T], in0=ps, scalar=gcol, in1=r[0:M, 0:T],
                                                     op0=ALU.mult, op1=ALU.mult), [R_ps, R_g, R_r], [R_xnf])
        yield
        P.op("act", lambda e: e.activation(out=xnb[0:M, 0:T], in_=xnf[0:M, 0:T], func=AF.Copy), [R_xnf], [R_xnb])
        yield
        P.op("pe", lambda e: e.matmul(rp[0:M, 0:T], lhsT=rm, rhs=xnb[0:M, 0:T], start=True, stop=True),
             [R_xnb, R_rm], [R_rp])
        P.op("pool", lambda e: e.tensor_tensor(out=xnf[0:M, 0:T], in0=xnf[0:M, 0:T], in1=cos_ap, op=ALU.mult),
             [R_xnf, R_tab], [R_xnf])
        yield
        P.op("dve", lambda e: e.tensor_tensor(out=t1[0:M, 0:T], in0=rp[0:M, 0:T], in1=sin_ap, op=ALU.mult),
             [R_rp, R_tab], [R_t1])
        P.op("dve", lambda e: e.tensor_tensor(out=out_ap, in0=xnf[0:M, 0:T], in1=t1[0:M, 0:T], op=ALU.add),
             [R_xnf, R_t1], [R_out])
        yield

    def normrope_tiles(self, S):
        Wk = {}
        Wk["sq"] = (S.sb([128, 512], BF16, "sq"), Res())
        Wk["sm"] = (S.ps([128, 512], F32, "sm"), Res())
        Wk["r"] = (S.sb([128, 512], F32, "r"), Res())
        Wk["xnf"] = (S.sb([128, 512], F32, "xnf"), Res())
        Wk["xnb"] = (S.sb([128, 512], BF16, "xnb"), Res())
        Wk["rp"] = (S.ps([128, 512], F32, "rp"), Res())
        Wk["t1"] = (S.sb([128, 512], F32, "t1"), Res())
        return Wk

    def qblocks(self, need_ctx_out):
        qb = []
        if need_ctx_out:
            qb.append((0, 256, [0, 1]))
        for b in range(8):
            qb.append((256 + 512 * b, 512, list(range(34))))
        return qb

    def phase_outproj(self, layer, wname, j, need_ctx_out, src=None, nk=8):
        P = self.P
        S = Scope(self)
        G_l, RG_l = self.load_bc(S, layer, 0, 2)
        if need_ctx_out:
            G_c, RG_c = self.load_bc(S, layer, 1, 2, q="act")
        stage = S.sb([128, 8 * D], F32, "stg")
        R_stage = Res()
        src = self.od if src is None else src
        wo = S.sb([128, nk, D], BF16, "wo")
        R_wo = Res()
        wv_ = self.W[wname][j].rearrange("(k p) n -> p k n", p=128)
        for k0 in range(0, nk, 8):
            self.load_cast(S, wv_[:, k0:k0 + 8, :], [128, 8, D], stage, R_stage, eng="act", into=(wo[:, k0:k0 + 8, :], R_wo))
        ot = [(S.sb([128, nk, 128], BF16, "ot"), Res()) for _ in range(2)]
        xt = [(S.sb([128, D], F32, "xt"), Res()) for _ in range(2)]
        yps = [(S.ps([128, 512], F32, "yps"), Res()) for _ in range(4)]
        odv = src.rearrange("(k p) t -> p k t", p=128)
        gts = list(range(0 if need_ctx_out else 2, 34))
        for n_i, gt in enumerate(gts):
            b = n_i % 2
            o_t, R_o = ot[b]
            x_t, R_xt = xt[b]
            P.dma("sp", o_t[:], odv[:, :, gt * 128:(gt + 1) * 128], reads=[self.R_od], writes=[R_o])
            P.dma("act", x_t[:], self.xrows(gt), reads=[self.R_x[gt]], writes=[R_xt])
            Gt, RG = (G_c, RG_c) if gt < 2 else (G_l, RG_l)
            for half in range(2):
                yp, R_yp = yps[2 * b + half]
                for k in range(nk):
                    P.op("pe", lambda e, yp=yp, k=k, o_t=o_t, half=half: e.matmul(
                        yp[:, :], lhsT=o_t[:, k, :], rhs=wo[:, k, half * 512:(half + 1) * 512], start=(k == 0), stop=(k == nk - 1)),
                        [R_o, R_wo], [R_yp])
                hs = slice(half * 512, (half + 1) * 512)
                P.op("dve", lambda e, yp=yp, Gt=Gt, x_t=x_t, hs=hs: e.tensor_tensor(out=yp[:, :], in0=yp[:, :], in1=Gt[:, hs], op=ALU.mult),
                     [R_yp, RG], [R_yp])
                P.op("dve", lambda e, yp=yp, x_t=x_t, hs=hs: e.tensor_tensor(out=x_t[:, hs], in0=yp[:, :], in1=x_t[:, hs], op=ALU.add),
                     [R_yp, R_xt], [R_xt])
            P.dma("sp", self.xrows(gt), x_t[:], reads=[R_xt], writes=[self.R_x[gt]])
        S.close()

    def phase_mla(self, layer):
        nc, P = self.nc, self.P
        j = layer // 3
        need_ctx_out = layer != DEPTH - 1
        L = ExitStack()
        cqT = L.enter_context(nc.sbuf_tensor("cqT%d" % layer, [128, 4, NTOK], BF16))
        ckvT = L.enter_context(nc.sbuf_tensor("ckvT%d" % layer, [128, 2, NTOK], BF16))
        krT = L.enter_context(nc.sbuf_tensor("krT%d" % layer, [32, NTOK], BF16))
        R_cq = [Res() for _ in range(9)]
        R_ckv = [Res() for _ in range(9)]
        R_kr = [Res() for _ in range(9)]
        blocks = [(0, 256)] + [(256 + 512 * b, 512) for b in range(8)]

        S = Scope(self)
        A_l, RA_l = self.load_bc(S, layer, 0, 1)
        B_l, RB_l = self.load_bc(S, layer, 0, 0, q="act")
        A_c, RA_c = self.load_bc(S, layer, 1, 1)
        B_c, RB_c = self.load_bc(S, layer, 1, 0, q="act")
        T_ = self.prenorm_tiles(S, npt=2, bf=True)
        stage = S.sb([128, 8 * 800], F32, "stg")
        R_stage = Res()
        win, R_win = self.load_cast(S, self.W["mla_w_in"][j].rearrange("(k p) n -> p k n", p=128), [128, 8, 800],
                                    stage, R_stage, eng="act", name="win")
        gq = S.sb([128, 4], F32, "gq")
        gkv = S.sb([128, 2], F32, "gkv")
        R_gl = Res()
        P.dma("act", gq[:], self.W["mla_q_norm_g"][j].rearrange("(a p) -> p a", p=128), writes=[R_gl], allow_slow_non_contiguous=True)
        P.dma("act", gkv[:], self.W["mla_kv_norm_g"][j].rearrange("(a p) -> p a", p=128), writes=[R_gl], allow_slow_non_contiguous=True)
        hTb = [(S.sb([128, 8, 512], BF16, "hTb"), Res()) for _ in range(2)]
        dps = [(S.ps([128, 512], F32, "dps"), Res()) for _ in range(4)]
        smp = (S.ps([128, 512], F32, "smp"), Res())
        sq = (S.sb([128, 512], BF16, "sq"), Res())
        rr = (S.sb([128, 512], F32, "rr"), Res())
        D2 = Defer()
        for bi, (t0, T) in enumerate(blocks):
            hb_, R_hb = hTb[bi % 2]
            for ti in range(T // 128):
                gt = t0 // 128 + ti
                if gt < 2:
                    hn, R_hn, pt, R_pt = self.prenorm_tile(T_, gt, A_c, RA_c, B_c, RB_c)
                else:
                    hn, R_hn, pt, R_pt = self.prenorm_tile(T_, gt, A_l, RA_l, B_l, RB_l)
                P.op("dve", lambda e, hb_=hb_, pt=pt, ti=ti: e.tensor_copy(out=hb_[:, :, ti * 128:(ti + 1) * 128], in_=pt[:]),
                     [R_pt], [R_hb])
            for grp, chunks, gain, dst, R_dst, nfeat in ((0, [0, 1, 2, 3], gq, cqT, R_cq, 512.0), (1, [4, 5], gkv, ckvT, R_ckv, 256.0)):
                smt, R_smt = smp
                for ci, cj in enumerate(chunks):
                    dp, R_dp = dps[ci]
                    for k in range(8):
                        P.op("pe", lambda e, dp=dp, k=k, cj=cj, hb_=hb_, T=T: e.matmul(
                            dp[:, 0:T], lhsT=win[:, k, cj * 128:(cj + 1) * 128], rhs=hb_[:, k, 0:T], start=(k == 0), stop=(k == 7)),
                            [R_win, R_hb], [R_dp])
                    sqt, R_sq = sq
                    P.op("act", lambda e, dp=dp, sqt=sqt, T=T: e.activation(out=sqt[:, 0:T], in_=dp[:, 0:T], func=AF.Square),
                         [R_dp], [R_sq])
                    P.op("pe", lambda e, sqt=sqt, smt=smt, T=T, ci=ci, n=len(chunks): e.matmul(
                        smt[:, 0:T], lhsT=self.ones_b[:, :], rhs=sqt[:, 0:T], start=(ci == 0), stop=(ci == n - 1)),
                        [R_sq, self.R_const], [R_smt])
                rt_, R_rr = rr
                P.op("act", lambda e, rt_=rt_, smt=smt, T=T, nfeat=nfeat: e.activation(
                    out=rt_[:, 0:T], in_=smt[:, 0:T], func=AF.Sqrt, bias=self.eps_t[:, 0:1], scale=1.0 / nfeat),
                    [R_smt, self.R_const], [R_rr])
                P.op("dve", lambda e, rt_=rt_, T=T: e.reciprocal(out=rt_[:, 0:T], in_=rt_[:, 0:T]), [R_rr], [R_rr])
                for ci, cj in enumerate(chunks):
                    dp, R_dp = dps[ci]
                    P.op("dve", lambda e, dp=dp, ci=ci, gain=gain, rt_=rt_, dst=dst, t0=t0, T=T: e.scalar_tensor_tensor(
                        out=dst[:, ci, t0:t0 + T], in0=dp[:, 0:T], scalar=gain[:, ci:ci + 1], in1=rt_[:, 0:T],
                        op0=ALU.mult, op1=ALU.mult), [R_dp, R_gl, R_rr], [R_dst[bi]])
            dp, R_dp = dps[0]
            for k in range(8):
                P.op("pe", lambda e, dp=dp, k=k, hb_=hb_, T=T: e.matmul(dp[0:32, 0:T], lhsT=win[:, k, 768:800], rhs=hb_[:, k, 0:T],
                                                                         start=(k == 0), stop=(k == 7)), [R_win, R_hb], [R_dp])
            P.op("act", lambda e, dp=dp, t0=t0, T=T: e.activation(out=krT[0:32, t0:t0 + T], in_=dp[0:32, 0:T], func=AF.Copy),
                 [R_dp], [R_kr[bi]])
        S.close()

        if int(_os_environ_get("DBG_MLA", 3)) < 2:
            L.close()
            return []
        S = Scope(self)
        stage = S.sb([128, 4 * 1536], F32, "stg")
        R_stage = Res()
        wuq, R_wuq = self.load_cast(S, self.W["mla_w_uq"][j].rearrange("(k p) n -> p k n", p=128), [128, 4, 1536],
                                    stage, R_stage, eng="act", name="wuq")
        wukv, R_wukv = self.load_cast(S, self.W["mla_w_ukv"][j].rearrange("(k p) n -> p k n", p=128), [128, 2, 2048],
                                      stage, R_stage, eng="act", name="wukv")
        wkp = S.sb([128, 2, 16, 96], BF16, "wkp")
        R_wkp = Res()
        P.op("pool", lambda e: e.memset(wkp[:], 0.0), [], [R_wkp])
        wv4 = wukv[:, :, :].rearrange("p a (h c) -> p a h c", h=16)
        for a in range(2):
            P.op("dve", lambda e, a=a: e.tensor_copy(out=wkp[:, a, :, 0:64], in_=wv4[:, a, :, 0:64]), [R_wukv, R_wkp], [R_wkp])
        rm, R_rm = self.load_cast(S, self.c_rm96, [96, 96], stage, R_stage, name="rm96")
        sel = S.sb([32, 96], BF16, "sel")
        R_sel96 = Res()
        P.op("pool", lambda e: e.memset(sel[:], 0.0), [], [R_sel96])
        P.op("dve", lambda e: e.tensor_copy(out=sel[:, 64:96], in_=self.ident_b[0:32, 0:32]), [self.R_const, R_sel96], [R_sel96])
        gqn = S.sb([96, 1], F32, "gqn")
        gkn = S.sb([96, 1], F32, "gkn")
        R_gh = Res()
        P.dma("act", gqn[:], self.W["mla_qn_g"][j].rearrange("(p o) -> p o", o=1), writes=[R_gh])
        P.dma("act", gkn[:], self.W["mla_kn_g"][j].rearrange("(p o) -> p o", o=1), writes=[R_gh])
        sps = [(S.ps([128, 512], F32, "sps"), Res()) for _ in range(7)]
        Wk = {}
        Wk["sq"] = (S.sb([128, 512], BF16, "sq"), Res())
        Wk["sm"] = sps[1]
        Wk["r"] = (S.sb([128, 512], F32, "r"), Res())
        Wk["xnf"] = (S.sb([128, 512], F32, "xnf"), Res())
        Wk["xnb"] = (S.sb([128, 512], BF16, "xnb"), Res())
        Wk["rp"] = sps[2]
        Wk["t1"] = (S.sb([128, 512], F32, "t1"), Res())
        Wk2 = {"sq": (S.sb([128, 512], BF16, "sq2"), Res()), "sm": sps[4], "r": (S.sb([128, 512], F32, "r2"), Res()),
               "xnf": (S.sb([128, 512], F32, "xnf2"), Res()), "xnb": (S.sb([128, 512], BF16, "xnb2"), Res()), "rp": sps[5],
               "t1": (S.sb([128, 512], F32, "t12"), Res())}
        cs = [(S.sb([96, 512], F32, "cos"), S.sb([96, 512], F32, "sin"), Res()) for _ in range(2)]
        QT = (S.sb([96, NTOK], BF16, "QT"), Res())
        KT = (S.sb([96, NTOK], BF16, "KT"), Res())
        Va = (S.sb([128, 34, 128], BF16, "Va"), Res())
        P.op("pool", lambda e: e.memset(Va[0][:, :, 64:128], 1.0), [], [Va[1]])
        gps = sps[0]
        acc = (S.ps([128, 512], F32, "acc"), Res())
        NB, GRP = 6, 3
        pts = [(S.sb([128, 512], BF16, "pt"), Res()) for _ in range(NB)]
        rec = (S.sb([64, 512], F32, "rec"), Res())
        otl = [(S.sb([64, 512], BF16, "ot"), Res()) for _ in range(2)]
        ncs = [0]
        scale = 96.0 ** -0.5

        def do_head(h):
            qt, R_qt = QT
            kt, R_kt = KT
            va, R_va = Va
            gp, R_gp = gps
            for bi, (t0, T) in enumerate(blocks):
                cos_t, sin_t, R_tab = cs[ncs[0] % 2]
                ncs[0] += 1
                P.dma("sp", cos_t[:, 0:T], self.c_cosA[:, t0:t0 + T], writes=[R_tab])
                P.dma("sp", sin_t[:, 0:T], self.c_sinA[:, t0:t0 + T], writes=[R_tab])
                def kchain(bi=bi, t0=t0, T=T, cos_t=cos_t, sin_t=sin_t, R_tab=R_tab):
                    for a in range(2):
                        P.op("pe", lambda e, a=a: e.matmul(gp[0:96, 0:T], lhsT=wkp[:, a, h, :], rhs=ckvT[:, a, t0:t0 + T],
                                                           start=(a == 0), stop=False), [R_wkp, R_ckv[bi]], [R_gp])
                    P.op("pe", lambda e: e.matmul(gp[0:96, 0:T], lhsT=sel[:, :], rhs=krT[0:32, t0:t0 + T],
                                                  start=False, stop=True), [R_sel96, R_kr[bi]], [R_gp])
                    yield
                    yield from self.normrope_gen(Wk, gp[0:96, 0:T], R_gp, 96, T, self.ones_b[0:96, 0:96], 1.0 / 96, gkn[:, 0:1], R_gh,
                                                 rm[:, :], R_rm, cos_t[:, 0:T], sin_t[:, 0:T], R_tab, kt[:, t0:t0 + T], R_kt)

                def qchain(bi=bi, t0=t0, T=T, cos_t=cos_t, sin_t=sin_t, R_tab=R_tab):
                    gq_, R_gq_ = sps[3]
                    for a in range(4):
                        P.op("pe", lambda e, a=a: e.matmul(gq_[0:96, 0:T], lhsT=wuq[:, a, h * 96:(h + 1) * 96],
                                                           rhs=cqT[:, a, t0:t0 + T], start=(a == 0), stop=(a == 3)),
                             [R_wuq, R_cq[bi]], [R_gq_])
                    yield
                    yield from self.normrope_gen(Wk2, gq_[0:96, 0:T], R_gq_, 96, T, self.ones_b[0:96, 0:96], 1.0 / 96, gqn[:, 0:1], R_gh,
                                                 rm[:, :], R_rm, cos_t[:, 0:T], sin_t[:, 0:T], R_tab, qt[:, t0:t0 + T], R_qt)
                chains = [kchain()]
                if bi > 0 or need_ctx_out:
                    chains.append(qchain())
                self.run_zip(chains)
                for ti in range(T // 128):
                    kc = t0 // 128 + ti
                    for a in range(2):
                        P.op("pe", lambda e, a=a, kc=kc, ti=ti: e.matmul(
                            gp[:, ti * 64:(ti + 1) * 64], lhsT=ckvT[:, a, kc * 128:(kc + 1) * 128], rhs=wv4[:, a, h, 64:128],
                            start=(a == 0), stop=(a == 1)), [R_wukv, R_ckv[bi]], [R_gp])
                nt = T // 128
                P.op("act", lambda e, t0=t0, nt=nt: e.activation(
                    out=va[:, t0 // 128:t0 // 128 + nt, 0:64], in_=gp[:, 0:nt * 64].rearrange("p (t c) -> p t c", t=nt),
                    func=AF.Copy), [R_gp], [R_va])
            if self.debug and h == 0:
                P.dma("sp", self.dbg_qt[0:96, :], qt[:, :], reads=[R_qt])
                P.dma("sp", self.dbg_kt[0:96, :], kt[:, :], reads=[R_kt])
                P.dma("sp", self.dbg_va, va[:, :, :].rearrange("p a b -> p (a b)"), reads=[R_va])
            for qi, (q0, T, kcs) in enumerate(self.qblocks(need_ctx_out)):
                ac, R_ac = (acc, sps[0])[qi % 2]
                n = len(kcs)

                def score(i):
                    sp_, R_sp = sps[1 + i % NB]
                    kc = kcs[i]
                    P.op("pe", lambda e, sp_=sp_, kc=kc, T=T, q0=q0: e.matmul(sp_[:, 0:T], lhsT=kt[:, kc * 128:(kc + 1) * 128],
                                                                   rhs=qt[:, q0:q0 + T], start=True, stop=True),
                         [R_kt, R_qt], [R_sp])
                    pt_, R_pt_ = pts[i % NB]
                    P.op("act", lambda e, sp_=sp_, pt_=pt_, T=T: e.activation(out=pt_[:, 0:T], in_=sp_[:, 0:T], func=AF.Exp, scale=scale),
                         [R_sp], [R_pt_])

                def pv(i, first, last):
                    pt_, R_pt_ = pts[i % NB]
                    kc = kcs[i]
                    P.op("pe", lambda e, pt_=pt_, kc=kc, T=T, ac=ac, first=first, last=last: e.matmul(
                        ac[:, 0:T], lhsT=va[:, kc, :], rhs=pt_[:, 0:T], start=first, stop=last), [R_va, R_pt_], [R_ac])
                groups = [list(range(a, min(a + GRP, n))) for a in range(0, n, GRP)]
                for i in groups[0]:
                    score(i)
                for gi, grp in enumerate(groups):
                    if gi + 1 < len(groups):
                        for i in groups[gi + 1]:
                            score(i)
                    for i in reversed(grp):
                        pv(i, first=(gi == 0 and i == grp[-1]), last=(gi == len(groups) - 1 and i == grp[0]))
                rc, R_rc = rec
                o_t, R_ot = otl[qi % 2]
                P.op("dve", lambda e, T=T, ac=ac, rc=rc: e.reciprocal(out=rc[:, 0:T], in_=ac[64:128, 0:T]), [R_ac], [R_rc])
                P.op("dve", lambda e, o_t=o_t, T=T, ac=ac, rc=rc: e.tensor_tensor(out=o_t[:, 0:T], in0=ac[0:64, 0:T], in1=rc[:, 0:T], op=ALU.mult),
                     [R_ac, R_rc], [R_ot])
                P.dma("sp", self.od[h * 64:(h + 1) * 64, q0:q0 + T], o_t[:, 0:T], reads=[R_ot], writes=[self.R_od])

        for h in range(int(_os_environ_get("DBG_NHEAD", 16))):
            do_head(h)
        S.close()
        L.close()
        if int(_os_environ_get("DBG_MLA", 3)) < 3:
            return []
        self.phase_outproj(layer, "mla_w_out", j, need_ctx_out)
        return []

    def phase_diff(self, layer):
        nc, P = self.nc, self.P
        j = layer // 3
        need_ctx_out = layer != DEPTH - 1
        lam_init = 0.8 - 0.6 * math.exp(-0.3 * layer)
        L = ExitStack()
        hT = L.enter_context(nc.sbuf_tensor("hT%d" % layer, [128, 8, NTOK], BF16))
        R_hT = [Res() for _ in range(9)]
        blocks = [(0, 256)] + [(256 + 512 * b, 512) for b in range(8)]
        S = Scope(self)
        A_l, RA_l = self.load_bc(S, layer, 0, 1)
        B_l, RB_l = self.load_bc(S, layer, 0, 0, q="act")
        A_c, RA_c = self.load_bc(S, layer, 1, 1)
        B_c, RB_c = self.load_bc(S, layer, 1, 0, q="act")
        T_ = self.prenorm_tiles(S, bf=True)
        D3 = Defer()
        for gt in range(34):
            if gt < 2:
                hn, R_hn, pt, R_pt = self.prenorm_tile(T_, gt, A_c, RA_c, B_c, RB_c)
                bi = 0
            else:
                hn, R_hn, pt, R_pt = self.prenorm_tile(T_, gt, A_l, RA_l, B_l, RB_l)
                bi = 1 + (gt - 2) // 4
            D3.push(lambda pt=pt, gt=gt, R_pt=R_pt, bi=bi: P.op(
                "dve", lambda e: e.tensor_copy(out=hT[:, :, gt * 128:(gt + 1) * 128], in_=pt[:]), [R_pt], [R_hT[bi]]))
        D3.flush()
        S.close()
        S = Scope(self)
        stage = S.sb([128, 8 * 128], F32, "stg")
        R_stage = Res()
        win = self.W["diff_w_in"][j].rearrange("(k p) n -> p k n", p=128)
        bd = S.sb([128, 128], BF16, "bd")
        R_bd = Res()
        P.op("pool", lambda e: e.memset(bd[:], 0.0), [], [R_bd])
        P.op("dve", lambda e: e.tensor_copy(out=bd[0:64, 0:64], in_=self.ones_b[0:64, 0:64]), [self.R_const, R_bd], [R_bd])
        P.op("dve", lambda e: e.tensor_copy(out=bd[64:128, 64:128], in_=self.ones_b[64:128, 0:64]), [self.R_const, R_bd], [R_bd])
        rm, R_rm = self.load_cast(S, self.c_rm128, [128, 128], stage, R_stage, name="rm128")
        gq = S.sb([128, 1], F32, "gq")
        gk = S.sb([128, 1], F32, "gk")
        gs = S.sb([128, 1], F32, "gs")
        R_gh = Res()
        for half in range(2):
            P.dma("act", gq[half * 64:(half + 1) * 64, :], self.W["diff_qn_g"][j].rearrange("(p o) -> p o", o=1), writes=[R_gh])
            P.dma("act", gk[half * 64:(half + 1) * 64, :], self.W["diff_kn_g"][j].rearrange("(p o) -> p o", o=1), writes=[R_gh])
        P.dma("act", gs[:], self.W["diff_sub_g"][j].rearrange("(p o) -> p o", o=1), writes=[R_gh])
        P.op("dve", lambda e: e.tensor_scalar(out=gs[:], in0=gs[:], scalar1=float(1.0 - lam_init), scalar2=None, op0=ALU.mult),
             [R_gh], [R_gh])
        lv = S.sb([1, 4, 64], F32, "lv")
        lw = S.sb([1, 8], F32, "lw")
        onesf = S.sb([1, 128], F32, "onesf")
        neglam = S.sb([128, 1], F32, "neglam")
        R_lv = Res()
        for n_i, nm in enumerate(("diff_lambda_q1", "diff_lambda_k1", "diff_lambda_q2", "diff_lambda_k2")):
            P.dma("act", lv[0:1, n_i, :], self.W[nm][j:j + 1, :], writes=[R_lv])
        P.op("dve", lambda e: e.memset(onesf[:], 1.0), [], [R_lv])
        P.op("dve", lambda e: e.memset(lw[:], 0.0), [], [R_lv])
        P.op("dve", lambda e: e.tensor_tensor(out=lv[0:1, 0, :], in0=lv[0:1, 0, :], in1=lv[0:1, 1, :], op=ALU.mult), [R_lv], [R_lv])
        P.op("dve", lambda e: e.tensor_tensor(out=lv[0:1, 1, :], in0=lv[0:1, 2, :], in1=lv[0:1, 3, :], op=ALU.mult), [R_lv], [R_lv])
        P.op("dve", lambda e: e.tensor_reduce(out=lw[0:1, 0:2], in_=lv[0:1, 0:2, :], axis=mybir.AxisListType.X, op=ALU.add),
             [R_lv], [R_lv])
        P.op("act", lambda e: e.activation(out=lw[0:1, 2:4], in_=lw[0:1, 0:2], func=AF.Exp), [R_lv], [R_lv])
        P.op("dve", lambda e: e.tensor_tensor(out=lw[0:1, 4:5], in0=lw[0:1, 3:4], in1=lw[0:1, 2:3], op=ALU.subtract), [R_lv], [R_lv])
        P.op("dve", lambda e: e.tensor_scalar(out=lw[0:1, 4:5], in0=lw[0:1, 4:5], scalar1=float(-lam_init), scalar2=None, op0=ALU.add),
             [R_lv], [R_lv])
        Bk = [(S.ps([128, 512], F32, "bk"), Res()) for _ in range(5)]
        sps = [(S.ps([128, 512], F32, "sps"), Res()) for _ in range(3)]
        P.op("pe", lambda e: e.matmul(Bk[4][0][:, 0:2], lhsT=onesf[0:1, :], rhs=lw[0:1, 4:6], start=True, stop=True), [R_lv], [Bk[4][1]])
        P.op("dve", lambda e: e.tensor_copy(out=neglam[:], in_=Bk[4][0][:, 0:1]), [Bk[4][1]], [R_gh])
        Wk = {}
        Wk["sq"] = (S.sb([128, 512], BF16, "sq"), Res())
        Wk["sm"] = Bk[1]
        Wk["r"] = (S.sb([128, 512], F32, "r"), Res())
        Wk["xnf"] = (S.sb([128, 512], F32, "xnf"), Res())
        Wk["xnb"] = (S.sb([128, 512], BF16, "xnb"), Res())
        Wk["rp"] = Bk[2]
        Wk["t1"] = (S.sb([128, 512], F32, "t1"), Res())
        Wk2 = {"sq": (S.sb([128, 512], BF16, "sq2"), Res()), "sm": sps[0], "r": (S.sb([128, 512], F32, "r2"), Res()),
               "xnf": (S.sb([128, 512], F32, "xnf2"), Res()), "xnb": (S.sb([128, 512], BF16, "xnb2"), Res()), "rp": sps[1],
               "t1": (S.sb([128, 512], F32, "t12"), Res())}
        cs = [(S.sb([128, 512], F32, "cos"), S.sb([128, 512], F32, "sin"), Res()) for _ in range(2)]
        QT = (S.sb([128, NTOK], BF16, "QT"), Res())
        KT = (S.sb([128, NTOK], BF16, "KT"), Res())
        Vh = (S.sb([128, 34, 128], BF16, "Vh"), Res())
        NB, GRP = 4, 2
        scb = sps + [Bk[2], Bk[3]]
        pts = [(S.sb([128, 512], BF16, "pt"), Res()) for _ in range(NB)]
        dsum = [(S.sb([128, 512], F32, "dsum"), Res()) for _ in range(2)]
        onesF = S.sb([128, 128], F32, "onesF")
        R_onesF = Res()
        P.op("pool", lambda e: e.memset(onesF[:], 1.0), [], [R_onesF])
        rr = [(S.sb([128, 512], F32, "rr"), Res()) for _ in range(2)]
        uu = [(S.sb([128, 512], F32, "uu"), Res()) for _ in range(2)]
        fin = [(S.sb([128, 512], BF16, "fin"), Res()) for _ in range(2)]
        ncs = [0]
        scale = 64.0 ** -0.5

        wqkv = [(S.sb([128, 8, 128], BF16, "wqkv"), Res()) for _ in range(3)]

        def do_head(h):
            wq, R_wq = self.load_cast(S, win[:, :, h * 128:(h + 1) * 128], [128, 8, 128], stage, R_stage, into=wqkv[0])
            wk, R_wk = self.load_cast(S, win[:, :, D + h * 128:D + (h + 1) * 128], [128, 8, 128], stage, R_stage, into=wqkv[1])
            wv, R_wv = self.load_cast(S, win[:, :, 2 * D + h * 128:2 * D + (h + 1) * 128], [128, 8, 128], stage, R_stage, into=wqkv[2])
            qt, R_qt = QT
            kt, R_kt = KT
            vh, R_vh = Vh
            gp, R_gp = Bk[0]
            for bi, (t0, T) in enumerate(blocks):
                cos_t, sin_t, R_tab = cs[ncs[0] % 2]
                ncs[0] += 1
                P.dma("sp", cos_t[:, 0:T], self.c_cosB[:, t0:t0 + T], writes=[R_tab])
                P.dma("sp", sin_t[:, 0:T], self.c_sinB[:, t0:t0 + T], writes=[R_tab])
                for (wt, R_wt, gcol, dst, R_dst) in ((wk, R_wk, gk, kt, R_kt), (wq, R_wq, gq, qt, R_qt)):
                    for k in range(8):
                        P.op("pe", lambda e, k=k, wt=wt, t0=t0, T=T: e.matmul(gp[:, 0:T], lhsT=wt[:, k, :], rhs=hT[:, k, t0:t0 + T],
                                                                               start=(k == 0), stop=(k == 7)), [R_wt, R_hT[bi]], [R_gp])
                    self.normrope(Wk, gp[:, 0:T], R_gp, 128, T, bd[:, :], 1.0 / 64, gcol[:, 0:1], R_gh,
                                  rm[:, :], R_rm, cos_t[:, 0:T], sin_t[:, 0:T], R_tab, dst[:, t0:t0 + T], R_dst)
                nt = T // 128
                for ti in range(nt):
                    kc = t0 // 128 + ti
                    for k in range(8):
                        P.op("pe", lambda e, k=k, kc=kc, ti=ti: e.matmul(gp[:, ti * 128:(ti + 1) * 128], lhsT=hT[:, k, kc * 128:(kc + 1) * 128],
                                                                          rhs=wv[:, k, :], start=(k == 0), stop=(k == 7)),
                             [R_wv, R_hT[bi]], [R_gp])
                P.op("act", lambda e, t0=t0, nt=nt: e.activation(
                    out=vh[:, t0 // 128:t0 // 128 + nt, :], in_=gp[:, 0:nt * 128].rearrange("p (t c) -> p t c", t=nt),
                    func=AF.Copy), [R_gp], [R_vh])
            for qi, (q0, T, kcs) in enumerate(self.qblocks(need_ctx_out)):
                n = len(kcs)
                seq = [(i, c) for i in range(n) for c in range(2)]

                def score(s_i):
                    i, c = seq[s_i]
                    sp_, R_sp = scb[s_i % NB]
                    kc = kcs[i]
                    P.op("pe", lambda e, sp_=sp_, kc=kc, c=c, T=T, q0=q0: e.matmul(
                        sp_[:, 0:T], lhsT=kt[c * 64:(c + 1) * 64, kc * 128:(kc + 1) * 128], rhs=qt[c * 64:(c + 1) * 64, q0:q0 + T],
                        start=True, stop=True), [R_kt, R_qt], [R_sp])
                    pt_, R_pt_ = pts[s_i % NB]
                    P.op("act", lambda e, sp_=sp_, pt_=pt_, T=T: e.activation(out=pt_[:, 0:T], in_=sp_[:, 0:T], func=AF.Exp, scale=scale),
                         [R_sp], [R_pt_])

                def pv(s_i):
                    i, c = seq[s_i]
                    pt_, R_pt_ = pts[s_i % NB]
                    kc = kcs[i]
                    ao, R_ao = Bk[c]
                    ds_, R_ds = dsum[c]
                    P.op("pe", lambda e, pt_=pt_, kc=kc, i=i, T=T, n=n, ao=ao: e.matmul(
                        ao[:, 0:T], lhsT=vh[:, kc, :], rhs=pt_[:, 0:T], start=(i == 0), stop=(i == n - 1)), [R_vh, R_pt_], [R_ao])
                    eng = "dve" if c == 0 else "pool"
                    if i == 0:
                        P.op(eng, lambda e, pt_=pt_, ds_=ds_, T=T: e.tensor_copy(out=ds_[:, 0:T], in_=pt_[:, 0:T]), [R_pt_], [R_ds])
                    else:
                        P.op(eng, lambda e, pt_=pt_, ds_=ds_, T=T: e.tensor_tensor(out=ds_[:, 0:T], in0=ds_[:, 0:T], in1=pt_[:, 0:T], op=ALU.add),
                             [R_pt_, R_ds], [R_ds])
                ns = len(seq)
                groups = [list(range(a_, min(a_ + GRP, ns))) for a_ in range(0, ns, GRP)]
                for s_i in groups[0]:
                    score(s_i)
                for gi, grp in enumerate(groups):
                    if gi + 1 < len(groups):
                        for s_i in groups[gi + 1]:
                            score(s_i)
                    for s_i in reversed(grp):
                        pv(s_i)
                sm_, R_sm = Bk[4]
                for c in range(2):
                    r_, R_r = rr[c]
                    u_, R_u = uu[c]
                    ao, R_ao = Bk[c]
                    ds_, R_ds = dsum[c]
                    P.op("pe", lambda e, ds_=ds_, T=T: e.matmul(sm_[:, 0:T], lhsT=onesF[:, :], rhs=ds_[:, 0:T], start=True, stop=True),
                         [R_ds, R_onesF], [R_sm])
                    P.op("dve", lambda e, r_=r_, T=T: e.reciprocal(out=r_[:, 0:T], in_=sm_[:, 0:T]), [R_sm], [R_r])
                    P.op("dve", lambda e, u_=u_, ao=ao, r_=r_, T=T: e.tensor_tensor(out=u_[:, 0:T], in0=ao[:, 0:T], in1=r_[:, 0:T], op=ALU.mult),
                         [R_ao, R_r], [R_u])
                u0, R_u0 = uu[0]
                u1, R_u1 = uu[1]
                P.op("dve", lambda e, u0=u0, u1=u1, T=T: e.scalar_tensor_tensor(out=u0[:, 0:T], in0=u1[:, 0:T], scalar=neglam[:, 0:1],
                                                                              in1=u0[:, 0:T], op0=ALU.mult, op1=ALU.add),
                     [R_u0, R_u1, R_gh], [R_u0])
                sq_, R_sq = Wk["sq"]
                sm_, R_sm = Bk[4]
                r_, R_r = rr[0]
                f_, R_f = fin[qi % 2]
                P.op("act", lambda e, u0=u0, sq_=sq_, T=T: e.activation(out=sq_[:, 0:T], in_=u0[:, 0:T], func=AF.Square), [R_u0], [R_sq])
                P.op("pe", lambda e, sq_=sq_, sm_=sm_, T=T: e.matmul(sm_[:, 0:T], lhsT=self.ones_b[:, :], rhs=sq_[:, 0:T], start=True, stop=True),
                     [R_sq, self.R_const], [R_sm])
                P.op("act", lambda e, r_=r_, sm_=sm_, T=T: e.activation(out=r_[:, 0:T], in_=sm_[:, 0:T], func=AF.Sqrt,
                                                                        bias=self.eps_t[:, 0:1], scale=1.0 / 128), [R_sm, self.R_const], [R_r])
                P.op("dve", lambda e, r_=r_, T=T: e.reciprocal(out=r_[:, 0:T], in_=r_[:, 0:T]), [R_r], [R_r])
                P.op("dve", lambda e, u0=u0, r_=r_, f_=f_, T=T: e.scalar_tensor_tensor(out=f_[:, 0:T], in0=u0[:, 0:T], scalar=gs[:, 0:1],
                                                                                    in1=r_[:, 0:T], op0=ALU.mult, op1=ALU.mult),
                     [R_u0, R_r, R_gh], [R_f])
                P.dma("sp", self.od[h * 128:(h + 1) * 128, q0:q0 + T], f_[:, 0:T], reads=[R_f], writes=[self.R_od])

        for h in range(int(_os_environ_get("DBG_NHEAD", 8))):
            do_head(h)
        S.close()
        L.close()
        self.phase_outproj(layer, "diff_w_out", j, need_ctx_out)
        return []

    def gelu_tile(self, G, z, R_z, out_ap, R_out, shape):
        P = self.P
        i = G["i"]
        G["i"] += 1
        s2, R_s2 = G["s2"][i % 2]
        w, R_w = G["w"][i % 2]
        sl = tuple(slice(0, d_) for d_ in shape)
        P.op("act", lambda e: e.activation(out=s2[sl], in_=z, func=AF.Square), [R_z], [R_s2])
        P.op("pool", lambda e: e.tensor_scalar(out=w[sl], in0=s2[sl], scalar1=0.044715, scalar2=1.0, op0=ALU.mult, op1=ALU.add),
             [R_s2], [R_w])
        P.op("dve", lambda e: e.tensor_tensor(out=w[sl], in0=w[sl], in1=z, op=ALU.mult), [R_w, R_z], [R_w])
        P.op("act", lambda e: e.activation(out=s2[sl], in_=w[sl], func=AF.Sigmoid, scale=1.5957691216057308), [R_w, R_s2], [R_s2])
        P.op("dve", lambda e: e.tensor_tensor(out=out_ap, in0=s2[sl], in1=z, op=ALU.mult), [R_s2, R_z], [R_out])

    def phase_chunk(self, layer):
        nc, P = self.nc, self.P
        j = layer // 3
        need_ctx_out = layer != DEPTH - 1
        gts_all = list(range(0 if need_ctx_out else 2, 34))
        blocks = ([(0, 256)] if need_ctx_out else []) + [(256 + 512 * b, 512) for b in range(8)]
        S = Scope(self)
        A_l, RA_l = self.load_bc(S, layer, 0, 1)
        B_l, RB_l = self.load_bc(S, layer, 0, 0, q="act")
        if need_ctx_out:
            A_c, RA_c = self.load_bc(S, layer, 1, 1)
            B_c, RB_c = self.load_bc(S, layer, 1, 0, q="act")
        T_ = self.prenorm_tiles(S, npt=2)
        stage = S.sb([128, 8 * 512], F32, "stg")
        R_stage = Res()
        win = S.sb([128, 8, 4096], BF16, "win")
        R_win = Res()
        wv_ = self.W["sg_w_in"][j].rearrange("(k p) n -> p k n", p=128)
        for c0 in range(0, 4096, 512):
            self.load_cast(S, wv_[:, :, c0:c0 + 512], [128, 8, 512], stage, R_stage, eng=("act" if (c0 // 512) % 2 else "dve"),
                           into=(win[:, :, c0:c0 + 512], R_win))
        wsn = stage[:, 0:1024].rearrange("p (g q) -> p g q", g=8)
        P.dma("sp", wsn, self.W["sg_w_s"][j].rearrange("g p q -> p g q"), writes=[R_stage])
        wsT = S.sb([128, 8, 128], BF16, "wsT")
        R_ws = Res()
        pt0, R_pt0 = T_["pt"][0]
        for g in range(8):
            P.op("pe", lambda e, g=g: e.transpose(out=pt0[:, g, :], in_=wsn[:, g, :], identity=self.ident_f[:]),
                 [R_stage, self.R_const], [R_pt0])
        P.op("dve", lambda e: e.tensor_copy(out=wsT[:], in_=pt0[:]), [R_pt0], [R_ws])
        bsb = S.sb([128, 8, 2, 128], F32, "bsb")
        R_bsb = Res()
        bsrc = self.W["sg_b_s"][j:j + 1].rearrange("o g p -> o (g p)").partition_broadcast(128).rearrange("q o (g p) -> q (o g) p", g=8)
        for r_ in range(2):
            P.dma("act", bsb[:, :, r_, :], bsrc, writes=[R_bsb])
        bsb16 = bsb[:, :, :, :].rearrange("q g r p -> q (g r) p")
        lng = S.sb([128, 2048], F32, "lng")
        lnb = S.sb([128, 2048], F32, "lnb")
        R_ln = Res()
        P.dma("act", lng[:], self.W["sg_ln_g"][j:j + 1, :].partition_broadcast(128), writes=[R_ln])
        P.dma("act", lnb[:], self.W["sg_ln_b"][j:j + 1, :].partition_broadcast(128), writes=[R_ln])
        hTb = (S.sb([128, 8, 512], BF16, "hTb"), Res())
        uT = (S.sb([128, 16, 512], BF16, "uT"), Res())
        vz = (S.sb([128, 2048], F32, "vz"), Res())
        vln = (S.sb([128, 2048], BF16, "vln"), Res())
        G = {"i": 0, "s2": [(S.sb([128, 512], F32, "gs2"), Res()) for _ in range(2)],
             "w": [(S.sb([128, 512], F32, "gw"), Res()) for _ in range(2)]}
        zps = [(S.ps([128, 512], F32, "zp"), Res()) for _ in range(2)]
        spp = [(S.ps([128, 4, 128], F32, "spp"), Res()) for _ in range(2)]
        tt = (S.sb([128, 4, 128], F32, "tt"), Res())
        prodT = [(S.sb([128, 16, 128], BF16, "prodT"), Res()) for _ in range(2)]
        st2 = (S.sb([128, 8], F32, "st2"), Res())
        nz = [0]
        pdv = self.pd.rearrange("(k p) t -> p k t", p=128)
        D4 = Defer()
        for bi, (t0, T) in enumerate(blocks):
            hb_, R_hb = hTb
            nt = T // 128
            for ti in range(nt):
                gt = t0 // 128 + ti
                if gt < 2:
                    hn, R_hn, pt, R_pt = self.prenorm_tile(T_, gt, A_c, RA_c, B_c, RB_c)
                else:
                    hn, R_hn, pt, R_pt = self.prenorm_tile(T_, gt, A_l, RA_l, B_l, RB_l)
                D4.push(lambda pt=pt, ti=ti, R_pt=R_pt: P.op(
                    "dve", lambda e: e.tensor_copy(out=hb_[:, :, ti * 128:(ti + 1) * 128], in_=pt[:]), [R_pt], [R_hb]))
            D4.flush()
            ut, R_ut = uT
            for cc in range(16):
                zp, R_zp = zps[nz[0] % 2]
                nz[0] += 1
                for k in range(8):
                    P.op("pe", lambda e, zp=zp, k=k, cc=cc, T=T: e.matmul(zp[:, 0:T], lhsT=win[:, k, cc * 128:(cc + 1) * 128],
                                                                           rhs=hb_[:, k, 0:T], start=(k == 0), stop=(k == 7)),
                         [R_win, R_hb], [R_zp])
                self.gelu_tile(G, zp[:, 0:T], R_zp, ut[:, cc, 0:T], R_ut, (128, T))
            for ti in range(nt):
                gt = t0 // 128 + ti
                vzt, R_vz = vz
                for nb in range(4):
                    zp, R_zp = zps[nz[0] % 2]
                    nz[0] += 1
                    for k in range(8):
                        P.op("pe", lambda e, zp=zp, k=k, nb=nb, ti=ti: e.matmul(
                            zp[:, :], lhsT=hb_[:, k, ti * 128:(ti + 1) * 128], rhs=win[:, k, 2048 + nb * 512:2048 + (nb + 1) * 512],
                            start=(k == 0), stop=(k == 7)), [R_win, R_hb], [R_zp])
                    self.gelu_tile(G, zp[:, :], R_zp, vzt[:, nb * 512:(nb + 1) * 512], R_vz, (128, 512))
                s_, R_s = st2
                junk, R_junk = T_["junk"]
                P.op("dve", lambda e: e.tensor_reduce(out=s_[:, 0:1], in_=vzt[:, :], axis=mybir.AxisListType.X, op=ALU.add), [R_vz], [R_s])
                for hf in range(2):
                    P.op("act", lambda e, hf=hf: e.activation(out=junk[:, :], in_=vzt[:, hf * 1024:(hf + 1) * 1024], func=AF.Square,
                                                                accum_out=s_[:, 1 + hf:2 + hf]), [R_vz], [R_junk, R_s])
                P.op("dve", lambda e: e.tensor_scalar(out=s_[:, 0:1], in0=s_[:, 0:1], scalar1=1.0 / 2048, scalar2=None, op0=ALU.mult), [R_s], [R_s])
                P.op("dve", lambda e: e.tensor_tensor(out=s_[:, 1:2], in0=s_[:, 1:2], in1=s_[:, 2:3], op=ALU.add), [R_s], [R_s])
                P.op("dve", lambda e: e.tensor_tensor(out=s_[:, 3:4], in0=s_[:, 0:1], in1=s_[:, 0:1], op=ALU.mult), [R_s], [R_s])
                P.op("dve", lambda e: e.scalar_tensor_tensor(out=s_[:, 4:5], in0=s_[:, 1:2], scalar=1.0 / 2048, in1=s_[:, 3:4],
                                                             op0=ALU.mult, op1=ALU.subtract), [R_s], [R_s])
                P.op("act", lambda e: e.activation(out=s_[:, 5:6], in_=s_[:, 4:5], func=AF.Sqrt, bias=self.eps_t[:, 0:1], scale=1.0),
                     [R_s, self.R_const], [R_s])
                P.op("dve", lambda e: e.reciprocal(out=s_[:, 5:6], in_=s_[:, 5:6]), [R_s], [R_s])
                P.op("dve", lambda e: e.scalar_tensor_tensor(out=s_[:, 6:7], in0=s_[:, 0:1], scalar=-1.0, in1=s_[:, 5:6],
                                                             op0=ALU.mult, op1=ALU.mult), [R_s], [R_s])
                P.op("act", lambda e: e.activation(out=vzt[:, :], in_=vzt[:, :], func=AF.Identity, bias=s_[:, 6:7], scale=s_[:, 5:6]),
                     [R_vz, R_s], [R_vz])
                P.op("dve", lambda e: e.tensor_tensor(out=vzt[:, :], in0=vzt[:, :], in1=lng[:, :], op=ALU.mult), [R_vz, R_ln], [R_vz])
                vl, R_vl = vln
                P.op("pool", lambda e: e.tensor_tensor(out=vl[:, :], in0=vzt[:, :], in1=lnb[:, :], op=ALU.add), [R_vz, R_ln], [R_vl])
                pr, R_pr = prodT[gt % 2]
                for m in range(4):
                    sp_, R_sp = spp[m % 2]
                    for q_ in range(4):
                        cc = 4 * m + q_
                        P.op("pe", lambda e, sp_=sp_, q_=q_, cc=cc: e.matmul(sp_[:, q_, :], lhsT=vl[:, cc * 128:(cc + 1) * 128],
                                                                             rhs=wsT[:, cc // 2, :], start=True, stop=True),
                             [R_vl, R_ws], [R_sp])
                    t_, R_t = tt
                    P.op("dve", lambda e, sp_=sp_, m=m: e.tensor_tensor(out=t_[:, :, :], in0=sp_[:, :, :], in1=bsb16[:, 4 * m:4 * m + 4, :], op=ALU.add),
                         [R_sp, R_bsb], [R_t])
                    P.op("pool", lambda e, m=m, ti=ti, pr=pr: e.tensor_tensor(out=pr[:, 4 * m:4 * m + 4, :], in0=t_[:, :, :],
                                                                                in1=ut[:, 4 * m:4 * m + 4, ti * 128:(ti + 1) * 128], op=ALU.mult),
                         [R_t, R_ut], [R_pr])
                P.dma("sp", pdv[:, :, gt * 128:(gt + 1) * 128], pr[:, :, :], reads=[R_pr], writes=[self.R_od])
        S.close()
        self.phase_outproj(layer, "sg_w_out", j, need_ctx_out, src=self.pd, nk=16)
        return []

    def phase_mix(self, layer):
        kind = layer % 3
        if kind == 0:
            return self.phase_mla(layer)
        if kind == 1:
            return self.phase_diff(layer)
        return self.phase_chunk(layer)


_CACHE = {}


def _get_nc(plan):
    key = tuple(plan)
    if key not in _CACHE:
        K = Kern(plan)
        _CACHE[key] = K.build()
    return _CACHE[key]


FULL_PLAN = [(k, l) for l in range(DEPTH) for k in ("mix", "moe")]


def _axial(rot_dim):
    n_rows = NLAT // 64
    rows = np.repeat(np.arange(n_rows, dtype=np.float32), 64)
    cols = np.tile(np.arange(64, dtype=np.float32), n_rows)
    n_freq = rot_dim // 4
    inv_freq = (np.float32(10000.0) ** (-np.arange(n_freq, dtype=np.float32) / np.float32(n_freq))).astype(np.float32)
    ang = np.concatenate([rows[:, None] * inv_freq, cols[:, None] * inv_freq], axis=-1).astype(np.float32)
    return np.cos(ang).astype(np.float32), np.sin(ang).astype(np.float32)


def _host_consts():
    c = {}
    cosa, sina = _axial(32)
    cosA = np.ones((96, NTOK), np.float32)
    sinA = np.zeros((96, NTOK), np.float32)
    cosA[64:80, NCTX:] = cosa.T
    cosA[80:96, NCTX:] = cosa.T
    sinA[64:80, NCTX:] = sina.T
    sinA[80:96, NCTX:] = sina.T
    c["c_cosA"], c["c_sinA"] = cosA, sinA
    rm = np.zeros((96, 96), np.float32)
    for m in range(64, 80):
        rm[m + 16, m] = -1.0
    for m in range(80, 96):
        rm[m - 16, m] = 1.0
    c["c_rm96"] = rm
    cosb, sinb = _axial(64)
    cosB = np.ones((128, NTOK), np.float32)
    sinB = np.zeros((128, NTOK), np.float32)
    rm2 = np.zeros((128, 128), np.float32)
    for blk in range(4):
        cosB[blk * 32:(blk + 1) * 32, NCTX:] = cosb.T
        sinB[blk * 32:(blk + 1) * 32, NCTX:] = sinb.T
    for c0 in (0, 64):
        for m in range(32):
            rm2[c0 + m + 32, c0 + m] = -1.0
            rm2[c0 + m, c0 + m + 32] = 1.0
    c["c_cosB"], c["c_sinB"], c["c_rm128"] = cosB, sinB, rm2
    return c


def make_in_maps(inputs, ncores=8):
    ident = np.eye(128, dtype=np.float32)
    shared = {n: np.ascontiguousarray(np.asarray(inputs[n], dtype=np.float32)) for n, _ in WEIGHT_SPECS}
    shared["c_ident"] = ident
    shared.update(_host_consts())
    maps = []
    c_ctx = np.asarray(inputs["c_ctx"], dtype=np.float32)
    for b in range(ncores):
        c = np.asarray(inputs["c"][b], dtype=np.float32)
        cc = np.stack([c, c_ctx], axis=-1).reshape(8, 128, 2).transpose(1, 0, 2)
        m = dict(shared)
        m["xin"] = np.ascontiguousarray(np.asarray(inputs["x"][b], dtype=np.float32))
        m["cin"] = np.ascontiguousarray(np.asarray(inputs["ctx"][b], dtype=np.float32))
        m["cc"] = np.ascontiguousarray(cc)
        maps.append(m)
    return maps


def kernel(**inputs):
    nc = _get_nc(FULL_PLAN)
    maps = make_in_maps(inputs, 8)
    res = run_bass_kernel_spmd(nc, maps, core_ids=list(range(8)))
    return np.stack([np.asarray(r["xl"], dtype=np.float32) for r in res.results], axis=0)
```
